# Optimizing a Trainium2 kernel written in Bass

```python
import math
import jax, jax.numpy as jnp
from jax import lax
import numpy as np

D_MODEL = 1024
BATCH = 8
SEQ = 2048
DEPTH = 4

HEAD_DIM = 64
ATT_WIDTH = D_MODEL // 2
RWKV_WIDTH = D_MODEL - ATT_WIDTH
N_ATT_HEADS = ATT_WIDTH // HEAD_DIM
N_RWKV_HEADS = RWKV_WIDTH // HEAD_DIM
DILATED_GROUPS = ((128, 1), (512, 4), (2048, 16))
Q_BLOCK = 128
N_BUCKETS = 32
MAX_DISTANCE = 2048
NEG_INF = -1e30
D_DECAY_LORA = 64
D_AAA_LORA = 64
D_MV_LORA = 32
D_GATE_LORA = 160
RWKV_COLS = 3 * RWKV_WIDTH + D_DECAY_LORA + D_AAA_LORA + D_GATE_LORA
RWKV_SPLITS = (RWKV_WIDTH, 2 * RWKV_WIDTH, 3 * RWKV_WIDTH,
               3 * RWKV_WIDTH + D_DECAY_LORA, 3 * RWKV_WIDTH + D_DECAY_LORA + D_AAA_LORA)
N_IN = 3 * ATT_WIDTH + RWKV_COLS
GN_EPS = HEAD_DIM * 1e-5
RMS_EPS = 1e-6
D_FF_DENSE = 2816
N_EXPERTS = 8
TOP_K = 2
D_FF_EXPERT = 3584
MOE_BLOCK = 128
N_DENSE = (DEPTH + 1) // 2
N_MOE = DEPTH // 2

kernel_name = "hymba_dilated_rwkv7_moe_adaln"


def rms_norm(x, g):
    xf = x.astype(jnp.float32)
    y = xf * lax.rsqrt(jnp.mean(xf * xf, axis=-1, keepdims=True) + RMS_EPS)
    return (y * g.astype(jnp.float32)).astype(x.dtype)


def t5_bucket(dist):
    n = np.asarray(dist)
    max_exact = N_BUCKETS // 2
    large = max_exact + (np.log(np.maximum(n, 1) / max_exact) / np.log(MAX_DISTANCE / max_exact)
                         * (N_BUCKETS - max_exact)).astype(np.int32)
    large = np.minimum(large, N_BUCKETS - 1)
    return np.where(n < max_exact, n, large).astype(np.int32)


def swiglu(x, wg, wu, wd):
    return (jax.nn.silu(x @ wg) * (x @ wu)) @ wd


def dilated_attention(q, k, v, rel_bias):
    B, S, H, hd = q.shape
    n_blk = S // Q_BLOCK
    scale = 1.0 / math.sqrt(hd)
    offsets = [np.arange(0, w + 1, d, dtype=np.int32) for (w, d) in DILATED_GROUPS]
    biases = [rel_bias[t5_bucket(off)].T.astype(jnp.float32) for off in offsets]

    def block(i):
        t = i * Q_BLOCK + jnp.arange(Q_BLOCK, dtype=jnp.int32)
        qb = lax.dynamic_slice_in_dim(q, i * Q_BLOCK, Q_BLOCK, axis=1) * scale
        outs, lses = [], []
        for off, bias in zip(offsets, biases):
            idx = t[:, None] - off[None, :]
            valid = idx >= 0
            idxc = jnp.maximum(idx, 0)
            kg = jnp.take(k, idxc, axis=1)
            vg = jnp.take(v, idxc, axis=1)
            s = jnp.einsum('bqhd,bqnhd->bhqn', qb, kg).astype(jnp.float32) + bias[None, :, None, :]
            s = jnp.where(valid[None, None], s, NEG_INF)
            lse = jax.nn.logsumexp(s, axis=-1)
            p = jnp.exp(s - lse[..., None])
            outs.append(jnp.einsum('bhqn,bqnhd->bqhd', p.astype(v.dtype), vg))
            lses.append(lse)
        wts = jax.nn.softmax(jnp.stack(lses), axis=0)
        wts = jnp.transpose(wts, (0, 1, 3, 2))[..., None]
        return jnp.sum(wts.astype(v.dtype) * jnp.stack(outs), axis=0)

    out = lax.map(block, jnp.arange(n_blk, dtype=jnp.int32))
    return jnp.transpose(out, (1, 0, 2, 3, 4)).reshape(B, S, H * hd)


def rwkv7_time_mix(p, v_first, v_res, mu, w0, w2, a0, a2, g2, k_k, k_a, r_k, ln_w, ln_b):
    B, S, _ = p.shape
    p_prev = jnp.pad(p, ((0, 0), (1, 0), (0, 0)))[:, :S]
    p = p + mu * (p_prev - p)
    r, k, v, wd, ad, gd = jnp.split(p, RWKV_SPLITS, axis=-1)
    w = -jax.nn.softplus(-(w0 + jnp.tanh(wd) @ w2)) - 0.5
    a = jax.nn.sigmoid(a0 + ad @ a2)
    g = jax.nn.sigmoid(gd) @ g2
    if v_res is None:
        v_first = v
    else:
        v0, v1, v2 = v_res
        v = v + (v_first - v) * jax.nn.sigmoid(v0 + (v @ v1) @ v2)

    def heads(t):
        return t.reshape(B, S, N_RWKV_HEADS, HEAD_DIM).astype(jnp.float32)

    kk = heads(k * k_k)
    kk = kk / jnp.maximum(jnp.linalg.norm(kk, axis=-1, keepdims=True), 1e-12)
    k = k * (1.0 + (a - 1.0) * k_a)
    rh, kh, vh, ah = heads(r), heads(k), heads(v), heads(a)
    decay = jnp.exp(-jnp.exp(heads(w)))

    def step(state, inp):
        r_t, w_t, k_t, v_t, kk_t, a_t = inp
        sa = jnp.einsum('bhvk,bhk->bhv', state, -kk_t)
        state = (state * w_t[:, :, None, :] + sa[..., None] * (kk_t * a_t)[:, :, None, :]
                 + v_t[..., None] * k_t[:, :, None, :])
        return state, jnp.einsum('bhvk,bhk->bhv', state, r_t)

    tm = lambda t: jnp.moveaxis(t, 1, 0)
    s0 = jnp.zeros((B, N_RWKV_HEADS, HEAD_DIM, HEAD_DIM), jnp.float32)
    _, y = lax.scan(step, s0, (tm(rh), tm(decay), tm(kh), tm(vh), tm(kk), tm(ah)))
    y = jnp.moveaxis(y, 0, 1)
    mean = jnp.mean(y, axis=-1, keepdims=True)
    var = jnp.mean(jnp.square(y - mean), axis=-1, keepdims=True)
    yn = ((y - mean) * lax.rsqrt(var + GN_EPS)).reshape(B, S, RWKV_WIDTH)
    yn = yn * ln_w.astype(jnp.float32) + ln_b.astype(jnp.float32)
    bonus = (jnp.sum(rh * kh * r_k.astype(jnp.float32), axis=-1, keepdims=True) * vh).reshape(B, S, RWKV_WIDTH)
    out = (yn + bonus) * g.astype(jnp.float32)
    return out.astype(p.dtype), v_first


def moe_swiglu(h, router_w, router_b, w_gate, w_up, w_down):
    B, S, D = h.shape
    T = B * S
    hf = h.reshape(T, D)
    logits = (hf @ router_w).astype(jnp.float32) + router_b.astype(jnp.float32)
    top_logits, top_idx = lax.top_k(logits, TOP_K)
    gates = jax.nn.softmax(top_logits, axis=-1)
    TK = T * TOP_K
    flat_e = top_idx.reshape(TK)
    flat_tok = jnp.arange(TK, dtype=jnp.int32) // TOP_K
    flat_gate = gates.reshape(TK)
    order = jnp.argsort(flat_e)
    sorted_e = flat_e[order]
    counts = jnp.zeros((N_EXPERTS,), jnp.int32).at[flat_e].add(1)
    starts = jnp.cumsum(counts) - counts
    padded = (counts + MOE_BLOCK - 1) // MOE_BLOCK * MOE_BLOCK
    pad_ends = jnp.cumsum(padded)
    pad_starts = pad_ends - padded
    dest = pad_starts[sorted_e] + jnp.arange(TK, dtype=jnp.int32) - starts[sorted_e]
    n_blocks = -(-TK // MOE_BLOCK) + N_EXPERTS
    n_rows = n_blocks * MOE_BLOCK
    row_tok = jnp.full((n_rows,), T, jnp.int32).at[dest].set(flat_tok[order])
    row_gate = jnp.zeros((n_rows,), jnp.float32).at[dest].set(flat_gate[order])
    block_start = jnp.arange(n_blocks, dtype=jnp.int32) * MOE_BLOCK
    block_e = jnp.minimum(jnp.searchsorted(pad_ends, block_start, side='right'), N_EXPERTS - 1)
    x_rows = jnp.concatenate([hf, jnp.zeros((1, D), hf.dtype)], axis=0)[row_tok]
    x_rows = x_rows.reshape(n_blocks, MOE_BLOCK, D)

    def expert_block(args):
        xb, e = args
        return swiglu(xb, w_gate[e], w_up[e], w_down[e])

    y_rows = lax.map(expert_block, (x_rows, block_e)).reshape(n_rows, D)
    y_rows = (y_rows.astype(jnp.float32) * row_gate[:, None]).astype(h.dtype)
    out = jnp.zeros((T + 1, D), h.dtype).at[row_tok].add(y_rows)
    return out[:T].reshape(B, S, D)


def setup_inputs(seed: int = 0) -> dict:
    key = jax.random.key(seed)
    ks = iter(jax.random.split(key, 40))
    f32 = jnp.float32

    def nrm(shape, scale):
        return jax.random.normal(next(ks), shape, f32) * scale

    def unif(shape, lo, hi):
        return jax.random.uniform(next(ks), shape, f32, lo, hi)

    D, RW, L = D_MODEL, RWKV_WIDTH, DEPTH
    return {
        "x": nrm((BATCH, SEQ, D), 1.0),
        "c": nrm((BATCH, D), 1.0),
        "w_ada": nrm((L, D, 6 * D), 0.5 * D ** -0.5),
        "b_ada": nrm((L, 6 * D), 0.02),
        "norm1_g": 1.0 + nrm((L, D), 0.05),
        "norm2_g": 1.0 + nrm((L, D), 0.05),
        "final_g": 1.0 + nrm((D,), 0.05),
        "w_in": nrm((L, D, N_IN), D ** -0.5),
        "w_out": nrm((L, D, D), D ** -0.5),
        "rel_bias": nrm((N_BUCKETS, N_ATT_HEADS), 0.3),
        "rwkv_mu": unif((L, RWKV_COLS), 0.0, 1.0),
        "rwkv_w0": unif((L, RW), -5.0, -0.5),
        "rwkv_w2": nrm((L, D_DECAY_LORA, RW), 0.3 * D_DECAY_LORA ** -0.5),
        "rwkv_a0": nrm((L, RW), 0.3),
        "rwkv_a2": nrm((L, D_AAA_LORA, RW), 0.3 * D_AAA_LORA ** -0.5),
        "rwkv_g2": nrm((L, D_GATE_LORA, RW), D_GATE_LORA ** -0.5),
        "rwkv_k_k": 0.85 + nrm((L, RW), 0.05),
        "rwkv_k_a": 1.0 + nrm((L, RW), 0.05),
        "rwkv_r_k": nrm((L, N_RWKV_HEADS, HEAD_DIM), 0.1),
        "rwkv_ln_w": 1.0 + nrm((L, RW), 0.05),
        "rwkv_ln_b": nrm((L, RW), 0.02),
        "rwkv_v0": nrm((L - 1, RW), 0.3),
        "rwkv_v1": nrm((L - 1, RW, D_MV_LORA), RW ** -0.5),
        "rwkv_v2": nrm((L - 1, D_MV_LORA, RW), 0.3 * D_MV_LORA ** -0.5),
        "ffn_w_gate": nrm((N_DENSE, D, D_FF_DENSE), D ** -0.5),
        "ffn_w_up": nrm((N_DENSE, D, D_FF_DENSE), D ** -0.5),
        "ffn_w_down": nrm((N_DENSE, D_FF_DENSE, D), D_FF_DENSE ** -0.5),
        "moe_router_w": nrm((N_MOE, D, N_EXPERTS), D ** -0.5),
        "moe_router_b": nrm((N_MOE, N_EXPERTS), 0.01),
        "moe_w_gate": nrm((N_MOE, N_EXPERTS, D, D_FF_EXPERT), D ** -0.5),
        "moe_w_up": nrm((N_MOE, N_EXPERTS, D, D_FF_EXPERT), D ** -0.5),
        "moe_w_down": nrm((N_MOE, N_EXPERTS, D_FF_EXPERT, D), D_FF_EXPERT ** -0.5),
    }


def reference(x, c, w_ada, b_ada, norm1_g, norm2_g, final_g, w_in, w_out, rel_bias,
              rwkv_mu, rwkv_w0, rwkv_w2, rwkv_a0, rwkv_a2, rwkv_g2, rwkv_k_k, rwkv_k_a,
              rwkv_r_k, rwkv_ln_w, rwkv_ln_b, rwkv_v0, rwkv_v1, rwkv_v2,
              ffn_w_gate, ffn_w_up, ffn_w_down,
              moe_router_w, moe_router_b, moe_w_gate, moe_w_up, moe_w_down):
    B, S, D = x.shape
    c_act = jax.nn.silu(c)
    v_first = None
    for l in range(DEPTH):
        mod = c_act @ w_ada[l] + b_ada[l]
        sh1, sc1, g1, sh2, sc2, g2 = [m[:, None, :] for m in jnp.split(mod, 6, axis=-1)]

        h = rms_norm(x, norm1_g[l]) * (1.0 + sc1) + sh1
        proj = h @ w_in[l]
        q = proj[..., :ATT_WIDTH].reshape(B, S, N_ATT_HEADS, HEAD_DIM)
        k = proj[..., ATT_WIDTH:2 * ATT_WIDTH].reshape(B, S, N_ATT_HEADS, HEAD_DIM)
        v = proj[..., 2 * ATT_WIDTH:3 * ATT_WIDTH].reshape(B, S, N_ATT_HEADS, HEAD_DIM)
        att = dilated_attention(q, k, v, rel_bias)
        v_res = None if l == 0 else (rwkv_v0[l - 1], rwkv_v1[l - 1], rwkv_v2[l - 1])
        rw, v_first = rwkv7_time_mix(proj[..., 3 * ATT_WIDTH:], v_first, v_res,
                                     rwkv_mu[l], rwkv_w0[l], rwkv_w2[l], rwkv_a0[l], rwkv_a2[l],
                                     rwkv_g2[l], rwkv_k_k[l], rwkv_k_a[l], rwkv_r_k[l],
                                     rwkv_ln_w[l], rwkv_ln_b[l])
        mix = jnp.concatenate([att, rw], axis=-1) @ w_out[l]
        x = x + g1 * mix

        h = rms_norm(x, norm2_g[l]) * (1.0 + sc2) + sh2
        i = l // 2
        if l % 2 == 0:
            ff = swiglu(h, ffn_w_gate[i], ffn_w_up[i], ffn_w_down[i])
        else:
            ff = moe_swiglu(h, moe_router_w[i], moe_router_b[i], moe_w_gate[i], moe_w_up[i], moe_w_down[i])
        x = x + g2 * ff
    return rms_norm(x, final_g)
```

```python
import contextlib
import os
import math
import numpy as np
import concourse.bass as bass
import concourse.mybir as mybir
from concourse.bass_utils import run_bass_kernel_spmd

F32 = mybir.dt.float32
BF16 = mybir.dt.bfloat16
AF = mybir.ActivationFunctionType
ALU = mybir.AluOpType
AX = mybir.AxisListType

S = 2048
D = 1024
NT = 16
DEPTH = 4
HD = 64
AW = 512
RW = 512
NIN = 3360
DFF = 2816
DFE = 3584
NE = 8
RMS_EPS = 1e-6
GN_EPS = 64 * 1e-5
N_DSEM = 48


class Tk:
    __slots__ = ("w", "r")

    def __init__(self):
        self.w = None
        self.r = {}


class Sch:
    def __init__(self, nc, es):
        self.nc = nc
        self.es = es
        self.eng = {"pe": nc.tensor, "dve": nc.vector, "act": nc.scalar, "pool": nc.gpsimd, "sp": nc.sync}
        self.sem = {k: es.enter_context(nc.semaphore("s_" + k)) for k in self.eng}
        self.cnt = {k: 0 for k in self.eng}
        self.waited = {k: {} for k in self.eng}
        self.dsem = [es.enter_context(nc.semaphore("d%d" % i)) for i in range(N_DSEM)]
        self.dval = [0] * N_DSEM
        self.dnext = {"sp": 0, "pool": 0, "act": 0}
        self.drange = {"sp": (0, N_DSEM // 2), "act": (0, N_DSEM // 2), "pool": (N_DSEM // 2, N_DSEM)}
        self.nins = 0

    def _semof(self, key):
        if isinstance(key, tuple):
            return self.dsem[key[1]]
        return self.sem[key]

    def _wait(self, e, deps):
        w = self.waited[e]
        for key, c in deps.items():
            if w.get(key, 0) < c:
                self.eng[e].wait_ge(self._semof(key), c)
                w[key] = c
                self.nins += 1

    def _deps(self, e, r, w):
        deps = {}

        def add(key, c):
            if deps.get(key, 0) < c:
                deps[key] = c
        for t in r:
            if t.w is not None:
                add(*t.w)
        for t in w:
            if t.w is not None and not (t.w[0] == e and e == "pe"):
                add(*t.w)
            for key, c in t.r.items():
                add(key, c)
        return deps

    def op(self, e, fn, r=(), w=()):
        self._wait(e, self._deps(e, r, w))
        ins = fn(self.eng[e])
        self.cnt[e] += 1
        c = self.cnt[e]
        ins.then_inc(self.sem[e], 1)
        self.nins += 1
        for t in r:
            t.r[e] = c
        for t in w:
            t.w = (e, c)
            t.r = {}
        return ins

    def dma(self, q, out, in_, r=(), w=(), **kw):
        lo, hi = self.drange[q]
        i = lo + self.dnext[q]
        self.dnext[q] = (self.dnext[q] + 1) % (hi - lo)
        key = ("d", i)
        deps = self._deps(key, r, w)
        if self.dval[i] > 0:
            deps[key] = max(deps.get(key, 0), self.dval[i])
        self._wait(q, deps)
        self.dval[i] += 16
        c = self.dval[i]
        self.eng[q].dma_start(out=out, in_=in_, **kw).then_inc(self.dsem[i], 16)
        self.nins += 1
        for t in r:
            t.r[key] = c
        for t in w:
            t.w = (key, c)
            t.r = {}

    def wait_all(self, e, tks):
        deps = {}
        for t in tks:
            if t.w is not None:
                if deps.get(t.w[0], 0) < t.w[1]:
                    deps[t.w[0]] = t.w[1]
        self._wait(e, deps)


class Buf:
    def __init__(self, t, n=1):
        self.t = t
        self.k = [Tk() for _ in range(n)]

    def __getitem__(self, idx):
        return self.t[idx]


def t5_bucket(n):
    n = np.asarray(n)
    max_exact = 16
    large = max_exact + (np.log(np.maximum(n, 1) / max_exact) / np.log(2048 / max_exact) * 16).astype(np.int32)
    large = np.minimum(large, 31)
    return np.where(n < max_exact, n, large).astype(np.int32)


def host_consts():
    c = {}
    c["ident"] = np.eye(128, dtype=np.float32)
    j = np.arange(128)[:, None]
    t = np.arange(128)[None, :]
    mus = (j < t).astype(np.float32)
    mui = (j <= t).astype(np.float32)
    c["mu2"] = np.concatenate([mus, mui], axis=1)
    c["mls"] = (j > t).astype(np.float32)
    sm = np.ones((128, S), np.float32)
    sm[:, ::128] = 0.0
    c["scanmask"] = sm
    p = np.arange(128)
    c["headsel"] = np.stack([(p < 64), (p >= 64)], axis=1).astype(np.float32)
    c["blockones"] = ((p[:, None] // 64) == (p[None, :] // 64)).astype(np.float32)
    s_ = np.arange(128)[:, None]
    u_ = np.arange(S)[None, :]
    d = u_ - s_
    mult = ((d >= 0) & (d <= 128)).astype(np.float32) + ((d >= 0) & (d % 4 == 0) & (d <= 512)).astype(np.float32) \
        + ((d >= 0) & (d % 16 == 0) & (d <= 2048)).astype(np.float32)
    c["amult"] = mult.astype(np.float32)
    c["_bucket"] = t5_bucket(np.maximum(d, 0))
    return c


class Ctx:
    pass


def build(nc, layers=(0, 1, 2, 3), taps=(), final=True, upto=None):
    es = contextlib.ExitStack()
    C = Ctx()
    C.nc = nc
    C.es = es
    C.taps = {}
    with es:
        _build(C, layers, taps, final, upto)
    return C


def _dram_in(nc, name, shape, dt=F32):
    return nc.dram_tensor(name, list(shape), dt, kind="ExternalInput").ap()


def _build(C, layers, taps, final, upto):
    nc = C.nc
    es = C.es
    sc = Sch(nc, es)
    C.sc = sc

    def sb(name, shape, dt, n=1):
        return Buf(es.enter_context(nc.sbuf_tensor(name, list(shape), dt)), n)

    def ps(name, shape, dt=F32):
        return Buf(es.enter_context(nc.psum_tensor(name, list(shape), dt)), 1)

    def dscr(name, shape, dt=F32, n=1):
        return Buf(nc.dram_tensor(name, list(shape), dt).ap(), n)

    def tap(name, shape):
        if name in taps:
            C.taps[name] = nc.dram_tensor("tap_" + name, list(shape), F32, kind="ExternalOutput").ap()
            return C.taps[name]
        return None

    x_in = _dram_in(nc, "x", [S, D])
    c_in = _dram_in(nc, "c", [D])
    cst = {k: _dram_in(nc, "k_" + k, v.shape) for k, v in host_consts().items() if not k.startswith("_")}
    abias_in = _dram_in(nc, "abias", [128, 8, S])
    final_g_in = _dram_in(nc, "final_g", [D])
    out_d = nc.dram_tensor("out", [S, D], F32, kind="ExternalOutput").ap()
    W = {}
    for l in layers:
        W[l] = {}
        def di(nm, shape):
            W[l][nm] = _dram_in(nc, "%s_%d" % (nm, l), shape)
        di("w_ada", [D, 6 * D]); di("b_ada", [6 * D]); di("norm1_g", [D]); di("norm2_g", [D])
        di("w_in", [D, NIN]); di("w_out", [D, D]); di("mu", [1824])
        di("w0", [RW]); di("w2", [64, RW]); di("a0", [RW]); di("a2", [64, RW]); di("g2", [160, RW])
        di("k_k", [RW]); di("k_a", [RW]); di("r_k", [RW]); di("ln_w", [RW]); di("ln_b", [RW])
        if l > 0:
            di("v0", [RW]); di("v1", [RW, 32]); di("v2", [32, RW])
        if l % 2 == 0:
            di("wg", [D, DFF]); di("wu", [D, DFF]); di("wd", [DFF, D])
        else:
            di("rw", [NE, D]); di("rb", [NE]); di("wg", [NE, D, DFE]); di("wu", [NE, D, DFE]); di("wd", [NE, DFE, D])

    xs_d = dscr("xs_d", [NT, 128, D], F32, NT)
    rT_d = dscr("rT_d", [4, 128, S], F32, 4)
    kT_d = dscr("kT_d", [4, 128, S], F32, 4)
    y_d = dscr("y_d", [NT, 128, RW], F32, NT)
    vf_d = dscr("vf_d", [NT, 128, RW], F32, NT)

    identb = sb("identb", [128, 128], BF16)
    mu2 = sb("mu2", [128, 256], F32)
    mls = sb("mls", [128, 128], F32)
    headsel = sb("headsel", [128, 2], BF16)
    blockones = sb("blockones", [128, 128], BF16)
    onesrow = sb("onesrow", [1, 128], BF16)
    cact = sb("cact", [128, 8], F32)
    cactb = sb("cactb", [128, 8, 128], BF16)
    modbc = sb("modbc", [128, 3 * D], F32)
    pcol = sb("pcol", [128, 8, 4], F32)
    rowb = sb("rowb", [1, 512], BF16)
    wbufs = [sb("wbuf%d" % i, [128, 8, 512], BF16, 2) for i in range(6)]
    C.wnext = 0
    ARENA = 143360
    arena_t = es.enter_context(nc.sbuf_tensor("arena", [128, ARENA // 2], BF16))
    amask_d = dscr("amask_d", [128, 8 * S], BF16)
    vr_d = dscr("vr_d", [NT, 128, RW], BF16, NT)
    prod_d = dscr("prod_d", [4, 128, S], BF16, 4)
    C.layers = list(layers)
    C.first_from_input = True
    C.tapx = {}
    C.tapo = {}
    for nm in taps:
        if nm.startswith("xmid"):
            C.tapx[int(nm[4:])] = nc.dram_tensor("tap_" + nm, [S, D], F32, kind="ExternalOutput").ap()
        if nm.startswith("xout"):
            C.tapo[int(nm[4:])] = nc.dram_tensor("tap_" + nm, [S, D], F32, kind="ExternalOutput").ap()
    psF = [ps("psF%d" % i, [128, 512], F32) for i in range(8)]
    psB = []
    for i in range(2):
        b_ = Buf(psF[6 + i].t[:, :].bitcast(BF16).rearrange("p (a b) -> p a b", a=8), 1)
        b_.k = psF[6 + i].k
        psB.append(b_)
    C.pf = 0

    def carve(off, shape, dt):
        n = 1
        for d_ in shape[1:]:
            n *= d_
        esz = 4 if dt == F32 else 2
        assert off % 4 == 0 and off + n * esz <= ARENA, (off, shape)
        ap = arena_t[0:shape[0], off // 2: off // 2 + n * esz // 2]
        if dt == F32:
            ap = ap.bitcast(F32)
        if len(shape) == 3:
            ap = ap.rearrange("p (a b) -> p a b", a=shape[1])
        elif len(shape) == 4:
            ap = ap.rearrange("p (a b c) -> p a b c", a=shape[1], b=shape[2])
        return Buf(ap, 1)

    class Lay:
        def __init__(self):
            self.off = 0

        def get(self, shape, dt, n=1):
            nb = (4 if dt == F32 else 2)
            for d_ in shape[1:]:
                nb *= d_
            nb = (nb + 31) // 32 * 32
            b_ = carve(self.off, shape, dt)
            b_.k = [Tk() for _ in range(n)]
            self.off += nb
            return b_

    def barrier():
        engs = list(sc.eng.keys())
        for e in engs:
            deps = {}
            for o in engs:
                if o != e and sc.cnt[o] > 0:
                    deps[o] = sc.cnt[o]
            for i in range(N_DSEM):
                if sc.dval[i] > 0:
                    deps[("d", i)] = sc.dval[i]
            sc._wait(e, deps)

    def nextw():
        b = wbufs[C.wnext]
        C.wnext = (C.wnext + 1) % len(wbufs)
        return b

    def nextps(lo=0, hi=6):
        C.pf = (C.pf + 1) % (hi - lo)
        return psF[lo + C.pf]

    op = sc.op
    dma = sc.dma

    dma("pool", identb[:], cst["ident"], w=identb.k)
    dma("sp", mu2[:], cst["mu2"], w=mu2.k)
    dma("sp", mls[:], cst["mls"], w=mls.k)
    dma("pool", headsel[:], cst["headsel"], w=headsel.k)
    dma("pool", blockones[:], cst["blockones"], w=blockones.k)
    op("dve", lambda e: e.memset(onesrow[:], 1.0), w=onesrow.k)
    L0 = Lay()
    amt = L0.get([128, S], F32)
    amm = L0.get([128, S], F32)
    amo = L0.get([128, S], BF16)
    dma("sp", amm[:], cst["amult"], w=amm.k)
    for h in range(8):
        dma("sp", amt[:], abias_in[:, h, :], w=amt.k)
        op("act", lambda e: e.activation(out=amt[:], in_=amt[:], func=AF.Exp), r=amt.k, w=amt.k)
        op("dve", lambda e: e.tensor_tensor(out=amo[:], in0=amt[:], in1=amm[:], op=ALU.mult), r=amt.k + amm.k, w=amo.k)
        dma("sp", amask_d[:, h * S:(h + 1) * S], amo[:], r=amo.k, w=amask_d.k)
    dma("sp", cact[:], c_in.rearrange("(c p) -> p c", p=128), w=cact.k, allow_slow_non_contiguous=True)
    op("act", lambda e: e.activation(out=cact[:], in_=cact[:], func=AF.Silu), r=cact.k, w=cact.k)
    op("dve", lambda e: e.tensor_copy(out=cactb[:], in_=cact[:].unsqueeze(2).to_broadcast([128, 8, 128])), r=cact.k, w=cactb.k)

    barrier()
    C.__dict__.update(locals())
    for li, l in enumerate(layers):
        _layer(C, l, first=(li == 0))
        if upto is not None and l == upto[0]:
            break
    if final:
        _final(C)
    for i in range(N_DSEM):
        if sc.dval[i]:
            nc.sync.wait_ge(sc.dsem[i], sc.dval[i])


def _layer(C, l, first):
    nc = C.nc; sc = C.sc; op = sc.op; dma = sc.dma
    Wl = C.W[l]
    psF = C.psF; psB = C.psB
    identb = C.identb; modbc = C.modbc; cactb = C.cactb; onesrow = C.onesrow; rowb = C.rowb
    nextw = C.nextw; Lay = C.Lay; carve = C.carve; barrier = C.barrier
    ARENA = C.ARENA
    xs_d = C.xs_d; rT_d = C.rT_d; kT_d = C.kT_d; y_d = C.y_d; vf_d = C.vf_d; vr_d = C.vr_d
    x_in = C.x_in
    SH1, A1, G1 = [modbc[:, i * D:(i + 1) * D] for i in range(3)]
    SH2, A2, G2 = SH1, A1, G1

    def bcast_row(vec_ap, n):
        return vec_ap.partition_broadcast(128)

    top = ARENA
    def topget(shape, dt):
        nonlocal top
        nb = 4 if dt == F32 else 2
        for d_ in shape[1:]:
            nb *= d_
        nb = (nb + 31) // 32 * 32
        top -= nb
        return carve(top, shape, dt)
    w2b = topget([128, RW], BF16); a2b = topget([128, RW], BF16)
    g2b = topget([128, RW], BF16); g2b2 = topget([128, RW], BF16)
    v1b = topget([128, 4, 32], BF16); v2b = topget([128, RW], BF16)
    lnw_bc = topget([128, RW], F32); lnb_bc = topget([128, RW], F32); v0_bc = topget([128, RW], F32)
    pcol = C.pcol
    twd = topget([128, S], BF16); adT = topget([128, S], BF16); sgd = topget([128, S], BF16); sgd2 = topget([128, S], BF16)
    att = topget([128, NT, AW], BF16)
    att.k = [Tk() for _ in range(NT)]
    TOP_P1 = twd_top = top + 16384
    TOP_P2 = top

    dma("pool", w2b[0:64, :], Wl["w2"], w=w2b.k)
    dma("pool", a2b[0:64, :], Wl["a2"], w=a2b.k)
    dma("pool", g2b[:, :], Wl["g2"][0:128, :], w=g2b.k)
    dma("pool", g2b2[0:32, :], Wl["g2"][128:160, :], w=g2b2.k)
    dma("sp", lnw_bc[:], Wl["ln_w"].partition_broadcast(128), w=lnw_bc.k)
    dma("sp", lnb_bc[:], Wl["ln_b"].partition_broadcast(128), w=lnb_bc.k)
    if l > 0:
        dma("pool", v1b[:], Wl["v1"].rearrange("(c p) n -> p c n", p=128), w=v1b.k)
        dma("pool", v2b[0:32, :], Wl["v2"], w=v2b.k)
        dma("sp", v0_bc[:], Wl["v0"].partition_broadcast(128), w=v0_bc.k)
    for i, nm in enumerate(["w0", "a0", "k_k", "k_a", "k_a", "r_k"]):
        dma("sp", pcol[:, i, :], Wl[nm].rearrange("(c p) -> p c", p=128), w=pcol.k, allow_slow_non_contiguous=True)
    op("dve", lambda e: e.tensor_scalar(out=pcol[:, 4, :], in0=pcol[:, 4, :], scalar1=-1.0, scalar2=1.0, op0=ALU.mult, op1=ALU.add),
       r=pcol.k, w=pcol.k)

    C.__dict__.update({k_: v_ for k_, v_ in locals().items() if k_ not in ("C",)})
    _adaln(C, l, 0)
    if os.environ.get("KSTOP", "") == "A":
        return

    L1 = Lay()
    QT = L1.get([128, 4, S], BF16); KT = L1.get([128, 4, S], BF16)
    Vaug = L1.get([128, NT, 8, 65], BF16)
    P2base = L1.off
    hT = L1.get([128, 8, S + 2], BF16)
    P1base = L1.off

    def src1(tt):
        if first:
            return x_in[tt * 128:(tt + 1) * 128, :], None
        return xs_d[tt], xs_d.k[tt]

    def norm_phase(L_, src, Abc, SHbc, hT_, router=None, hook=None):
        modbc = C.modbc
        xt = [L_.get([128, D], F32) for _ in range(2)]
        hf = L_.get([128, D], F32)
        hb = [L_.get([128, D], BF16) for _ in range(2)]
        junk = L_.get([128, D], BF16)
        ss = L_.get([128, NT, 4], F32, NT)
        op("dve", lambda e: e.memset(hT_[:, :, 0:1], 0.0), w=hT_.k)
        for tt in range(NT):
            x_ = xt[tt % 2]
            ap, tk = src(tt)
            dma("sp", x_[:], ap, r=([tk] if tk is not None else []), w=x_.k)
            op("act", lambda e: e.activation(out=junk[:], in_=x_[:], func=AF.Square, accum_out=ss[:, tt, 0:1]),
               r=x_.k, w=junk.k + [ss.k[tt]])
            op("act", lambda e: e.activation(out=ss[:, tt, 1:2], in_=ss[:, tt, 0:1], func=AF.Sqrt, bias=RMS_EPS, scale=1.0 / D),
               r=[ss.k[tt]], w=[ss.k[tt]])
            op("dve", lambda e: e.reciprocal(out=ss[:, tt, 2:3], in_=ss[:, tt, 1:2]), r=[ss.k[tt]], w=[ss.k[tt]])
            op("dve", lambda e: e.scalar_tensor_tensor(out=hf[:], in0=x_[:], scalar=ss[:, tt, 2:3], in1=Abc, op0=ALU.mult, op1=ALU.mult),
               r=x_.k + [ss.k[tt]] + modbc.k, w=hf.k)
            h_ = hb[tt % 2]
            if router is None:
                op("pool", lambda e: e.tensor_tensor(out=h_[:], in0=hf[:], in1=SHbc, op=ALU.add), r=hf.k + modbc.k, w=h_.k)
            else:
                op("pool", lambda e: e.tensor_tensor(out=hf[:], in0=hf[:], in1=SHbc, op=ALU.add), r=hf.k + modbc.k, w=hf.k)
                op("act", lambda e: e.copy(out=h_[:], in_=hf[:]), r=hf.k, w=h_.k)
                router(tt, hf)
            pb = psB[tt % 2]
            for kc in range(8):
                op("pe", lambda e: e.transpose(out=pb[:, kc, :], in_=h_[:, kc * 128:(kc + 1) * 128], identity=identb[:]),
                   r=h_.k + identb.k, w=pb.k)
            op("act", lambda e: e.copy(out=hT_[:, :, 1 + tt * 128:1 + (tt + 1) * 128], in_=pb[:]), r=pb.k, w=[hT_.k[(tt // 4) % len(hT_.k)]])
            if hook is not None:
                hook(tt)

    norm_phase(L1, src1, A1, SH1, hT)
    barrier()
    L1.off = P1base
    mubc = L1.get([128, 1824], F32)
    dma("sp", mubc[:], Wl["mu"].partition_broadcast(128), w=mubc.k)
    stg = [L1.get([128, 512], F32) for _ in range(2)]
    stgb = [L1.get([128, 512], BF16) for _ in range(2)]
    vTs = [L1.get([128, 512], BF16) for _ in range(2)]
    zT = L1.get([128, S], BF16)
    vft = [L1.get([128, 512], F32) for _ in range(2)]
    gt = L1.get([128, 512], F32)
    assert L1.off <= TOP_P1, (L1.off, TOP_P1)
    op("pool", lambda e: e.memset(Vaug[:, :, :, 64:65], 1.0), w=Vaug.k)
    C.stgi = 0

    def load_w(c0, cw):
        wb_ = nextw()
        dma("pool", wb_[:, :, 0:cw], Wl["w_in"][:, c0:c0 + cw].rearrange("(kc p) n -> p kc n", p=128), w=wb_.k)
        return wb_

    def scaled(wb_, c0, cw):
        W1 = nextw(); W2 = nextw()
        mu_b = mubc[:, c0 - 1536:c0 - 1536 + cw].unsqueeze(1).to_broadcast([128, 8, cw])
        op("dve", lambda e: e.tensor_tensor(out=W2[:, :, 0:cw], in0=wb_[:, :, 0:cw], in1=mu_b, op=ALU.mult), r=wb_.k + mubc.k, w=W2.k)
        op("pool", lambda e: e.tensor_tensor(out=W1[:, :, 0:cw], in0=wb_[:, :, 0:cw], in1=W2[:, :, 0:cw], op=ALU.subtract),
           r=wb_.k + W2.k, w=W1.k)
        return [(W1, 0), (W2, 1)]

    def fm_proj(wlist, sub0, subw, tg, evac):
        pt = psF[C.pf % 4]; C.pf += 1
        n = len(wlist) * 8
        i = 0
        for (w_, shift) in wlist:
            for kc in range(8):
                o = 1 + tg * 512 - shift
                op("pe", lambda e: e.matmul(pt[0:subw, :], lhsT=w_[:, kc, sub0:sub0 + subw], rhs=hT[:, kc, o:o + 512],
                                            start=(i == 0), stop=(i == n - 1)), r=w_.k + hT.k, w=pt.k)
                i += 1
        evac(pt)

    def tm_proj(wlist, tt, ncols):
        pt = psF[C.pf % 4]; C.pf += 1
        n = len(wlist) * 8
        i = 0
        for (w_, shift) in wlist:
            for kc in range(8):
                o = 1 + tt * 128 - shift
                op("pe", lambda e: e.matmul(pt[:, 0:ncols], lhsT=hT[:, kc, o:o + 128], rhs=w_[:, kc, 0:ncols],
                                            start=(i == 0), stop=(i == n - 1)), r=w_.k + hT.k, w=pt.k)
                i += 1
        return pt

    for (dst, c0, scl) in ((QT, 0, 0.125), (KT, 512, 1.0)):
        wb = load_w(c0, 512)
        for sub in range(4):
            for tg in range(4):
                def ev(pt, dst=dst, sub=sub, tg=tg, scl=scl):
                    op("act", lambda e: e.mul(out=dst[:, sub, tg * 512:(tg + 1) * 512], in_=pt[:], mul=scl), r=pt.k, w=dst.k)
                fm_proj([(wb, 0)], sub * 128, 128, tg, ev)
    wb = load_w(1024, 512)
    for tt in range(NT):
        pt = tm_proj([(wb, 0)], tt, 512)
        op("dve", lambda e: e.tensor_copy(out=Vaug[:, tt, :, 0:64], in_=pt[:].rearrange("p (h d) -> p h d", h=8)), r=pt.k, w=Vaug.k)
    for (dst_d, c0) in ((rT_d, 1536), (kT_d, 2048)):
        wb = load_w(c0, 512)
        wl = scaled(wb, c0, 512)
        for sub in range(4):
            for tg in range(4):
                def ev(pt, dst_d=dst_d, sub=sub, tg=tg):
                    s_ = stg[C.stgi % 2]; C.stgi += 1
                    op("act", lambda e: e.copy(out=s_[:], in_=pt[:]), r=pt.k, w=s_.k)
                    dma("sp", dst_d[sub, :, tg * 512:(tg + 1) * 512], s_[:], r=s_.k, w=[dst_d.k[sub]])
                fm_proj(wl, sub * 128, 128, tg, ev)
    wb = load_w(2560, 512)
    wl = scaled(wb, 2560, 512)
    if l > 0:
        for tg in range(4):
            zp = psF[4]
            for sub in range(4):
                def ev(pt, sub=sub, tg=tg):
                    v_ = vTs[sub % 2]
                    op("act", lambda e: e.copy(out=v_[:], in_=pt[:]), r=pt.k, w=v_.k)
                    op("pe", lambda e: e.matmul(zp[0:32, :], lhsT=v1b[:, sub, :], rhs=v_[:], start=(sub == 0), stop=(sub == 3)),
                       r=v1b.k + v_.k, w=zp.k)
                fm_proj(wl, sub * 128, 128, tg, ev)
            op("act", lambda e: e.copy(out=zT[0:32, tg * 512:(tg + 1) * 512], in_=zp[0:32, :]), r=zp.k, w=zT.k)
    for tt in range(NT):
        pt = tm_proj(wl, tt, 512)
        s_ = stg[tt % 2]; sb_ = stgb[tt % 2]
        op("act", lambda e: e.copy(out=s_[:], in_=pt[:]), r=pt.k, w=s_.k)
        if l == 0:
            dma("sp", vf_d[tt], s_[:], r=s_.k, w=[vf_d.k[tt]])
            op("dve", lambda e: e.tensor_copy(out=sb_[:], in_=s_[:]), r=s_.k, w=sb_.k)
        else:
            vf = vft[tt % 2]
            dma("sp", vf[:], vf_d[tt], r=[vf_d.k[tt]], w=vf.k)
            gp = psF[5]
            op("pe", lambda e: e.matmul(gp[:], lhsT=zT[0:32, tt * 128:(tt + 1) * 128], rhs=v2b[0:32, :], start=True, stop=True),
               r=zT.k + v2b.k, w=gp.k)
            op("dve", lambda e: e.tensor_tensor(out=gt[:], in0=gp[:], in1=v0_bc[:], op=ALU.add), r=gp.k + v0_bc.k, w=gt.k)
            op("act", lambda e: e.activation(out=gt[:], in_=gt[:], func=AF.Sigmoid), r=gt.k, w=gt.k)
            op("dve", lambda e: e.tensor_tensor(out=vf[:], in0=vf[:], in1=s_[:], op=ALU.subtract), r=vf.k + s_.k, w=vf.k)
            op("dve", lambda e: e.tensor_tensor(out=vf[:], in0=vf[:], in1=gt[:], op=ALU.mult), r=vf.k + gt.k, w=vf.k)
            op("dve", lambda e: e.tensor_tensor(out=sb_[:], in0=vf[:], in1=s_[:], op=ALU.add), r=vf.k + s_.k, w=sb_.k)
        dma("sp", vr_d[tt], sb_[:], r=sb_.k, w=[vr_d.k[tt]])
    wb = load_w(3072, 288)
    wl = scaled(wb, 3072, 288)
    for (sub0, subw, dstb, fn) in ((0, 64, twd, AF.Tanh), (64, 64, adT, AF.Copy), (128, 128, sgd, AF.Sigmoid), (256, 32, sgd2, AF.Sigmoid)):
        for tg in range(4):
            def ev(pt, subw=subw, dstb=dstb, fn=fn, tg=tg):
                op("act", lambda e: e.activation(out=dstb[0:subw, tg * 512:(tg + 1) * 512], in_=pt[0:subw, :], func=fn), r=pt.k, w=dstb.k)
            fm_proj(wl, sub0, subw, tg, ev)
    barrier()
    C.__dict__.update({k_: v_ for k_, v_ in locals().items() if k_ not in ("C",)})
    stop = os.environ.get("KSTOP", "")
    if stop == "P1":
        return
    _attention(C, l)
    if stop == "P2":
        return
    _rwkv(C, l)
    if stop == "P3":
        return
    _outproj(C, l)
    if stop == "P4":
        return
    _ffn(C, l)


def _adaln(C, l, half):
    sc = C.sc; op = sc.op; dma = sc.dma
    Wl = C.W[l]
    psF = C.psF; modbc = C.modbc; cactb = C.cactb; onesrow = C.onesrow; rowb = C.rowb
    LA = C.Lay()
    ng = LA.get([128, D], F32)
    dma("sp", ng[:], Wl["norm1_g" if half == 0 else "norm2_g"].partition_broadcast(128), w=ng.k)
    for ch in range(6):
        gch = half * 6 + ch
        wb = C.nextw()
        dma("pool", wb[:], Wl["w_ada"][:, gch * 512:(gch + 1) * 512].rearrange("(kc p) n -> p kc n", p=128), w=wb.k)
        dma("pool", rowb[:], Wl["b_ada"][gch * 512:(gch + 1) * 512].rearrange("(o n) -> o n", o=1), w=rowb.k)
        pt = psF[ch % 2]
        for kc in range(8):
            op("pe", lambda e: e.matmul(pt[:], lhsT=cactb[:, kc, :], rhs=wb[:, kc, :], start=(kc == 0), stop=False),
               r=cactb.k + wb.k, w=pt.k)
        op("pe", lambda e: e.matmul(pt[:], lhsT=onesrow[0:1, :], rhs=rowb[0:1, :], start=False, stop=True),
           r=onesrow.k + rowb.k, w=pt.k)
        which, hh = ch // 2, ch % 2
        dst = modbc[:, ch * 512:(ch + 1) * 512]
        if which == 1:
            op("dve", lambda e: e.scalar_tensor_tensor(out=dst, in0=pt[:], scalar=1.0, in1=ng[:, hh * 512:(hh + 1) * 512],
                                                       op0=ALU.add, op1=ALU.mult), r=pt.k + ng.k, w=modbc.k)
        else:
            op("act", lambda e: e.copy(out=dst, in_=pt[:]), r=pt.k, w=modbc.k)
    C.barrier()


def _attention(C, l):
    sc = C.sc; op = sc.op; dma = sc.dma
    psF = C.psF
    QT = C.QT; KT = C.KT; Vaug = C.Vaug; att = C.att
    L2 = C.Lay()
    L2.off = C.P2base
    amask = L2.get([128, 8, S], BF16)
    Pt = [L2.get([128, 512], BF16) for _ in range(4)]
    rz = L2.get([128, 8], F32)
    assert L2.off <= C.TOP_P2
    for h in range(8):
        dma("sp", amask[:, h, :], C.amask_d[:, h * S:(h + 1) * S], r=C.amask_d.k, w=amask.k)
    steps = [(h, qg, j) for h in range(8) for qg in range(4) for j in range(4 * qg + 4)]

    def unit_a(i):
        h, qg, j = steps[i]
        hp, po = h // 2, (h % 2) * 64
        qlo = max(4 * qg, j)
        nq = (4 * qg + 4 - qlo) * 128
        pS = psF[4 + (i % 4)]
        P_ = Pt[i % 4]
        op("pe", lambda e: e.matmul(pS[:, 0:nq], lhsT=KT[po:po + 64, hp, j * 128:(j + 1) * 128],
                                    rhs=QT[po:po + 64, hp, qlo * 128:qlo * 128 + nq], start=True, stop=True),
           r=KT.k + QT.k, w=pS.k)
        op("act", lambda e: e.activation(out=P_[:, 0:nq], in_=pS[:, 0:nq], func=AF.Exp), r=pS.k, w=P_.k)
        u0 = (qlo - j) * 128
        op("dve", lambda e: e.tensor_tensor(out=P_[:, 0:nq], in0=P_[:, 0:nq], in1=amask[:, h, u0:u0 + nq], op=ALU.mult),
           r=P_.k + amask.k, w=P_.k)

    def unit_b(i):
        h, qg, j = steps[i]
        qlo = max(4 * qg, j)
        P_ = Pt[i % 4]
        for ii in range(qlo, 4 * qg + 4):
            pO = psF[ii - 4 * qg]
            op("pe", lambda e: e.matmul(pO[:, 0:65], lhsT=P_[:, (ii - qlo) * 128:(ii - qlo + 1) * 128], rhs=Vaug[:, j, h, :],
                                        start=(j == 0), stop=(j == ii)), r=P_.k + Vaug.k, w=pO.k)
        if j == 4 * qg + 3:
            for ib in range(4):
                ii = 4 * qg + ib
                pO = psF[ib]
                op("dve", lambda e: e.reciprocal(out=rz[:, h:h + 1], in_=pO[:, 64:65]), r=pO.k, w=rz.k)
                op("dve", lambda e: e.tensor_scalar(out=att[:, ii, h * 64:(h + 1) * 64], in0=pO[:, 0:64], scalar1=rz[:, h:h + 1], scalar2=None,
                                                    op0=ALU.mult), r=pO.k + rz.k, w=[att.k[ii]])

    LOOK = 3
    for i in range(min(LOOK, len(steps))):
        unit_a(i)
    for i in range(len(steps)):
        unit_b(i)
        if i + LOOK < len(steps):
            unit_a(i + LOOK)
    barrier = C.barrier
    barrier()


def _rwkv(C, l):
    sc = C.sc
    defer = {"lst": None}

    def op(e, fn, r=(), w=()):
        if defer["lst"] is not None:
            defer["lst"].append(lambda: sc.op(e, fn, r, w))
            return None
        return sc.op(e, fn, r, w)

    def dma(q, out, in_, r=(), w=(), **kw):
        if defer["lst"] is not None:
            defer["lst"].append(lambda: sc.dma(q, out, in_, r, w, **kw))
            return None
        return sc.dma(q, out, in_, r, w, **kw)
    psF = C.psF; psB = C.psB
    identb = C.identb; mu2 = C.mu2; mls = C.mls; blockones = C.blockones
    pcol = C.pcol; w2b = C.w2b; a2b = C.a2b; twd = C.twd; adT = C.adT
    rT_d = C.rT_d; kT_d = C.kT_d; y_d = C.y_d; vr_d = C.vr_d
    L3 = C.Lay()
    ARt = L3.get([128, NT, 256], BF16, 4)
    scanmask = L3.get([128, S], BF16)
    dma("pool", scanmask[:], C.cst["scanmask"], w=scanmask.k)
    bt = L3.get([128, S], BF16, 4); kt = L3.get([128, S], BF16, 4); bh = L3.get([128, S], BF16, 4); kh = L3.get([128, S], BF16, 4)
    prodb = L3.get([128, S], BF16)
    WC = L3.get([128, NT], F32, 4)
    rf = L3.get([128, 512], F32); kf = L3.get([128, 512], F32); lw = L3.get([128, 512], F32); af = L3.get([128, 512], F32)
    Lc = L3.get([128, 512], F32); t1 = L3.get([128, 512], F32); t2 = L3.get([128, 512], F32); t3 = L3.get([128, 512], F32)
    t4 = L3.get([128, 512], F32); sqb = L3.get([128, 512], BF16)
    N1s = [L3.get([128, 4, 256], BF16) for _ in range(2)]
    N2s = [L3.get([128, 4, 256], BF16) for _ in range(2)]
    N3s = L3.get([128, 4, 128], BF16)
    Ab = [L3.get([128, 4, 128], BF16) for _ in range(2)]
    ATb = [L3.get([128, 4, 128], BF16) for _ in range(2)]
    Pb = [L3.get([128, 4, 128], BF16) for _ in range(2)]
    Tinv = [L3.get([128, 4, 128], BF16) for _ in range(2)]
    ZB = [L3.get([128, 2, 2, 128], BF16) for _ in range(2)]
    ZK = [L3.get([128, 2, 2, 128], BF16) for _ in range(2)]
    Vt = [L3.get([128, RW], BF16) for _ in range(4)]
    Xs = L3.get([128, 128], BF16); Us = L3.get([128, 128], BF16)
    Sf = L3.get([128, 64], F32); Sbf = L3.get([128, 2, 64], BF16)
    ystg = [L3.get([128, 128], F32) for _ in range(2)]
    assert L3.off <= C.TOP_P2, (L3.off, C.TOP_P2)
    if os.environ.get("KDEBUG"):
        print("L3.off", L3.off, "TOP_P2", C.TOP_P2)
    prod_d = C.prod_d
    NEG = -math.exp(-0.5)
    for z_ in ZB + ZK:
        op("pool", lambda e: e.memset(z_[:], 0.0), w=z_.k)

    def prep_unit(hp, tg):
        pc = lambda i: pcol[:, i, hp:hp + 1]
        hcols = slice(hp * 128, (hp + 1) * 128)
        ts_ = slice(tg * 512, (tg + 1) * 512)
        dma("sp", rf[:], rT_d[hp, :, ts_], r=[rT_d.k[hp]], w=rf.k)
        dma("sp", kf[:], kT_d[hp, :, ts_], r=[kT_d.k[hp]], w=kf.k)
        pz = psF[5]
        op("pe", lambda e: e.matmul(pz[:], lhsT=w2b[0:64, hcols], rhs=twd[0:64, ts_], start=True, stop=True), r=w2b.k + twd.k, w=pz.k)
        op("act", lambda e: e.activation(out=lw[:], in_=pz[:], func=AF.Sigmoid, bias=pc(0), scale=1.0), r=pz.k + pcol.k, w=lw.k)
        op("pool", lambda e: e.tensor_scalar(out=lw[:], in0=lw[:], scalar1=NEG, scalar2=None, op0=ALU.mult), r=lw.k, w=lw.k)
        pz2 = psF[5]
        op("pe", lambda e: e.matmul(pz2[:], lhsT=a2b[0:64, hcols], rhs=adT[0:64, ts_], start=True, stop=True), r=a2b.k + adT.k, w=pz2.k)
        op("act", lambda e: e.activation(out=af[:], in_=pz2[:], func=AF.Sigmoid, bias=pc(1), scale=1.0), r=pz2.k + pcol.k, w=af.k)
        op("dve", lambda e: e.tensor_tensor_scan(out=Lc[:], data0=scanmask[:, ts_], data1=lw[:], initial=0.0, op0=ALU.mult, op1=ALU.add),
           r=scanmask.k + lw.k, w=Lc.k)
        op("dve", lambda e: e.tensor_scalar(out=t1[:], in0=kf[:], scalar1=pc(2), scalar2=None, op0=ALU.mult), r=kf.k + pcol.k, w=t1.k)
        op("act", lambda e: e.activation(out=sqb[:], in_=t1[:], func=AF.Square), r=t1.k, w=sqb.k)
        pn = psF[5]
        op("pe", lambda e: e.matmul(pn[:], lhsT=blockones[:], rhs=sqb[:], start=True, stop=True), r=blockones.k + sqb.k, w=pn.k)
        op("act", lambda e: e.activation(out=t2[:], in_=pn[:], func=AF.Sqrt), r=pn.k, w=t2.k)
        op("dve", lambda e: e.tensor_scalar(out=t2[:], in0=t2[:], scalar1=1e-12, scalar2=None, op0=ALU.max), r=t2.k, w=t2.k)
        op("dve", lambda e: e.reciprocal(out=t2[:], in_=t2[:]), r=t2.k, w=t2.k)
        op("dve", lambda e: e.tensor_tensor(out=t1[:], in0=t1[:], in1=t2[:], op=ALU.mult), r=t1.k + t2.k, w=t1.k)
        op("dve", lambda e: e.tensor_scalar(out=t3[:], in0=af[:], scalar1=pc(3), scalar2=pc(4), op0=ALU.mult, op1=ALU.add),
           r=af.k + pcol.k, w=t3.k)
        op("dve", lambda e: e.tensor_tensor(out=kf[:], in0=kf[:], in1=t3[:], op=ALU.mult), r=kf.k + t3.k, w=kf.k)
        op("dve", lambda e: e.tensor_tensor(out=t3[:], in0=t1[:], in1=af[:], op=ALU.mult), r=t1.k + af.k, w=t3.k)
        op("dve", lambda e: e.scalar_tensor_tensor(out=prodb[:, ts_], in0=rf[:], scalar=pc(5), in1=kf[:], op0=ALU.mult, op1=ALU.mult),
           r=rf.k + kf.k + pcol.k, w=prodb.k)
        op("act", lambda e: e.activation(out=t2[:], in_=Lc[:], func=AF.Exp), r=Lc.k, w=t2.k)
        op("dve", lambda e: e.tensor_tensor(out=ARt[:, 4 * tg:4 * tg + 4, 128:256], in0=rf[:].rearrange("p (a b) -> p a b", a=4),
                                            in1=t2[:].rearrange("p (a b) -> p a b", a=4), op=ALU.mult), r=rf.k + t2.k, w=[ARt.k[tg]])
        op("pool", lambda e: e.tensor_tensor(out=t4[:], in0=Lc[:], in1=lw[:], op=ALU.subtract), r=Lc.k + lw.k, w=t4.k)
        op("act", lambda e: e.activation(out=t4[:], in_=t4[:], func=AF.Exp), r=t4.k, w=t4.k)
        op("dve", lambda e: e.scalar_tensor_tensor(out=ARt[:, 4 * tg:4 * tg + 4, 0:128], in0=t1[:].rearrange("p (a b) -> p a b", a=4),
                                                   scalar=-1.0, in1=t4[:].rearrange("p (a b) -> p a b", a=4), op0=ALU.mult, op1=ALU.mult),
           r=t1.k + t4.k, w=[ARt.k[tg]])
        op("act", lambda e: e.activation(out=t2[:], in_=Lc[:], func=AF.Exp, scale=-1.0), r=Lc.k, w=t2.k)
        op("dve", lambda e: e.tensor_tensor(out=bt[:, ts_], in0=t3[:], in1=t2[:], op=ALU.mult), r=t3.k + t2.k, w=[bt.k[tg]])
        op("dve", lambda e: e.tensor_tensor(out=kt[:, ts_], in0=kf[:], in1=t2[:], op=ALU.mult), r=kf.k + t2.k, w=[kt.k[tg]])
        for q in range(4):
            cs = slice(q * 128, (q + 1) * 128)
            op("act", lambda e, cs=cs, q=q: e.activation(out=t4[:, cs], in_=Lc[:, cs], func=AF.Exp, scale=-1.0, bias=Lc[:, q * 128 + 127:q * 128 + 128]),
               r=Lc.k, w=t4.k)
        op("dve", lambda e: e.tensor_tensor(out=bh[:, ts_], in0=t3[:], in1=t4[:], op=ALU.mult), r=t3.k + t4.k, w=[bh.k[tg]])
        op("dve", lambda e: e.tensor_tensor(out=kh[:, ts_], in0=kf[:], in1=t4[:], op=ALU.mult), r=kf.k + t4.k, w=[kh.k[tg]])
        op("act", lambda e: e.activation(out=WC[:, 4 * tg:4 * tg + 4], in_=Lc[:, 127::128], func=AF.Exp), r=Lc.k, w=[WC.k[tg]])
        if tg == 3:
            dma("sp", prod_d[hp], prodb[:], r=prodb.k, w=[prod_d.k[hp]])

    pending = []

    def queue_prep(hp_, tg_):
        defer["lst"] = []
        prep_unit(hp_, tg_)
        lst = defer["lst"]
        defer["lst"] = None
        pending.extend((hp_, tg_, t) for t in lst)

    def pop_prep(n):
        for _ in range(n):
            if pending:
                pending.pop(0)[2]()

    def flush_prep(hp_, tg_):
        while pending and (pending[0][0], pending[0][1]) <= (hp_, tg_):
            pending.pop(0)[2]()

    for tg in range(4):
        prep_unit(0, tg)
    for hp in range(4):
        op("dve", lambda e: e.memset(Sf[:], 0.0), w=Sf.k)
        op("dve", lambda e: e.memset(Sbf[:], 0.0), w=Sbf.k)
        def pre_units(b2):
            par = b2 % 2
            n1 = N1s[par]; n2 = N2s[par]; ti_ = Tinv[par]; zb = ZB[par]; zk = ZK[par]
            st = {}
            units = []

            def u_nmat():
                mu2b = mu2[:].unsqueeze(1).to_broadcast([128, 2, 256])
                tg_ = b2 // 2
                for rnd in range(3):
                    for idx in range(4):
                        hd, ti = idx // 2, idx % 2
                        tt = 2 * b2 + ti
                        po = hd * 64
                        tcs = slice(tt * 128, (tt + 1) * 128)
                        pb_ = psF[3 + hd]
                        if rnd == 0:
                            op("pe", lambda e: e.matmul(pb_[:, ti * 256:ti * 256 + 256], lhsT=bt[po:po + 64, tcs], rhs=ARt[po:po + 64, tt, :], start=True, stop=True),
                               r=[bt.k[tg_], ARt.k[tg_]], w=pb_.k)
                        elif rnd == 1:
                            op("pe", lambda e: e.matmul(pb_[:, ti * 256:ti * 256 + 256], lhsT=kt[po:po + 64, tcs], rhs=ARt[po:po + 64, tt, :], start=True, stop=True),
                               r=[kt.k[tg_], ARt.k[tg_]], w=pb_.k)
                        else:
                            op("pe", lambda e: e.matmul(pb_[:, ti * 128:(ti + 1) * 128], lhsT=ARt[po:po + 64, tt, 0:128], rhs=bt[po:po + 64, tcs],
                                                        start=True, stop=True), r=[bt.k[tg_], ARt.k[tg_]], w=pb_.k)
                    for b_ in range(2):
                        pb_ = psF[3 + b_]
                        if rnd == 0:
                            op("dve", lambda e: e.tensor_tensor(out=n1[:, 2 * b_:2 * b_ + 2, :], in0=pb_[:].rearrange("p (a b) -> p a b", a=2),
                                                                in1=mu2b, op=ALU.mult), r=pb_.k + mu2.k, w=n1.k)
                        elif rnd == 1:
                            op("dve", lambda e: e.tensor_tensor(out=n2[:, 2 * b_:2 * b_ + 2, :], in0=pb_[:].rearrange("p (a b) -> p a b", a=2),
                                                                in1=mu2b, op=ALU.mult), r=pb_.k + mu2.k, w=n2.k)
                        else:
                            op("dve", lambda e: e.tensor_tensor(out=N3s[:, 2 * b_:2 * b_ + 2, :], in0=pb_[:, 0:256].rearrange("p (a b) -> p a b", a=2),
                                                                in1=mls[:].unsqueeze(1).to_broadcast([128, 2, 128]), op=ALU.mult), r=pb_.k + mls.k, w=N3s.k)
                op("dve", lambda e: e.tensor_tensor(out=Pb[0][:], in0=n1[:, :, 0:128], in1=identb[:].unsqueeze(1).to_broadcast([128, 4, 128]), op=ALU.add),
                   r=n1.k + identb.k, w=Pb[0].k)
                st["A"] = (lambda idx: n1[:, idx, 0:128]); st["AT"] = (lambda idx: N3s[:, idx, :])
                st["Ak"] = n1.k; st["ATk"] = N3s.k; st["P"] = Pb[0]
            units.append(u_nmat)

            def mk_level(lev):
                def u():
                    pA, pAT, pP = psF[0], psF[1], psF[2]
                    An = Ab[lev % 2]; ATn = ATb[lev % 2]
                    Pn = Pb[lev % 2] if lev < 6 else ti_
                    A_cur, AT_cur, A_k, AT_k, P_cur = st["A"], st["AT"], st["Ak"], st["ATk"], st["P"]
                    for idx in range(4):
                        cs = slice(idx * 128, (idx + 1) * 128)
                        if lev < 6:
                            op("pe", lambda e: e.matmul(pA[:, cs], lhsT=AT_cur(idx), rhs=A_cur(idx), start=True, stop=True), r=A_k + AT_k, w=pA.k)
                        op("pe", lambda e: e.matmul(pAT[:, cs], lhsT=A_cur(idx), rhs=AT_cur(idx), start=True, stop=True), r=A_k + AT_k, w=pAT.k)
                    if lev < 6:
                        op("act", lambda e: e.copy(out=An[:], in_=pA[:].rearrange("p (a b) -> p a b", a=4)), r=pA.k, w=An.k)
                    op("dve", lambda e: e.tensor_copy(out=ATn[:], in_=pAT[:].rearrange("p (a b) -> p a b", a=4)), r=pAT.k, w=ATn.k)
                    for idx in range(4):
                        cs = slice(idx * 128, (idx + 1) * 128)
                        op("pe", lambda e: e.matmul(pP[:, cs], lhsT=ATn[:, idx, :], rhs=P_cur[:, idx, :], start=True, stop=False), r=ATn.k + P_cur.k, w=pP.k)
                        op("pe", lambda e: e.matmul(pP[:, cs], lhsT=identb[:], rhs=P_cur[:, idx, :], start=False, stop=True), r=identb.k + P_cur.k, w=pP.k)
                    op("act", lambda e: e.copy(out=Pn[:], in_=pP[:].rearrange("p (a b) -> p a b", a=4)), r=pP.k, w=Pn.k)
                    st["A"] = (lambda idx: An[:, idx, :]); st["AT"] = (lambda idx: ATn[:, idx, :])
                    st["Ak"] = An.k; st["ATk"] = ATn.k; st["P"] = Pn
                return u
            for lev in range(1, 7):
                units.append(mk_level(lev))

            def u_tr():
                pb = psB[0]
                for ti in range(2):
                    tt = 2 * b2 + ti
                    tcs = slice(tt * 128, (tt + 1) * 128)
                    op("pe", lambda e: e.transpose(out=pb[:, 2 * ti, :], in_=bh[:, tcs], identity=identb[:]), r=[bh.k[b2 // 2]] + identb.k, w=pb.k)
                    op("pe", lambda e: e.transpose(out=pb[:, 2 * ti + 1, :], in_=kh[:, tcs], identity=identb[:]), r=[kh.k[b2 // 2]] + identb.k, w=pb.k)
                for ti in range(2):
                    for hd in range(2):
                        hs = slice(hd * 64, hd * 64 + 64)
                        op("act", lambda e: e.copy(out=zb[:, ti, hd, hs], in_=pb[:, 2 * ti, hs]), r=pb.k, w=zb.k)
                        op("act", lambda e: e.copy(out=zk[:, ti, hd, hs], in_=pb[:, 2 * ti + 1, hs]), r=pb.k, w=zk.k)
            units.append(u_tr)
            return units

        def seq_units(b2):
            par = b2 % 2
            n1 = N1s[par]; n2 = N2s[par]; ti_ = Tinv[par]; zb = ZB[par]; zk = ZK[par]
            pq = psF[7]
            units = []
            for ti in range(2):
                tt = 2 * b2 + ti
                v_ = Vt[tt % 4]

                def hv(hd, v_=v_):
                    return v_[:, (2 * hp + hd) * 64:(2 * hp + hd) * 64 + 64]

                def u_x(ti=ti, tt=tt, v_=v_, hv=hv):
                    dma("sp", v_[:], vr_d[tt], r=[vr_d.k[tt]], w=v_.k)
                    for hd in range(2):
                        idx = hd * 2 + ti
                        op("pe", lambda e: e.matmul(pq[:, hd * 64:hd * 64 + 64], lhsT=n2[:, idx, 0:128], rhs=hv(hd), start=True, stop=False),
                           r=n2.k + v_.k, w=pq.k)
                        op("pe", lambda e: e.matmul(pq[:, hd * 64:hd * 64 + 64], lhsT=ARt[:, tt, 0:128], rhs=Sbf[:, hd, :], start=False, stop=True),
                           r=[ARt.k[b2 // 2]] + Sbf.k, w=pq.k)
                    op("act", lambda e: e.copy(out=Xs[:], in_=pq[:, 0:128]), r=pq.k, w=Xs.k)

                def u_u(ti=ti, tt=tt):
                    for hd in range(2):
                        idx = hd * 2 + ti
                        op("pe", lambda e: e.matmul(pq[:, 128 + hd * 64:128 + hd * 64 + 64], lhsT=ti_[:, idx, :], rhs=Xs[:, hd * 64:hd * 64 + 64], start=True, stop=True),
                           r=ti_.k + Xs.k, w=pq.k)
                    op("dve", lambda e: e.tensor_copy(out=Us[:], in_=pq[:, 128:256]), r=pq.k, w=Us.k)

                def u_y(ti=ti, tt=tt, v_=v_, hv=hv):
                    for hd in range(2):
                        idx = hd * 2 + ti
                        yc = slice(256 + hd * 64, 256 + hd * 64 + 64)
                        op("pe", lambda e: e.matmul(pq[:, yc], lhsT=ARt[:, tt, 128:256], rhs=Sbf[:, hd, :], start=True, stop=False),
                           r=[ARt.k[b2 // 2]] + Sbf.k, w=pq.k)
                        op("pe", lambda e: e.matmul(pq[:, yc], lhsT=n1[:, idx, 128:256], rhs=Us[:, hd * 64:hd * 64 + 64], start=False, stop=False),
                           r=n1.k + Us.k, w=pq.k)
                        op("pe", lambda e: e.matmul(pq[:, yc], lhsT=n2[:, idx, 128:256], rhs=hv(hd), start=False, stop=True), r=n2.k + v_.k, w=pq.k)
                    for hd in range(2):
                        op("pe", lambda e: e.matmul(pq[:, 384:448], lhsT=zb[:, ti, hd, :], rhs=Us[:, hd * 64:hd * 64 + 64], start=(hd == 0), stop=False),
                           r=zb.k + Us.k, w=pq.k)
                        op("pe", lambda e: e.matmul(pq[:, 384:448], lhsT=zk[:, ti, hd, :], rhs=hv(hd), start=False, stop=(hd == 1)), r=zk.k + v_.k, w=pq.k)

                def u_s(ti=ti, tt=tt):
                    op("dve", lambda e: e.scalar_tensor_tensor(out=Sf[:], in0=Sf[:], scalar=WC[:, tt:tt + 1], in1=pq[:, 384:448], op0=ALU.mult, op1=ALU.add),
                       r=Sf.k + [WC.k[b2 // 2]] + pq.k, w=Sf.k)
                    op("act", lambda e: e.copy(out=Sbf[0:64, 0, :], in_=Sf[0:64, :]), r=Sf.k, w=Sbf.k)
                    op("act", lambda e: e.copy(out=Sbf[64:128, 1, :], in_=Sf[64:128, :]), r=Sf.k, w=Sbf.k)
                    ys = ystg[tt % 2]
                    op("act", lambda e: e.copy(out=ys[:], in_=pq[:, 256:384]), r=pq.k, w=ys.k)
                    dma("sp", y_d[tt][:, hp * 128:(hp + 1) * 128], ys[:], r=ys.k, w=[y_d.k[tt]])
                units += [u_x, u_u, u_y, u_s]
            return units

        flush_prep(hp, 0)
        for u in pre_units(0):
            u()
        for b2 in range(8):
            sq = seq_units(b2)
            if b2 + 1 < 8:
                flush_prep(hp, (b2 + 1) // 2)
                pr = pre_units(b2 + 1)
            else:
                pr = []
            for k in range(8):
                if k < len(pr):
                    pr[k]()
                sq[k]()
                pop_prep(int(os.environ.get("POPN", "3")))
            if hp + 1 < 4 and b2 % 2 == 1:
                queue_prep(hp + 1, b2 // 2)
    flush_prep(9, 9)
    for _ in range(int(os.environ.get("EXTRA", "0"))):
        op("pe", lambda e: e.matmul(psF[0][:, 0:128], lhsT=identb[:], rhs=identb[:], start=True, stop=True), r=identb.k, w=psF[0].k)
    C.barrier()


def _outproj(C, l):
    sc = C.sc; op = sc.op; dma = sc.dma
    psF = C.psF; psB = C.psB; identb = C.identb
    Wl = C.W[l]
    att = C.att; sgd = C.sgd; sgd2 = C.sgd2; g2b = C.g2b; g2b2 = C.g2b2; lnw_bc = C.lnw_bc; lnb_bc = C.lnb_bc
    y_d = C.y_d; vr_d = C.vr_d; xs_d = C.xs_d; prod_d = C.prod_d; headsel = C.headsel
    G1 = C.G1
    L4 = C.Lay()
    yt = [L4.get([128, RW], F32) for _ in range(2)]
    vt = [L4.get([128, RW], BF16) for _ in range(2)]
    pr = [L4.get([128, 4, 128], BF16) for _ in range(2)]
    sq = L4.get([128, RW], F32); yc = L4.get([128, RW], F32); bo = L4.get([128, RW], F32)
    st = L4.get([128, 8, 8], F32)
    rwo = [L4.get([128, RW], BF16) for _ in range(2)]
    catT = [L4.get([128, 8, 128], BF16) for _ in range(2)]
    xt = [L4.get([128, D], F32) for _ in range(2)]
    xn = [L4.get([128, D], F32) for _ in range(2)]
    assert L4.off <= C.TOP_P2
    wo = [C.nextw(), C.nextw()]
    for nh in range(2):
        dma("pool", wo[nh][:], Wl["w_out"][:, nh * 512:(nh + 1) * 512].rearrange("(kc p) n -> p kc n", p=128), w=wo[nh].k)
    def unit_a(tt):
        y_ = yt[tt % 2]; v_ = vt[tt % 2]; p_ = pr[tt % 2]
        dma("sp", y_[:], y_d[tt], r=[y_d.k[tt]], w=y_.k)
        dma("sp", v_[:], vr_d[tt], r=[vr_d.k[tt]], w=v_.k)
        dma("sp", p_[:], prod_d.t[:, :, tt * 128:(tt + 1) * 128].rearrange("a p t -> p a t"), r=prod_d.k, w=p_.k)
        y3 = y_[:].rearrange("p (h d) -> p h d", h=8)
        S1, S2, MEAN, MSQ, VAR, RSTD, RK = [st[:, i, :] for i in range(7)]
        op("dve", lambda e: e.tensor_reduce(out=S1, in_=y3, axis=AX.X, op=ALU.add), r=y_.k, w=st.k)
        op("pool", lambda e: e.tensor_tensor(out=sq[:], in0=y_[:], in1=y_[:], op=ALU.mult), r=y_.k, w=sq.k)
        op("dve", lambda e: e.tensor_reduce(out=S2, in_=sq[:].rearrange("p (h d) -> p h d", h=8), axis=AX.X, op=ALU.add), r=sq.k + st.k, w=st.k)
        op("dve", lambda e: e.tensor_scalar(out=MEAN, in0=S1, scalar1=1.0 / 64, scalar2=None, op0=ALU.mult), r=st.k, w=st.k)
        op("dve", lambda e: e.tensor_tensor(out=MSQ, in0=MEAN, in1=MEAN, op=ALU.mult), r=st.k, w=st.k)
        op("dve", lambda e: e.scalar_tensor_tensor(out=VAR, in0=S2, scalar=1.0 / 64, in1=MSQ, op0=ALU.mult, op1=ALU.subtract), r=st.k, w=st.k)
        op("act", lambda e: e.activation(out=RSTD, in_=VAR, func=AF.Sqrt, bias=GN_EPS, scale=1.0), r=st.k, w=st.k)
        op("dve", lambda e: e.reciprocal(out=RSTD, in_=RSTD), r=st.k, w=st.k)
        yc3 = yc[:].rearrange("p (h d) -> p h d", h=8)
        op("dve", lambda e: e.tensor_tensor(out=yc3, in0=y3, in1=MEAN.unsqueeze(2).to_broadcast([128, 8, 64]), op=ALU.subtract), r=y_.k + st.k, w=yc.k)
        op("dve", lambda e: e.tensor_tensor(out=yc3, in0=yc3, in1=RSTD.unsqueeze(2).to_broadcast([128, 8, 64]), op=ALU.mult), r=yc.k + st.k, w=yc.k)
        op("pool", lambda e: e.tensor_tensor(out=yc[:], in0=yc[:], in1=lnw_bc[:], op=ALU.mult), r=yc.k + lnw_bc.k, w=yc.k)
        op("pool", lambda e: e.tensor_tensor(out=yc[:], in0=yc[:], in1=lnb_bc[:], op=ALU.add), r=yc.k + lnb_bc.k, w=yc.k)
        prk = psF[0]
        for hp in range(4):
            op("pe", lambda e: e.matmul(prk[:, 2 * hp:2 * hp + 2], lhsT=p_[:, hp, :], rhs=headsel[:], start=True, stop=True), r=p_.k + headsel.k, w=prk.k)
        op("act", lambda e: e.copy(out=RK, in_=prk[:, 0:8]), r=prk.k, w=st.k)
        op("dve", lambda e: e.tensor_tensor(out=bo[:].rearrange("p (h d) -> p h d", h=8), in0=v_[:].rearrange("p (h d) -> p h d", h=8),
                                            in1=RK.unsqueeze(2).to_broadcast([128, 8, 64]), op=ALU.mult), r=v_.k + st.k, w=bo.k)
        op("pool", lambda e: e.tensor_tensor(out=yc[:], in0=yc[:], in1=bo[:], op=ALU.add), r=yc.k + bo.k, w=yc.k)
        pg = psF[1]
        tcs = slice(tt * 128, (tt + 1) * 128)
        op("pe", lambda e: e.matmul(pg[:], lhsT=sgd[:, tcs], rhs=g2b[:, :], start=True, stop=False), r=sgd.k + g2b.k, w=pg.k)
        op("pe", lambda e: e.matmul(pg[:], lhsT=sgd2[0:32, tcs], rhs=g2b2[0:32, :], start=False, stop=True), r=sgd2.k + g2b2.k, w=pg.k)
        ro = rwo[tt % 2]
        op("dve", lambda e: e.tensor_tensor(out=ro[:], in0=pg[:], in1=yc[:], op=ALU.mult), r=pg.k + yc.k, w=ro.k)

    def unit_b(tt):
        ro = rwo[tt % 2]
        pb = psB[tt % 2]
        for kc in range(8):
            src = att[:, tt, kc * 128:(kc + 1) * 128] if kc < 4 else ro[:, (kc - 4) * 128:(kc - 3) * 128]
            op("pe", lambda e: e.transpose(out=pb[:, kc, :], in_=src, identity=identb[:]), r=[att.k[tt]] + ro.k + identb.k, w=pb.k)
        cT = catT[tt % 2]
        op("act", lambda e: e.copy(out=cT[:], in_=pb[:]), r=pb.k, w=cT.k)
        x_ = xt[tt % 2]; xo = xn[tt % 2]
        if l == C.layers[0] and C.first_from_input:
            dma("sp", x_[:], C.x_in[tt * 128:(tt + 1) * 128, :], w=x_.k)
        else:
            dma("sp", x_[:], xs_d[tt], r=[xs_d.k[tt]], w=x_.k)
        for nh in range(2):
            po_ = psF[2 + nh]
            for kc in range(8):
                op("pe", lambda e: e.matmul(po_[:], lhsT=cT[:, kc, :], rhs=wo[nh][:, kc, :], start=(kc == 0), stop=(kc == 7)), r=cT.k + wo[nh].k, w=po_.k)
            ns = slice(nh * 512, (nh + 1) * 512)
            op("dve", lambda e: e.tensor_tensor(out=xo[:, ns], in0=po_[:], in1=G1[:, ns], op=ALU.mult), r=po_.k + C.modbc.k, w=xo.k)
        op("pool", lambda e: e.tensor_tensor(out=xo[:], in0=xo[:], in1=x_[:], op=ALU.add), r=xo.k + x_.k, w=xo.k)
        dma("sp", xs_d[tt], xo[:], r=xo.k, w=[xs_d.k[tt]])
        if l in C.tapx:
            dma("sp", C.tapx[l][tt * 128:(tt + 1) * 128, :], xo[:], r=xo.k)
    unit_a(0)
    for tt in range(NT):
        if tt + 1 < NT:
            unit_a(tt + 1)
        unit_b(tt)
    C.barrier()


def _ffn(C, l):
    sc = C.sc; op = sc.op; dma = sc.dma
    psF = C.psF
    Wl = C.W[l]
    xs_d = C.xs_d
    moe = (l % 2 == 1)
    _adaln(C, l, 1)
    L5 = C.Lay()
    hT = L5.get([128, 8, S + 2], BF16, 4)
    gates = L5.get([128, NT, 8], F32, NT)
    base = L5.off
    if moe:
        rwbc = L5.get([128, 8, D], F32)
        rbbc = L5.get([128, 8], F32)
        lgt = L5.get([128, 16], F32)
        jk = L5.get([128, D], F32)
        for e_ in range(NE):
            dma("sp", rwbc[:, e_, :], Wl["rw"][e_].partition_broadcast(128), w=rwbc.k)
        dma("sp", rbbc[:], Wl["rb"].partition_broadcast(128), w=rbbc.k)

        def router(tt, hf):
            LG = lgt[:, 0:8]
            for e_ in range(NE):
                op("dve", lambda e: e.scalar_tensor_tensor(out=jk[:], in0=hf[:], scalar=1.0, in1=rwbc[:, e_, :], op0=ALU.mult, op1=ALU.mult,
                                                           accum_out=lgt[:, e_:e_ + 1]), r=hf.k + rwbc.k, w=jk.k + lgt.k)
            M1, NM1, M2, E2 = [lgt[:, 8 + i:9 + i] for i in range(4)]
            EQ = jk[:, 0:8]; L2_ = jk[:, 8:16]; EX = jk[:, 16:24]; SEL = jk[:, 24:32]
            op("dve", lambda e: e.tensor_tensor(out=LG, in0=LG, in1=rbbc[:], op=ALU.add), r=lgt.k + rbbc.k, w=lgt.k)
            op("dve", lambda e: e.tensor_reduce(out=M1, in_=LG, axis=AX.X, op=ALU.max), r=lgt.k, w=lgt.k)
            op("dve", lambda e: e.tensor_scalar(out=EQ, in0=LG, scalar1=M1, scalar2=None, op0=ALU.is_equal), r=lgt.k, w=jk.k)
            op("dve", lambda e: e.scalar_tensor_tensor(out=L2_, in0=EQ, scalar=-1e30, in1=LG, op0=ALU.mult, op1=ALU.add), r=jk.k + lgt.k, w=jk.k)
            op("dve", lambda e: e.tensor_reduce(out=M2, in_=L2_, axis=AX.X, op=ALU.max), r=jk.k, w=lgt.k)
            op("dve", lambda e: e.tensor_scalar(out=SEL, in0=LG, scalar1=M2, scalar2=None, op0=ALU.is_ge), r=lgt.k, w=jk.k)
            op("dve", lambda e: e.tensor_scalar(out=NM1, in0=M1, scalar1=-1.0, scalar2=None, op0=ALU.mult), r=lgt.k, w=lgt.k)
            op("act", lambda e: e.activation(out=EX, in_=LG, func=AF.Exp, bias=NM1, scale=1.0), r=lgt.k, w=jk.k)
            op("act", lambda e: e.activation(out=E2, in_=M2, func=AF.Exp, bias=NM1, scale=1.0), r=lgt.k, w=lgt.k)
            op("dve", lambda e: e.tensor_scalar(out=E2, in0=E2, scalar1=1.0, scalar2=None, op0=ALU.add), r=lgt.k, w=lgt.k)
            op("dve", lambda e: e.reciprocal(out=E2, in_=E2), r=lgt.k, w=lgt.k)
            op("dve", lambda e: e.scalar_tensor_tensor(out=gates[:, tt, :], in0=EX, scalar=E2, in1=SEL, op0=ALU.mult, op1=ALU.mult),
               r=jk.k + lgt.k, w=[gates.k[tt]])
    else:
        router = None
    norm_off = L5.off
    L5.off = base
    acc = L5.get([128, NT, D], F32, NT)
    actT = [L5.get([128, 2, S], BF16) for _ in range(2)]
    sgt = [L5.get([128, 512], F32) for _ in range(2)]
    xt = [L5.get([128, D], F32) for _ in range(2)]
    L5end = L5.off
    L5.off = norm_off
    nE = NE if moe else 1
    F_ = DFE if moe else DFF
    nfg = F_ // 256
    its = [(e_, fg) for e_ in range(nE) for fg in range(nfg)]
    sets = [(C.wbufs[2 * i], C.wbufs[2 * i + 1]) for i in range(3)]

    def wdview(wdb):
        return wdb[:, 0:4, :].rearrange("p a b -> p (a b)").rearrange("p (c n) -> p c n", c=2)

    def load(i):
        e_, fg = its[i]
        wgu, wdb = sets[i % 3]
        wg = Wl["wg"][e_] if moe else Wl["wg"]
        wu = Wl["wu"][e_] if moe else Wl["wu"]
        wd = Wl["wd"][e_] if moe else Wl["wd"]
        f0 = fg * 256
        if os.environ.get("NOLOAD") and i > 2:
            return
        dma("pool", wgu[:, :, 0:256], wg[:, f0:f0 + 256].rearrange("(kc p) n -> p kc n", p=128), w=wgu.k)
        dma("pool", wgu[:, :, 256:512], wu[:, f0:f0 + 256].rearrange("(kc p) n -> p kc n", p=128), w=wgu.k)
        dma("pool", wdview(wdb), wd[f0:f0 + 256, :].rearrange("(c p) n -> p c n", p=128), w=wdb.k)

    def G_units(i):
        wgu, wdb = sets[i % 3]
        aT = actT[i % 2]
        units = []
        for tg in range(4):
            for fc in range(2):
                def u(tg=tg, fc=fc):
                    pG = psF[(2 * fc) % 4]; pU = psF[(2 * fc + 1) % 4]
                    for kc in range(8):
                        op("pe", lambda e: e.matmul(pG[:], lhsT=wgu[:, kc, fc * 128:(fc + 1) * 128], rhs=hT[:, kc, 1 + tg * 512:1 + (tg + 1) * 512],
                                                    start=(kc == 0), stop=(kc == 7)), r=wgu.k + [hT.k[tg]], w=pG.k)
                    for kc in range(8):
                        op("pe", lambda e: e.matmul(pU[:], lhsT=wgu[:, kc, 256 + fc * 128:256 + (fc + 1) * 128], rhs=hT[:, kc, 1 + tg * 512:1 + (tg + 1) * 512],
                                                    start=(kc == 0), stop=(kc == 7)), r=wgu.k + [hT.k[tg]], w=pU.k)
                    s_ = sgt[fc]
                    op("act", lambda e: e.activation(out=s_[:], in_=pG[:], func=AF.Silu), r=pG.k, w=s_.k)
                    op("dve", lambda e: e.tensor_tensor(out=aT[:, fc, tg * 512:(tg + 1) * 512], in0=pU[:], in1=s_[:], op=ALU.mult), r=pU.k + s_.k, w=aT.k)
                units.append(u)
        return units

    def D_units(i):
        e_, fg = its[i]
        wgu, wdb = sets[i % 3]
        wdv = wdview(wdb)
        aT = actT[i % 2]
        units = []
        for tt in range(NT):
            def u(tt=tt):
                for nh in range(2):
                    pD = psF[4 + (2 * tt + nh) % 4]
                    for fc in range(2):
                        op("pe", lambda e: e.matmul(pD[:], lhsT=aT[:, fc, tt * 128:(tt + 1) * 128], rhs=wdv[:, fc, nh * 512:(nh + 1) * 512],
                                                    start=(fc == 0), stop=(fc == 1)), r=aT.k + wdb.k, w=pD.k)
                    a_ = acc[:, tt, nh * 512:(nh + 1) * 512]
                    gsc = gates[:, tt, e_:e_ + 1] if moe else 1.0
                    rk = ([gates.k[tt]] if moe else [])
                    if i == 0:
                        op("dve", lambda e: e.tensor_scalar(out=a_, in0=pD[:], scalar1=gsc, scalar2=None, op0=ALU.mult), r=pD.k + rk, w=[acc.k[tt]])
                    else:
                        op("dve", lambda e: e.scalar_tensor_tensor(out=a_, in0=pD[:], scalar=gsc, in1=a_, op0=ALU.mult, op1=ALU.add),
                           r=pD.k + rk + [acc.k[tt]], w=[acc.k[tt]])
            units.append(u)
        return units

    n_it = len(its)
    load(0)
    if n_it > 1:
        load(1)
    g0 = G_units(0)

    def hook(tt):
        if tt % 4 == 3:
            tg = tt // 4
            g0[2 * tg]()
            g0[2 * tg + 1]()
    C.norm_phase(L5, lambda tt: (xs_d[tt], xs_d.k[tt]), C.A2, C.SH2, hT, router, hook)
    assert L5.off <= base + NT * D * 4, (L5.off, base)
    C.barrier()
    for i in range(n_it):
        if i + 2 < n_it:
            load(i + 2)
        d = D_units(i)
        g = G_units(i + 1) if i + 1 < n_it else []
        for k in range(8):
            if k < len(g):
                g[k]()
            d[2 * k]()
            d[2 * k + 1]()
    for tt in range(NT):
        x_ = xt[tt % 2]
        dma("sp", x_[:], xs_d[tt], r=[xs_d.k[tt]], w=x_.k)
        op("dve", lambda e: e.tensor_tensor(out=acc[:, tt, :], in0=acc[:, tt, :], in1=C.G2, op=ALU.mult), r=[acc.k[tt]] + C.modbc.k, w=[acc.k[tt]])
        op("pool", lambda e: e.tensor_tensor(out=x_[:], in0=x_[:], in1=acc[:, tt, :], op=ALU.add), r=x_.k + [acc.k[tt]], w=x_.k)
        dma("sp", xs_d[tt], x_[:], r=x_.k, w=[xs_d.k[tt]])
        if l in C.tapo:
            dma("sp", C.tapo[l][tt * 128:(tt + 1) * 128, :], x_[:], r=x_.k)
    C.barrier()


def _final(C):
    sc = C.sc; op = sc.op; dma = sc.dma
    xs_d = C.xs_d
    Lf = C.Lay()
    fg = Lf.get([128, D], F32)
    xt = [Lf.get([128, D], F32) for _ in range(2)]
    xo = [Lf.get([128, D], F32) for _ in range(2)]
    junk = Lf.get([128, D], BF16)
    ss = Lf.get([128, NT, 4], F32, NT)
    dma("sp", fg[:], C.final_g_in.partition_broadcast(128), w=fg.k)
    for tt in range(NT):
        x_ = xt[tt % 2]; o_ = xo[tt % 2]
        dma("sp", x_[:], xs_d[tt], r=[xs_d.k[tt]], w=x_.k)
        op("act", lambda e: e.activation(out=junk[:], in_=x_[:], func=AF.Square, accum_out=ss[:, tt, 0:1]), r=x_.k, w=junk.k + [ss.k[tt]])
        op("act", lambda e: e.activation(out=ss[:, tt, 1:2], in_=ss[:, tt, 0:1], func=AF.Sqrt, bias=RMS_EPS, scale=1.0 / D), r=[ss.k[tt]], w=[ss.k[tt]])
        op("dve", lambda e: e.reciprocal(out=ss[:, tt, 2:3], in_=ss[:, tt, 1:2]), r=[ss.k[tt]], w=[ss.k[tt]])
        op("dve", lambda e: e.scalar_tensor_tensor(out=o_[:], in0=x_[:], scalar=ss[:, tt, 2:3], in1=fg[:], op0=ALU.mult, op1=ALU.mult),
           r=x_.k + [ss.k[tt]] + fg.k, w=o_.k)
        dma("sp", C.out_d[tt * 128:(tt + 1) * 128, :], o_[:], r=o_.k)


_CONSTS = None


def make_in_maps(inputs, layers=(0, 1, 2, 3), cores=range(8)):
    global _CONSTS
    if _CONSTS is None:
        _CONSTS = host_consts()
    cst = _CONSTS
    f = lambda a: np.ascontiguousarray(np.asarray(a, dtype=np.float32))
    rel = np.asarray(inputs["rel_bias"], np.float32)
    abias = np.ascontiguousarray(rel[cst["_bucket"]].transpose(0, 2, 1))
    shared = {"k_" + k: f(v) for k, v in cst.items() if not k.startswith("_")}
    shared["abias"] = abias
    shared["final_g"] = f(inputs["final_g"])
    for l in layers:
        i = l // 2
        def put(nm, a):
            shared["%s_%d" % (nm, l)] = f(a)
        put("w_ada", inputs["w_ada"][l]); put("b_ada", inputs["b_ada"][l]); put("norm1_g", inputs["norm1_g"][l]); put("norm2_g", inputs["norm2_g"][l])
        put("w_in", inputs["w_in"][l]); put("w_out", inputs["w_out"][l]); put("mu", inputs["rwkv_mu"][l])
        put("w0", inputs["rwkv_w0"][l]); put("w2", inputs["rwkv_w2"][l]); put("a0", inputs["rwkv_a0"][l]); put("a2", inputs["rwkv_a2"][l])
        put("g2", inputs["rwkv_g2"][l]); put("k_k", inputs["rwkv_k_k"][l]); put("k_a", inputs["rwkv_k_a"][l])
        put("r_k", np.asarray(inputs["rwkv_r_k"][l]).reshape(-1)); put("ln_w", inputs["rwkv_ln_w"][l]); put("ln_b", inputs["rwkv_ln_b"][l])
        if l > 0:
            put("v0", inputs["rwkv_v0"][l - 1]); put("v1", inputs["rwkv_v1"][l - 1]); put("v2", inputs["rwkv_v2"][l - 1])
        if l % 2 == 0:
            put("wg", inputs["ffn_w_gate"][i]); put("wu", inputs["ffn_w_up"][i]); put("wd", inputs["ffn_w_down"][i])
        else:
            put("rw", np.asarray(inputs["moe_router_w"][i]).T); put("rb", inputs["moe_router_b"][i])
            put("wg", inputs["moe_w_gate"][i]); put("wu", inputs["moe_w_up"][i]); put("wd", inputs["moe_w_down"][i])
    maps = []
    for b in cores:
        m = dict(shared)
        m["x"] = f(inputs["x"][b])
        m["c"] = f(inputs["c"][b])
        maps.append(m)
    return maps


def kernel(**inputs):
    nc = bass.Bass("TRN2", target_bir_lowering=False)
    build(nc)
    maps = make_in_maps(inputs)
    res = run_bass_kernel_spmd(nc, maps, core_ids=list(range(8)))
    out = np.stack([np.asarray(r["out"], dtype=np.float32) for r in res.results], axis=0)
    return out
```

```python
import contextlib
import os
import math
import numpy as np
import concourse.bass as bass
import concourse.mybir as mybir
from concourse.bass_utils import run_bass_kernel_spmd

F32 = mybir.dt.float32
BF16 = mybir.dt.bfloat16
AF = mybir.ActivationFunctionType
ALU = mybir.AluOpType
AX = mybir.AxisListType

S = 2048
D = 1024
NT = 16
DEPTH = 4
HD = 64
AW = 512
RW = 512
NIN = 3360
DFF = 2816
DFE = 3584
NE = 8
RMS_EPS = 1e-6
GN_EPS = 64 * 1e-5
N_DSEM = 48


class Tk:
    __slots__ = ("w", "r")

    def __init__(self):
        self.w = None
        self.r = {}


class Sch:
    def __init__(self, nc, es):
        self.nc = nc
        self.es = es
        self.eng = {"pe": nc.tensor, "dve": nc.vector, "act": nc.scalar, "pool": nc.gpsimd, "sp": nc.sync}
        self.sem = {k: es.enter_context(nc.semaphore("s_" + k)) for k in self.eng}
        self.cnt = {k: 0 for k in self.eng}
        self.waited = {k: {} for k in self.eng}
        self.dsem = [es.enter_context(nc.semaphore("d%d" % i)) for i in range(N_DSEM)]
        self.dval = [0] * N_DSEM
        self.dnext = {"sp": 0, "pool": 0, "act": 0}
        self.drange = {"sp": (0, N_DSEM // 2), "act": (0, N_DSEM // 2), "pool": (N_DSEM // 2, N_DSEM)}
        self.nins = 0

    def _semof(self, key):
        if isinstance(key, tuple):
            return self.dsem[key[1]]
        return self.sem[key]

    def _wait(self, e, deps):
        w = self.waited[e]
        for key, c in deps.items():
            if w.get(key, 0) < c:
                self.eng[e].wait_ge(self._semof(key), c)
                w[key] = c
                self.nins += 1

    def _deps(self, e, r, w):
        deps = {}

        def add(key, c):
            if deps.get(key, 0) < c:
                deps[key] = c
        for t in r:
            if t.w is not None:
                add(*t.w)
        for t in w:
            if t.w is not None and not (t.w[0] == e and e == "pe"):
                add(*t.w)
            for key, c in t.r.items():
                add(key, c)
        return deps

    def op(self, e, fn, r=(), w=()):
        self._wait(e, self._deps(e, r, w))
        ins = fn(self.eng[e])
        self.cnt[e] += 1
        c = self.cnt[e]
        ins.then_inc(self.sem[e], 1)
        self.nins += 1
        for t in r:
            t.r[e] = c
        for t in w:
            t.w = (e, c)
            t.r = {}
        return ins

    def dma(self, q, out, in_, r=(), w=(), **kw):
        lo, hi = self.drange[q]
        i = lo + self.dnext[q]
        self.dnext[q] = (self.dnext[q] + 1) % (hi - lo)
        key = ("d", i)
        deps = self._deps(key, r, w)
        if self.dval[i] > 0:
            deps[key] = max(deps.get(key, 0), self.dval[i])
        self._wait(q, deps)
        self.dval[i] += 16
        c = self.dval[i]
        self.eng[q].dma_start(out=out, in_=in_, **kw).then_inc(self.dsem[i], 16)
        self.nins += 1
        for t in r:
            t.r[key] = c
        for t in w:
            t.w = (key, c)
            t.r = {}

    def wait_all(self, e, tks):
        deps = {}
        for t in tks:
            if t.w is not None:
                if deps.get(t.w[0], 0) < t.w[1]:
                    deps[t.w[0]] = t.w[1]
        self._wait(e, deps)


class Buf:
    def __init__(self, t, n=1):
        self.t = t
        self.k = [Tk() for _ in range(n)]

    def __getitem__(self, idx):
        return self.t[idx]


def t5_bucket(n):
    n = np.asarray(n)
    max_exact = 16
    large = max_exact + (np.log(np.maximum(n, 1) / max_exact) / np.log(2048 / max_exact) * 16).astype(np.int32)
    large = np.minimum(large, 31)
    return np.where(n < max_exact, n, large).astype(np.int32)


def host_consts():
    c = {}
    c["ident"] = np.eye(128, dtype=np.float32)
    j = np.arange(128)[:, None]
    t = np.arange(128)[None, :]
    mus = (j < t).astype(np.float32)
    mui = (j <= t).astype(np.float32)
    c["mu2"] = np.concatenate([mus, mui], axis=1)
    c["mls"] = (j > t).astype(np.float32)
    sm = np.ones((128, S), np.float32)
    sm[:, ::128] = 0.0
    c["scanmask"] = sm
    p = np.arange(128)
    c["headsel"] = np.stack([(p < 64), (p >= 64)], axis=1).astype(np.float32)
    c["blockones"] = ((p[:, None] // 64) == (p[None, :] // 64)).astype(np.float32)
    s_ = np.arange(128)[:, None]
    u_ = np.arange(S)[None, :]
    d = u_ - s_
    mult = ((d >= 0) & (d <= 128)).astype(np.float32) + ((d >= 0) & (d % 4 == 0) & (d <= 512)).astype(np.float32) \
        + ((d >= 0) & (d % 16 == 0) & (d <= 2048)).astype(np.float32)
    c["amult"] = mult.astype(np.float32)
    c["_bucket"] = t5_bucket(np.maximum(d, 0))
    return c


class Ctx:
    pass


def build(nc, layers=(0, 1, 2, 3), taps=(), final=True, upto=None):
    es = contextlib.ExitStack()
    C = Ctx()
    C.nc = nc
    C.es = es
    C.taps = {}
    with es:
        _build(C, layers, taps, final, upto)
    return C


def _dram_in(nc, name, shape, dt=F32):
    return nc.dram_tensor(name, list(shape), dt, kind="ExternalInput").ap()


def _build(C, layers, taps, final, upto):
    nc = C.nc
    es = C.es
    sc = Sch(nc, es)
    C.sc = sc

    def sb(name, shape, dt, n=1):
        return Buf(es.enter_context(nc.sbuf_tensor(name, list(shape), dt)), n)

    def ps(name, shape, dt=F32):
        return Buf(es.enter_context(nc.psum_tensor(name, list(shape), dt)), 1)

    def dscr(name, shape, dt=F32, n=1):
        return Buf(nc.dram_tensor(name, list(shape), dt).ap(), n)

    def tap(name, shape):
        if name in taps:
            C.taps[name] = nc.dram_tensor("tap_" + name, list(shape), F32, kind="ExternalOutput").ap()
            return C.taps[name]
        return None

    x_in = _dram_in(nc, "x", [S, D])
    c_in = _dram_in(nc, "c", [D])
    cst = {k: _dram_in(nc, "k_" + k, v.shape) for k, v in host_consts().items() if not k.startswith("_")}
    abias_in = _dram_in(nc, "abias", [128, 8, S])
    final_g_in = _dram_in(nc, "final_g", [D])
    out_d = nc.dram_tensor("out", [S, D], F32, kind="ExternalOutput").ap()
    W = {}
    for l in layers:
        W[l] = {}
        def di(nm, shape):
            W[l][nm] = _dram_in(nc, "%s_%d" % (nm, l), shape)
        di("w_ada", [D, 6 * D]); di("b_ada", [6 * D]); di("norm1_g", [D]); di("norm2_g", [D])
        di("w_in", [D, NIN]); di("w_out", [D, D]); di("mu", [1824])
        di("w0", [RW]); di("w2", [64, RW]); di("a0", [RW]); di("a2", [64, RW]); di("g2", [160, RW])
        di("k_k", [RW]); di("k_a", [RW]); di("r_k", [RW]); di("ln_w", [RW]); di("ln_b", [RW])
        if l > 0:
            di("v0", [RW]); di("v1", [RW, 32]); di("v2", [32, RW])
        if l % 2 == 0:
            di("wg", [D, DFF]); di("wu", [D, DFF]); di("wd", [DFF, D])
        else:
            di("rw", [NE, D]); di("rb", [NE]); di("wg", [NE, D, DFE]); di("wu", [NE, D, DFE]); di("wd", [NE, DFE, D])

    xs_d = dscr("xs_d", [NT, 128, D], F32, NT)
    rT_d = dscr("rT_d", [4, 128, S], F32, 4)
    kT_d = dscr("kT_d", [4, 128, S], F32, 4)
    y_d = dscr("y_d", [NT, 128, RW], F32, NT)
    vf_d = dscr("vf_d", [NT, 128, RW], F32, NT)

    identb = sb("identb", [128, 128], BF16)
    mu2 = sb("mu2", [128, 256], F32)
    mls = sb("mls", [128, 128], F32)
    headsel = sb("headsel", [128, 2], BF16)
    blockones = sb("blockones", [128, 128], BF16)
    onesrow = sb("onesrow", [1, 128], BF16)
    cact = sb("cact", [128, 8], F32)
    cactb = sb("cactb", [128, 8, 128], BF16)
    modbc = sb("modbc", [128, 3 * D], F32)
    pcol = sb("pcol", [128, 8, 4], F32)
    rowb = sb("rowb", [1, 512], BF16)
    wbufs = [sb("wbuf%d" % i, [128, 8, 512], BF16, 2) for i in range(6)]
    C.wnext = 0
    ARENA = 143360
    arena_t = es.enter_context(nc.sbuf_tensor("arena", [128, ARENA // 2], BF16))
    amask_d = dscr("amask_d", [128, 8 * S], BF16)
    vr_d = dscr("vr_d", [NT, 128, RW], BF16, NT)
    prod_d = dscr("prod_d", [4, 128, S], BF16, 4)
    C.layers = list(layers)
    C.first_from_input = True
    C.tapx = {}
    C.tapo = {}
    for nm in taps:
        if nm.startswith("xmid"):
            C.tapx[int(nm[4:])] = nc.dram_tensor("tap_" + nm, [S, D], F32, kind="ExternalOutput").ap()
        if nm.startswith("xout"):
            C.tapo[int(nm[4:])] = nc.dram_tensor("tap_" + nm, [S, D], F32, kind="ExternalOutput").ap()
    psF = [ps("psF%d" % i, [128, 512], F32) for i in range(8)]
    psB = []
    for i in range(2):
        b_ = Buf(psF[6 + i].t[:, :].bitcast(BF16).rearrange("p (a b) -> p a b", a=8), 1)
        b_.k = psF[6 + i].k
        psB.append(b_)
    C.pf = 0

    def carve(off, shape, dt):
        n = 1
        for d_ in shape[1:]:
            n *= d_
        esz = 4 if dt == F32 else 2
        assert off % 4 == 0 and off + n * esz <= ARENA, (off, shape)
        ap = arena_t[0:shape[0], off // 2: off // 2 + n * esz // 2]
        if dt == F32:
            ap = ap.bitcast(F32)
        if len(shape) == 3:
            ap = ap.rearrange("p (a b) -> p a b", a=shape[1])
        elif len(shape) == 4:
            ap = ap.rearrange("p (a b c) -> p a b c", a=shape[1], b=shape[2])
        return Buf(ap, 1)

    class Lay:
        def __init__(self):
            self.off = 0

        def get(self, shape, dt, n=1):
            nb = (4 if dt == F32 else 2)
            for d_ in shape[1:]:
                nb *= d_
            nb = (nb + 31) // 32 * 32
            b_ = carve(self.off, shape, dt)
            b_.k = [Tk() for _ in range(n)]
            self.off += nb
            return b_

    def barrier():
        engs = list(sc.eng.keys())
        for e in engs:
            deps = {}
            for o in engs:
                if o != e and sc.cnt[o] > 0:
                    deps[o] = sc.cnt[o]
            for i in range(N_DSEM):
                if sc.dval[i] > 0:
                    deps[("d", i)] = sc.dval[i]
            sc._wait(e, deps)

    def nextw():
        b = wbufs[C.wnext]
        C.wnext = (C.wnext + 1) % len(wbufs)
        return b

    def nextps(lo=0, hi=6):
        C.pf = (C.pf + 1) % (hi - lo)
        return psF[lo + C.pf]

    op = sc.op
    dma = sc.dma

    dma("pool", identb[:], cst["ident"], w=identb.k)
    dma("sp", mu2[:], cst["mu2"], w=mu2.k)
    dma("sp", mls[:], cst["mls"], w=mls.k)
    dma("pool", headsel[:], cst["headsel"], w=headsel.k)
    dma("pool", blockones[:], cst["blockones"], w=blockones.k)
    op("dve", lambda e: e.memset(onesrow[:], 1.0), w=onesrow.k)
    L0 = Lay()
    amt = L0.get([128, S], F32)
    amm = L0.get([128, S], F32)
    amo = L0.get([128, S], BF16)
    dma("sp", amm[:], cst["amult"], w=amm.k)
    for h in range(8):
        dma("sp", amt[:], abias_in[:, h, :], w=amt.k)
        op("act", lambda e: e.activation(out=amt[:], in_=amt[:], func=AF.Exp), r=amt.k, w=amt.k)
        op("dve", lambda e: e.tensor_tensor(out=amo[:], in0=amt[:], in1=amm[:], op=ALU.mult), r=amt.k + amm.k, w=amo.k)
        dma("sp", amask_d[:, h * S:(h + 1) * S], amo[:], r=amo.k, w=amask_d.k)
    dma("sp", cact[:], c_in.rearrange("(c p) -> p c", p=128), w=cact.k, allow_slow_non_contiguous=True)
    op("act", lambda e: e.activation(out=cact[:], in_=cact[:], func=AF.Silu), r=cact.k, w=cact.k)
    op("dve", lambda e: e.tensor_copy(out=cactb[:], in_=cact[:].unsqueeze(2).to_broadcast([128, 8, 128])), r=cact.k, w=cactb.k)

    barrier()
    C.__dict__.update(locals())
    for li, l in enumerate(layers):
        _layer(C, l, first=(li == 0))
        if upto is not None and l == upto[0]:
            break
    if final:
        _final(C)
    for i in range(N_DSEM):
        if sc.dval[i]:
            nc.sync.wait_ge(sc.dsem[i], sc.dval[i])


def _layer(C, l, first):
    nc = C.nc; sc = C.sc; op = sc.op; dma = sc.dma
    Wl = C.W[l]
    psF = C.psF; psB = C.psB
    identb = C.identb; modbc = C.modbc; cactb = C.cactb; onesrow = C.onesrow; rowb = C.rowb
    nextw = C.nextw; Lay = C.Lay; carve = C.carve; barrier = C.barrier
    ARENA = C.ARENA
    xs_d = C.xs_d; rT_d = C.rT_d; kT_d = C.kT_d; y_d = C.y_d; vf_d = C.vf_d; vr_d = C.vr_d
    x_in = C.x_in
    SH1, A1, G1 = [modbc[:, i * D:(i + 1) * D] for i in range(3)]
    SH2, A2, G2 = SH1, A1, G1

    def bcast_row(vec_ap, n):
        return vec_ap.partition_broadcast(128)

    top = ARENA
    def topget(shape, dt):
        nonlocal top
        nb = 4 if dt == F32 else 2
        for d_ in shape[1:]:
            nb *= d_
        nb = (nb + 31) // 32 * 32
        top -= nb
        return carve(top, shape, dt)
    w2b = topget([128, RW], BF16); a2b = topget([128, RW], BF16)
    g2b = topget([128, RW], BF16); g2b2 = topget([128, RW], BF16)
    v1b = topget([128, 4, 32], BF16); v2b = topget([128, RW], BF16)
    lnw_bc = topget([128, RW], F32); lnb_bc = topget([128, RW], F32); v0_bc = topget([128, RW], F32)
    pcol = C.pcol
    twd = topget([128, S], BF16); adT = topget([128, S], BF16); sgd = topget([128, S], BF16); sgd2 = topget([128, S], BF16)
    att = topget([128, NT, AW], BF16)
    att.k = [Tk() for _ in range(NT)]
    TOP_P1 = twd_top = top + 16384
    TOP_P2 = top

    dma("pool", w2b[0:64, :], Wl["w2"], w=w2b.k)
    dma("pool", a2b[0:64, :], Wl["a2"], w=a2b.k)
    dma("pool", g2b[:, :], Wl["g2"][0:128, :], w=g2b.k)
    dma("pool", g2b2[0:32, :], Wl["g2"][128:160, :], w=g2b2.k)
    dma("sp", lnw_bc[:], Wl["ln_w"].partition_broadcast(128), w=lnw_bc.k)
    dma("sp", lnb_bc[:], Wl["ln_b"].partition_broadcast(128), w=lnb_bc.k)
    if l > 0:
        dma("pool", v1b[:], Wl["v1"].rearrange("(c p) n -> p c n", p=128), w=v1b.k)
        dma("pool", v2b[0:32, :], Wl["v2"], w=v2b.k)
        dma("sp", v0_bc[:], Wl["v0"].partition_broadcast(128), w=v0_bc.k)
    for i, nm in enumerate(["w0", "a0", "k_k", "k_a", "k_a", "r_k"]):
        dma("sp", pcol[:, i, :], Wl[nm].rearrange("(c p) -> p c", p=128), w=pcol.k, allow_slow_non_contiguous=True)
    op("dve", lambda e: e.tensor_scalar(out=pcol[:, 4, :], in0=pcol[:, 4, :], scalar1=-1.0, scalar2=1.0, op0=ALU.mult, op1=ALU.add),
       r=pcol.k, w=pcol.k)

    C.__dict__.update({k_: v_ for k_, v_ in locals().items() if k_ not in ("C",)})
    _adaln(C, l, 0)
    if os.environ.get("KSTOP", "") == "A":
        return

    L1 = Lay()
    QT = L1.get([128, 4, S], BF16); KT = L1.get([128, 4, S], BF16)
    Vaug = L1.get([128, NT, 8, 65], BF16)
    P2base = L1.off
    hT = L1.get([128, 8, S + 2], BF16)
    P1base = L1.off

    def src1(tt):
        if first:
            return x_in[tt * 128:(tt + 1) * 128, :], None
        return xs_d[tt], xs_d.k[tt]

    def norm_phase(L_, src, Abc, SHbc, hT_, router=None, hook=None):
        modbc = C.modbc
        xt = [L_.get([128, D], F32) for _ in range(2)]
        hf = L_.get([128, D], F32)
        hb = [L_.get([128, D], BF16) for _ in range(2)]
        junk = L_.get([128, D], BF16)
        ss = L_.get([128, NT, 4], F32, NT)
        op("dve", lambda e: e.memset(hT_[:, :, 0:1], 0.0), w=hT_.k)
        for tt in range(NT):
            x_ = xt[tt % 2]
            ap, tk = src(tt)
            dma("sp", x_[:], ap, r=([tk] if tk is not None else []), w=x_.k)
            op("act", lambda e: e.activation(out=junk[:], in_=x_[:], func=AF.Square, accum_out=ss[:, tt, 0:1]),
               r=x_.k, w=junk.k + [ss.k[tt]])
            op("act", lambda e: e.activation(out=ss[:, tt, 1:2], in_=ss[:, tt, 0:1], func=AF.Sqrt, bias=RMS_EPS, scale=1.0 / D),
               r=[ss.k[tt]], w=[ss.k[tt]])
            op("dve", lambda e: e.reciprocal(out=ss[:, tt, 2:3], in_=ss[:, tt, 1:2]), r=[ss.k[tt]], w=[ss.k[tt]])
            op("dve", lambda e: e.scalar_tensor_tensor(out=hf[:], in0=x_[:], scalar=ss[:, tt, 2:3], in1=Abc, op0=ALU.mult, op1=ALU.mult),
               r=x_.k + [ss.k[tt]] + modbc.k, w=hf.k)
            h_ = hb[tt % 2]
            if router is None:
                op("dve", lambda e: e.tensor_tensor(out=h_[:], in0=hf[:], in1=SHbc, op=ALU.add), r=hf.k + modbc.k, w=h_.k)
            else:
                op("pool", lambda e: e.tensor_tensor(out=hf[:], in0=hf[:], in1=SHbc, op=ALU.add), r=hf.k + modbc.k, w=hf.k)
                op("act", lambda e: e.copy(out=h_[:], in_=hf[:]), r=hf.k, w=h_.k)
                router(tt, hf)
            pb = psB[tt % 2]
            for kc in range(8):
                op("pe", lambda e: e.transpose(out=pb[:, kc, :], in_=h_[:, kc * 128:(kc + 1) * 128], identity=identb[:]),
                   r=h_.k + identb.k, w=pb.k)
            op("act", lambda e: e.copy(out=hT_[:, :, 1 + tt * 128:1 + (tt + 1) * 128], in_=pb[:]), r=pb.k, w=[hT_.k[(tt // 4) % len(hT_.k)]])
            if hook is not None:
                hook(tt)

    norm_phase(L1, src1, A1, SH1, hT)
    barrier()
    L1.off = P1base
    mubc = L1.get([128, 1824], F32)
    dma("sp", mubc[:], Wl["mu"].partition_broadcast(128), w=mubc.k)
    stg = [L1.get([128, 512], F32) for _ in range(2)]
    stgb = [L1.get([128, 512], BF16) for _ in range(2)]
    vTs = [L1.get([128, 512], BF16) for _ in range(2)]
    zT = L1.get([128, S], BF16)
    vft = [L1.get([128, 512], F32) for _ in range(2)]
    gt = L1.get([128, 512], F32)
    assert L1.off <= TOP_P1, (L1.off, TOP_P1)
    op("pool", lambda e: e.memset(Vaug[:, :, :, 64:65], 1.0), w=Vaug.k)
    C.stgi = 0

    def load_w(c0, cw):
        wb_ = nextw()
        dma("pool", wb_[:, :, 0:cw], Wl["w_in"][:, c0:c0 + cw].rearrange("(kc p) n -> p kc n", p=128), w=wb_.k)
        return wb_

    def scaled(wb_, c0, cw):
        W1 = nextw(); W2 = nextw()
        mu_b = mubc[:, c0 - 1536:c0 - 1536 + cw].unsqueeze(1).to_broadcast([128, 8, cw])
        op("dve", lambda e: e.tensor_tensor(out=W2[:, :, 0:cw], in0=wb_[:, :, 0:cw], in1=mu_b, op=ALU.mult), r=wb_.k + mubc.k, w=W2.k)
        op("pool", lambda e: e.tensor_tensor(out=W1[:, :, 0:cw], in0=wb_[:, :, 0:cw], in1=W2[:, :, 0:cw], op=ALU.subtract),
           r=wb_.k + W2.k, w=W1.k)
        return [(W1, 0), (W2, 1)]

    def fm_proj(wlist, sub0, subw, tg, evac):
        pt = psF[C.pf % 4]; C.pf += 1
        n = len(wlist) * 8
        i = 0
        for (w_, shift) in wlist:
            for kc in range(8):
                o = 1 + tg * 512 - shift
                op("pe", lambda e: e.matmul(pt[0:subw, :], lhsT=w_[:, kc, sub0:sub0 + subw], rhs=hT[:, kc, o:o + 512],
                                            start=(i == 0), stop=(i == n - 1)), r=w_.k + hT.k, w=pt.k)
                i += 1
        evac(pt)

    def tm_proj(wlist, tt, ncols):
        pt = psF[C.pf % 4]; C.pf += 1
        n = len(wlist) * 8
        i = 0
        for (w_, shift) in wlist:
            for kc in range(8):
                o = 1 + tt * 128 - shift
                op("pe", lambda e: e.matmul(pt[:, 0:ncols], lhsT=hT[:, kc, o:o + 128], rhs=w_[:, kc, 0:ncols],
                                            start=(i == 0), stop=(i == n - 1)), r=w_.k + hT.k, w=pt.k)
                i += 1
        return pt

    for (dst, c0, scl) in ((QT, 0, 0.125), (KT, 512, 1.0)):
        wb = load_w(c0, 512)
        for sub in range(4):
            for tg in range(4):
                def ev(pt, dst=dst, sub=sub, tg=tg, scl=scl):
                    op("act", lambda e: e.mul(out=dst[:, sub, tg * 512:(tg + 1) * 512], in_=pt[:], mul=scl), r=pt.k, w=dst.k)
                fm_proj([(wb, 0)], sub * 128, 128, tg, ev)
    wb = load_w(1024, 512)
    for tt in range(NT):
        pt = tm_proj([(wb, 0)], tt, 512)
        op("dve", lambda e: e.tensor_copy(out=Vaug[:, tt, :, 0:64], in_=pt[:].rearrange("p (h d) -> p h d", h=8)), r=pt.k, w=Vaug.k)
    for (dst_d, c0) in ((rT_d, 1536), (kT_d, 2048)):
        wb = load_w(c0, 512)
        wl = scaled(wb, c0, 512)
        for sub in range(4):
            for tg in range(4):
                def ev(pt, dst_d=dst_d, sub=sub, tg=tg):
                    s_ = stg[C.stgi % 2]; C.stgi += 1
                    op("act", lambda e: e.copy(out=s_[:], in_=pt[:]), r=pt.k, w=s_.k)
                    dma("sp", dst_d[sub, :, tg * 512:(tg + 1) * 512], s_[:], r=s_.k, w=[dst_d.k[sub]])
                fm_proj(wl, sub * 128, 128, tg, ev)
    wb = load_w(2560, 512)
    wl = scaled(wb, 2560, 512)
    if l > 0:
        for tg in range(4):
            zp = psF[4]
            for sub in range(4):
                def ev(pt, sub=sub, tg=tg):
                    v_ = vTs[sub % 2]
                    op("act", lambda e: e.copy(out=v_[:], in_=pt[:]), r=pt.k, w=v_.k)
                    op("pe", lambda e: e.matmul(zp[0:32, :], lhsT=v1b[:, sub, :], rhs=v_[:], start=(sub == 0), stop=(sub == 3)),
                       r=v1b.k + v_.k, w=zp.k)
                fm_proj(wl, sub * 128, 128, tg, ev)
            op("act", lambda e: e.copy(out=zT[0:32, tg * 512:(tg + 1) * 512], in_=zp[0:32, :]), r=zp.k, w=zT.k)
    for tt in range(NT):
        pt = tm_proj(wl, tt, 512)
        s_ = stg[tt % 2]; sb_ = stgb[tt % 2]
        op("act", lambda e: e.copy(out=s_[:], in_=pt[:]), r=pt.k, w=s_.k)
        if l == 0:
            dma("sp", vf_d[tt], s_[:], r=s_.k, w=[vf_d.k[tt]])
            op("dve", lambda e: e.tensor_copy(out=sb_[:], in_=s_[:]), r=s_.k, w=sb_.k)
        else:
            vf = vft[tt % 2]
            dma("sp", vf[:], vf_d[tt], r=[vf_d.k[tt]], w=vf.k)
            gp = psF[5]
            op("pe", lambda e: e.matmul(gp[:], lhsT=zT[0:32, tt * 128:(tt + 1) * 128], rhs=v2b[0:32, :], start=True, stop=True),
               r=zT.k + v2b.k, w=gp.k)
            op("dve", lambda e: e.tensor_tensor(out=gt[:], in0=gp[:], in1=v0_bc[:], op=ALU.add), r=gp.k + v0_bc.k, w=gt.k)
            op("act", lambda e: e.activation(out=gt[:], in_=gt[:], func=AF.Sigmoid), r=gt.k, w=gt.k)
            op("dve", lambda e: e.tensor_tensor(out=vf[:], in0=vf[:], in1=s_[:], op=ALU.subtract), r=vf.k + s_.k, w=vf.k)
            op("dve", lambda e: e.tensor_tensor(out=vf[:], in0=vf[:], in1=gt[:], op=ALU.mult), r=vf.k + gt.k, w=vf.k)
            op("dve", lambda e: e.tensor_tensor(out=sb_[:], in0=vf[:], in1=s_[:], op=ALU.add), r=vf.k + s_.k, w=sb_.k)
        dma("sp", vr_d[tt], sb_[:], r=sb_.k, w=[vr_d.k[tt]])
    wb = load_w(3072, 288)
    wl = scaled(wb, 3072, 288)
    for (sub0, subw, dstb, fn) in ((0, 64, twd, AF.Tanh), (64, 64, adT, AF.Copy), (128, 128, sgd, AF.Sigmoid), (256, 32, sgd2, AF.Sigmoid)):
        for tg in range(4):
            def ev(pt, subw=subw, dstb=dstb, fn=fn, tg=tg):
                op("act", lambda e: e.activation(out=dstb[0:subw, tg * 512:(tg + 1) * 512], in_=pt[0:subw, :], func=fn), r=pt.k, w=dstb.k)
            fm_proj(wl, sub0, subw, tg, ev)
    barrier()
    C.__dict__.update({k_: v_ for k_, v_ in locals().items() if k_ not in ("C",)})
    stop = os.environ.get("KSTOP", "")
    if stop == "P1":
        return
    _attention(C, l)
    if stop == "P2":
        return
    _rwkv(C, l)
    if stop == "P3":
        return
    _outproj(C, l)
    if stop == "P4":
        return
    _ffn(C, l)


def _adaln(C, l, half):
    sc = C.sc; op = sc.op; dma = sc.dma
    Wl = C.W[l]
    psF = C.psF; modbc = C.modbc; cactb = C.cactb; onesrow = C.onesrow; rowb = C.rowb
    LA = C.Lay()
    ng = LA.get([128, D], F32)
    dma("sp", ng[:], Wl["norm1_g" if half == 0 else "norm2_g"].partition_broadcast(128), w=ng.k)
    for ch in range(6):
        gch = half * 6 + ch
        wb = C.nextw()
        dma("pool", wb[:], Wl["w_ada"][:, gch * 512:(gch + 1) * 512].rearrange("(kc p) n -> p kc n", p=128), w=wb.k)
        dma("pool", rowb[:], Wl["b_ada"][gch * 512:(gch + 1) * 512].rearrange("(o n) -> o n", o=1), w=rowb.k)
        pt = psF[ch % 2]
        for kc in range(8):
            op("pe", lambda e: e.matmul(pt[:], lhsT=cactb[:, kc, :], rhs=wb[:, kc, :], start=(kc == 0), stop=False),
               r=cactb.k + wb.k, w=pt.k)
        op("pe", lambda e: e.matmul(pt[:], lhsT=onesrow[0:1, :], rhs=rowb[0:1, :], start=False, stop=True),
           r=onesrow.k + rowb.k, w=pt.k)
        which, hh = ch // 2, ch % 2
        dst = modbc[:, ch * 512:(ch + 1) * 512]
        if which == 1:
            op("dve", lambda e: e.scalar_tensor_tensor(out=dst, in0=pt[:], scalar=1.0, in1=ng[:, hh * 512:(hh + 1) * 512],
                                                       op0=ALU.add, op1=ALU.mult), r=pt.k + ng.k, w=modbc.k)
        else:
            op("act", lambda e: e.copy(out=dst, in_=pt[:]), r=pt.k, w=modbc.k)
    C.barrier()


def _attention(C, l):
    sc = C.sc; op = sc.op; dma = sc.dma
    psF = C.psF
    QT = C.QT; KT = C.KT; Vaug = C.Vaug; att = C.att
    L2 = C.Lay()
    L2.off = C.P2base
    amask = L2.get([128, 8, S], BF16)
    Pt = [L2.get([128, 512], BF16) for _ in range(4)]
    rz = L2.get([128, 8], F32)
    assert L2.off <= C.TOP_P2
    for h in range(8):
        dma("sp", amask[:, h, :], C.amask_d[:, h * S:(h + 1) * S], r=C.amask_d.k, w=amask.k)
    steps = [(h, qg, j) for h in range(8) for qg in range(4) for j in range(4 * qg + 4)]

    def unit_a(i):
        h, qg, j = steps[i]
        hp, po = h // 2, (h % 2) * 64
        qlo = max(4 * qg, j)
        nq = (4 * qg + 4 - qlo) * 128
        pS = psF[4 + (i % 4)]
        P_ = Pt[i % 4]
        op("pe", lambda e: e.matmul(pS[:, 0:nq], lhsT=KT[po:po + 64, hp, j * 128:(j + 1) * 128],
                                    rhs=QT[po:po + 64, hp, qlo * 128:qlo * 128 + nq], start=True, stop=True),
           r=KT.k + QT.k, w=pS.k)
        op("act", lambda e: e.activation(out=P_[:, 0:nq], in_=pS[:, 0:nq], func=AF.Exp), r=pS.k, w=P_.k)
        u0 = (qlo - j) * 128
        op("dve", lambda e: e.tensor_tensor(out=P_[:, 0:nq], in0=P_[:, 0:nq], in1=amask[:, h, u0:u0 + nq], op=ALU.mult),
           r=P_.k + amask.k, w=P_.k)

    def unit_b(i):
        h, qg, j = steps[i]
        qlo = max(4 * qg, j)
        P_ = Pt[i % 4]
        for ii in range(qlo, 4 * qg + 4):
            pO = psF[ii - 4 * qg]
            op("pe", lambda e: e.matmul(pO[:, 0:65], lhsT=P_[:, (ii - qlo) * 128:(ii - qlo + 1) * 128], rhs=Vaug[:, j, h, :],
                                        start=(j == 0), stop=(j == ii)), r=P_.k + Vaug.k, w=pO.k)
        if j == 4 * qg + 3:
            for ib in range(4):
                ii = 4 * qg + ib
                pO = psF[ib]
                op("dve", lambda e: e.reciprocal(out=rz[:, h:h + 1], in_=pO[:, 64:65]), r=pO.k, w=rz.k)
                op("dve", lambda e: e.tensor_scalar(out=att[:, ii, h * 64:(h + 1) * 64], in0=pO[:, 0:64], scalar1=rz[:, h:h + 1], scalar2=None,
                                                    op0=ALU.mult), r=pO.k + rz.k, w=[att.k[ii]])

    LOOK = 3
    for i in range(min(LOOK, len(steps))):
        unit_a(i)
    for i in range(len(steps)):
        unit_b(i)
        if i + LOOK < len(steps):
            unit_a(i + LOOK)
    barrier = C.barrier
    barrier()


def _rwkv(C, l):
    sc = C.sc
    defer = {"lst": None}

    def op(e, fn, r=(), w=()):
        if defer["lst"] is not None:
            defer["lst"].append(lambda: sc.op(e, fn, r, w))
            return None
        return sc.op(e, fn, r, w)

    def dma(q, out, in_, r=(), w=(), **kw):
        if defer["lst"] is not None:
            defer["lst"].append(lambda: sc.dma(q, out, in_, r, w, **kw))
            return None
        return sc.dma(q, out, in_, r, w, **kw)
    psF = C.psF; psB = C.psB
    identb = C.identb; mu2 = C.mu2; mls = C.mls; blockones = C.blockones
    pcol = C.pcol; w2b = C.w2b; a2b = C.a2b; twd = C.twd; adT = C.adT
    rT_d = C.rT_d; kT_d = C.kT_d; y_d = C.y_d; vr_d = C.vr_d
    L3 = C.Lay()
    ARt = L3.get([128, NT, 256], BF16, 4)
    scanmask = L3.get([128, S], BF16)
    dma("pool", scanmask[:], C.cst["scanmask"], w=scanmask.k)
    bt = L3.get([128, S], BF16, 4); kt = L3.get([128, S], BF16, 4); bh = L3.get([128, S], BF16, 4); kh = L3.get([128, S], BF16, 4)
    prodb = L3.get([128, S], BF16)
    WC = L3.get([128, NT], F32, 4)
    rf = L3.get([128, 512], F32); kf = L3.get([128, 512], F32); lw = L3.get([128, 512], F32); af = L3.get([128, 512], F32)
    Lc = L3.get([128, 512], F32); t1 = L3.get([128, 512], F32); t2 = L3.get([128, 512], F32); t3 = L3.get([128, 512], F32)
    t4 = L3.get([128, 512], F32); sqb = L3.get([128, 512], BF16)
    N1s = [L3.get([128, 4, 256], BF16) for _ in range(2)]
    N2s = [L3.get([128, 4, 256], BF16) for _ in range(2)]
    N3s = L3.get([128, 4, 128], BF16)
    Ab = [L3.get([128, 4, 128], BF16) for _ in range(2)]
    ATb = [L3.get([128, 4, 128], BF16) for _ in range(2)]
    Pb = [L3.get([128, 4, 128], BF16) for _ in range(2)]
    Tinv = [L3.get([128, 4, 128], BF16) for _ in range(2)]
    ZB = [L3.get([128, 2, 2, 128], BF16) for _ in range(2)]
    ZK = [L3.get([128, 2, 2, 128], BF16) for _ in range(2)]
    Vt = [L3.get([128, RW], BF16) for _ in range(4)]
    Xs = L3.get([128, 128], BF16); Us = L3.get([128, 128], BF16)
    Sf = L3.get([128, 64], F32); Sbf = L3.get([128, 2, 64], BF16)
    ystg = [L3.get([128, 128], F32) for _ in range(2)]
    assert L3.off <= C.TOP_P2, (L3.off, C.TOP_P2)
    if os.environ.get("KDEBUG"):
        print("L3.off", L3.off, "TOP_P2", C.TOP_P2)
    prod_d = C.prod_d
    NEG = -math.exp(-0.5)
    for z_ in ZB + ZK:
        op("pool", lambda e: e.memset(z_[:], 0.0), w=z_.k)

    def prep_unit(hp, tg):
        pc = lambda i: pcol[:, i, hp:hp + 1]
        hcols = slice(hp * 128, (hp + 1) * 128)
        ts_ = slice(tg * 512, (tg + 1) * 512)
        dma("sp", rf[:], rT_d[hp, :, ts_], r=[rT_d.k[hp]], w=rf.k)
        dma("sp", kf[:], kT_d[hp, :, ts_], r=[kT_d.k[hp]], w=kf.k)
        pz = psF[5]
        op("pe", lambda e: e.matmul(pz[:], lhsT=w2b[0:64, hcols], rhs=twd[0:64, ts_], start=True, stop=True), r=w2b.k + twd.k, w=pz.k)
        op("act", lambda e: e.activation(out=lw[:], in_=pz[:], func=AF.Sigmoid, bias=pc(0), scale=1.0), r=pz.k + pcol.k, w=lw.k)
        op("pool", lambda e: e.tensor_scalar(out=lw[:], in0=lw[:], scalar1=NEG, scalar2=None, op0=ALU.mult), r=lw.k, w=lw.k)
        pz2 = psF[5]
        op("pe", lambda e: e.matmul(pz2[:], lhsT=a2b[0:64, hcols], rhs=adT[0:64, ts_], start=True, stop=True), r=a2b.k + adT.k, w=pz2.k)
        op("act", lambda e: e.activation(out=af[:], in_=pz2[:], func=AF.Sigmoid, bias=pc(1), scale=1.0), r=pz2.k + pcol.k, w=af.k)
        op("dve", lambda e: e.tensor_tensor_scan(out=Lc[:], data0=scanmask[:, ts_], data1=lw[:], initial=0.0, op0=ALU.mult, op1=ALU.add),
           r=scanmask.k + lw.k, w=Lc.k)
        op("dve", lambda e: e.tensor_scalar(out=t1[:], in0=kf[:], scalar1=pc(2), scalar2=None, op0=ALU.mult), r=kf.k + pcol.k, w=t1.k)
        op("act", lambda e: e.activation(out=sqb[:], in_=t1[:], func=AF.Square), r=t1.k, w=sqb.k)
        pn = psF[5]
        op("pe", lambda e: e.matmul(pn[:], lhsT=blockones[:], rhs=sqb[:], start=True, stop=True), r=blockones.k + sqb.k, w=pn.k)
        op("act", lambda e: e.activation(out=t2[:], in_=pn[:], func=AF.Sqrt), r=pn.k, w=t2.k)
        op("dve", lambda e: e.tensor_scalar(out=t2[:], in0=t2[:], scalar1=1e-12, scalar2=None, op0=ALU.max), r=t2.k, w=t2.k)
        op("dve", lambda e: e.reciprocal(out=t2[:], in_=t2[:]), r=t2.k, w=t2.k)
        op("dve", lambda e: e.tensor_tensor(out=t1[:], in0=t1[:], in1=t2[:], op=ALU.mult), r=t1.k + t2.k, w=t1.k)
        op("dve", lambda e: e.tensor_scalar(out=t3[:], in0=af[:], scalar1=pc(3), scalar2=pc(4), op0=ALU.mult, op1=ALU.add),
           r=af.k + pcol.k, w=t3.k)
        op("dve", lambda e: e.tensor_tensor(out=kf[:], in0=kf[:], in1=t3[:], op=ALU.mult), r=kf.k + t3.k, w=kf.k)
        op("dve", lambda e: e.tensor_tensor(out=t3[:], in0=t1[:], in1=af[:], op=ALU.mult), r=t1.k + af.k, w=t3.k)
        op("dve", lambda e: e.scalar_tensor_tensor(out=prodb[:, ts_], in0=rf[:], scalar=pc(5), in1=kf[:], op0=ALU.mult, op1=ALU.mult),
           r=rf.k + kf.k + pcol.k, w=prodb.k)
        op("act", lambda e: e.activation(out=t2[:], in_=Lc[:], func=AF.Exp), r=Lc.k, w=t2.k)
        op("dve", lambda e: e.tensor_tensor(out=ARt[:, 4 * tg:4 * tg + 4, 128:256], in0=rf[:].rearrange("p (a b) -> p a b", a=4),
                                            in1=t2[:].rearrange("p (a b) -> p a b", a=4), op=ALU.mult), r=rf.k + t2.k, w=[ARt.k[tg]])
        op("pool", lambda e: e.tensor_tensor(out=t4[:], in0=Lc[:], in1=lw[:], op=ALU.subtract), r=Lc.k + lw.k, w=t4.k)
        op("act", lambda e: e.activation(out=t4[:], in_=t4[:], func=AF.Exp), r=t4.k, w=t4.k)
        op("dve", lambda e: e.scalar_tensor_tensor(out=ARt[:, 4 * tg:4 * tg + 4, 0:128], in0=t1[:].rearrange("p (a b) -> p a b", a=4),
                                                   scalar=-1.0, in1=t4[:].rearrange("p (a b) -> p a b", a=4), op0=ALU.mult, op1=ALU.mult),
           r=t1.k + t4.k, w=[ARt.k[tg]])
        op("act", lambda e: e.activation(out=t2[:], in_=Lc[:], func=AF.Exp, scale=-1.0), r=Lc.k, w=t2.k)
        op("dve", lambda e: e.tensor_tensor(out=bt[:, ts_], in0=t3[:], in1=t2[:], op=ALU.mult), r=t3.k + t2.k, w=[bt.k[tg]])
        op("dve", lambda e: e.tensor_tensor(out=kt[:, ts_], in0=kf[:], in1=t2[:], op=ALU.mult), r=kf.k + t2.k, w=[kt.k[tg]])
        for q in range(4):
            cs = slice(q * 128, (q + 1) * 128)
            op("act", lambda e, cs=cs, q=q: e.activation(out=t4[:, cs], in_=Lc[:, cs], func=AF.Exp, scale=-1.0, bias=Lc[:, q * 128 + 127:q * 128 + 128]),
               r=Lc.k, w=t4.k)
        op("dve", lambda e: e.tensor_tensor(out=bh[:, ts_], in0=t3[:], in1=t4[:], op=ALU.mult), r=t3.k + t4.k, w=[bh.k[tg]])
        op("dve", lambda e: e.tensor_tensor(out=kh[:, ts_], in0=kf[:], in1=t4[:], op=ALU.mult), r=kf.k + t4.k, w=[kh.k[tg]])
        op("act", lambda e: e.activation(out=WC[:, 4 * tg:4 * tg + 4], in_=Lc[:, 127::128], func=AF.Exp), r=Lc.k, w=[WC.k[tg]])
        if tg == 3:
            dma("sp", prod_d[hp], prodb[:], r=prodb.k, w=[prod_d.k[hp]])

    pending = []

    def queue_prep(hp_, tg_):
        defer["lst"] = []
        prep_unit(hp_, tg_)
        lst = defer["lst"]
        defer["lst"] = None
        pending.extend((hp_, tg_, t) for t in lst)

    def pop_prep(n):
        for _ in range(n):
            if pending:
                pending.pop(0)[2]()

    def flush_prep(hp_, tg_):
        while pending and (pending[0][0], pending[0][1]) <= (hp_, tg_):
            pending.pop(0)[2]()

    for tg in range(4):
        prep_unit(0, tg)
    for hp in range(4):
        op("dve", lambda e: e.memset(Sf[:], 0.0), w=Sf.k)
        op("dve", lambda e: e.memset(Sbf[:], 0.0), w=Sbf.k)
        def pre_units(b2):
            par = b2 % 2
            n1 = N1s[par]; n2 = N2s[par]; ti_ = Tinv[par]; zb = ZB[par]; zk = ZK[par]
            st = {}
            units = []

            def u_nmat():
                mu2b = mu2[:].unsqueeze(1).to_broadcast([128, 2, 256])
                tg_ = b2 // 2
                for rnd in range(3):
                    for idx in range(4):
                        hd, ti = idx // 2, idx % 2
                        tt = 2 * b2 + ti
                        po = hd * 64
                        tcs = slice(tt * 128, (tt + 1) * 128)
                        pb_ = psF[3 + hd]
                        if rnd == 0:
                            op("pe", lambda e: e.matmul(pb_[:, ti * 256:ti * 256 + 256], lhsT=bt[po:po + 64, tcs], rhs=ARt[po:po + 64, tt, :], start=True, stop=True),
                               r=[bt.k[tg_], ARt.k[tg_]], w=pb_.k)
                        elif rnd == 1:
                            op("pe", lambda e: e.matmul(pb_[:, ti * 256:ti * 256 + 256], lhsT=kt[po:po + 64, tcs], rhs=ARt[po:po + 64, tt, :], start=True, stop=True),
                               r=[kt.k[tg_], ARt.k[tg_]], w=pb_.k)
                        else:
                            op("pe", lambda e: e.matmul(pb_[:, ti * 128:(ti + 1) * 128], lhsT=ARt[po:po + 64, tt, 0:128], rhs=bt[po:po + 64, tcs],
                                                        start=True, stop=True), r=[bt.k[tg_], ARt.k[tg_]], w=pb_.k)
                    for b_ in range(2):
                        pb_ = psF[3 + b_]
                        if rnd == 0:
                            op("dve", lambda e: e.tensor_tensor(out=n1[:, 2 * b_:2 * b_ + 2, :], in0=pb_[:].rearrange("p (a b) -> p a b", a=2),
                                                                in1=mu2b, op=ALU.mult), r=pb_.k + mu2.k, w=n1.k)
                        elif rnd == 1:
                            op("dve", lambda e: e.tensor_tensor(out=n2[:, 2 * b_:2 * b_ + 2, :], in0=pb_[:].rearrange("p (a b) -> p a b", a=2),
                                                                in1=mu2b, op=ALU.mult), r=pb_.k + mu2.k, w=n2.k)
                        else:
                            op("dve", lambda e: e.tensor_tensor(out=N3s[:, 2 * b_:2 * b_ + 2, :], in0=pb_[:, 0:256].rearrange("p (a b) -> p a b", a=2),
                                                                in1=mls[:].unsqueeze(1).to_broadcast([128, 2, 128]), op=ALU.mult), r=pb_.k + mls.k, w=N3s.k)
                op("dve", lambda e: e.tensor_tensor(out=Pb[0][:], in0=n1[:, :, 0:128], in1=identb[:].unsqueeze(1).to_broadcast([128, 4, 128]), op=ALU.add),
                   r=n1.k + identb.k, w=Pb[0].k)
                st["A"] = (lambda idx: n1[:, idx, 0:128]); st["AT"] = (lambda idx: N3s[:, idx, :])
                st["Ak"] = n1.k; st["ATk"] = N3s.k; st["P"] = Pb[0]
            units.append(u_nmat)

            def mk_level(lev):
                def u():
                    pA, pAT, pP = psF[0], psF[1], psF[2]
                    An = Ab[lev % 2]; ATn = ATb[lev % 2]
                    Pn = Pb[lev % 2] if lev < 6 else ti_
                    A_cur, AT_cur, A_k, AT_k, P_cur = st["A"], st["AT"], st["Ak"], st["ATk"], st["P"]
                    for idx in range(4):
                        cs = slice(idx * 128, (idx + 1) * 128)
                        if lev < 6:
                            op("pe", lambda e: e.matmul(pA[:, cs], lhsT=AT_cur(idx), rhs=A_cur(idx), start=True, stop=True), r=A_k + AT_k, w=pA.k)
                        op("pe", lambda e: e.matmul(pAT[:, cs], lhsT=A_cur(idx), rhs=AT_cur(idx), start=True, stop=True), r=A_k + AT_k, w=pAT.k)
                    if lev < 6:
                        op("act", lambda e: e.copy(out=An[:], in_=pA[:].rearrange("p (a b) -> p a b", a=4)), r=pA.k, w=An.k)
                    op("dve", lambda e: e.tensor_copy(out=ATn[:], in_=pAT[:].rearrange("p (a b) -> p a b", a=4)), r=pAT.k, w=ATn.k)
                    for idx in range(4):
                        cs = slice(idx * 128, (idx + 1) * 128)
                        op("pe", lambda e: e.matmul(pP[:, cs], lhsT=ATn[:, idx, :], rhs=P_cur[:, idx, :], start=True, stop=False), r=ATn.k + P_cur.k, w=pP.k)
                        op("pe", lambda e: e.matmul(pP[:, cs], lhsT=identb[:], rhs=P_cur[:, idx, :], start=False, stop=True), r=identb.k + P_cur.k, w=pP.k)
                    op("act", lambda e: e.copy(out=Pn[:], in_=pP[:].rearrange("p (a b) -> p a b", a=4)), r=pP.k, w=Pn.k)
                    st["A"] = (lambda idx: An[:, idx, :]); st["AT"] = (lambda idx: ATn[:, idx, :])
                    st["Ak"] = An.k; st["ATk"] = ATn.k; st["P"] = Pn
                return u
            for lev in range(1, 7):
                units.append(mk_level(lev))

            def u_tr():
                pb = psB[0]
                for ti in range(2):
                    tt = 2 * b2 + ti
                    tcs = slice(tt * 128, (tt + 1) * 128)
                    op("pe", lambda e: e.transpose(out=pb[:, 2 * ti, :], in_=bh[:, tcs], identity=identb[:]), r=[bh.k[b2 // 2]] + identb.k, w=pb.k)
                    op("pe", lambda e: e.transpose(out=pb[:, 2 * ti + 1, :], in_=kh[:, tcs], identity=identb[:]), r=[kh.k[b2 // 2]] + identb.k, w=pb.k)
                for ti in range(2):
                    for hd in range(2):
                        hs = slice(hd * 64, hd * 64 + 64)
                        op("act", lambda e: e.copy(out=zb[:, ti, hd, hs], in_=pb[:, 2 * ti, hs]), r=pb.k, w=zb.k)
                        op("act", lambda e: e.copy(out=zk[:, ti, hd, hs], in_=pb[:, 2 * ti + 1, hs]), r=pb.k, w=zk.k)
            units.append(u_tr)
            return units

        def seq_units(b2):
            par = b2 % 2
            n1 = N1s[par]; n2 = N2s[par]; ti_ = Tinv[par]; zb = ZB[par]; zk = ZK[par]
            pq = psF[7]
            units = []
            for ti in range(2):
                tt = 2 * b2 + ti
                v_ = Vt[tt % 4]

                def hv(hd, v_=v_):
                    return v_[:, (2 * hp + hd) * 64:(2 * hp + hd) * 64 + 64]

                def u_x(ti=ti, tt=tt, v_=v_, hv=hv):
                    dma("sp", v_[:], vr_d[tt], r=[vr_d.k[tt]], w=v_.k)
                    for hd in range(2):
                        idx = hd * 2 + ti
                        op("pe", lambda e: e.matmul(pq[:, hd * 64:hd * 64 + 64], lhsT=n2[:, idx, 0:128], rhs=hv(hd), start=True, stop=False),
                           r=n2.k + v_.k, w=pq.k)
                        op("pe", lambda e: e.matmul(pq[:, hd * 64:hd * 64 + 64], lhsT=ARt[:, tt, 0:128], rhs=Sbf[:, hd, :], start=False, stop=True),
                           r=[ARt.k[b2 // 2]] + Sbf.k, w=pq.k)
                    op("act", lambda e: e.copy(out=Xs[:], in_=pq[:, 0:128]), r=pq.k, w=Xs.k)

                def u_u(ti=ti, tt=tt):
                    for hd in range(2):
                        idx = hd * 2 + ti
                        op("pe", lambda e: e.matmul(pq[:, 128 + hd * 64:128 + hd * 64 + 64], lhsT=ti_[:, idx, :], rhs=Xs[:, hd * 64:hd * 64 + 64], start=True, stop=True),
                           r=ti_.k + Xs.k, w=pq.k)
                    op("dve", lambda e: e.tensor_copy(out=Us[:], in_=pq[:, 128:256]), r=pq.k, w=Us.k)

                def u_y(ti=ti, tt=tt, v_=v_, hv=hv):
                    for hd in range(2):
                        idx = hd * 2 + ti
                        yc = slice(256 + hd * 64, 256 + hd * 64 + 64)
                        op("pe", lambda e: e.matmul(pq[:, yc], lhsT=ARt[:, tt, 128:256], rhs=Sbf[:, hd, :], start=True, stop=False),
                           r=[ARt.k[b2 // 2]] + Sbf.k, w=pq.k)
                        op("pe", lambda e: e.matmul(pq[:, yc], lhsT=n1[:, idx, 128:256], rhs=Us[:, hd * 64:hd * 64 + 64], start=False, stop=False),
                           r=n1.k + Us.k, w=pq.k)
                        op("pe", lambda e: e.matmul(pq[:, yc], lhsT=n2[:, idx, 128:256], rhs=hv(hd), start=False, stop=True), r=n2.k + v_.k, w=pq.k)
                    for hd in range(2):
                        op("pe", lambda e: e.matmul(pq[:, 384:448], lhsT=zb[:, ti, hd, :], rhs=Us[:, hd * 64:hd * 64 + 64], start=(hd == 0), stop=False),
                           r=zb.k + Us.k, w=pq.k)
                        op("pe", lambda e: e.matmul(pq[:, 384:448], lhsT=zk[:, ti, hd, :], rhs=hv(hd), start=False, stop=(hd == 1)), r=zk.k + v_.k, w=pq.k)

                def u_s(ti=ti, tt=tt):
                    op("dve", lambda e: e.scalar_tensor_tensor(out=Sf[:], in0=Sf[:], scalar=WC[:, tt:tt + 1], in1=pq[:, 384:448], op0=ALU.mult, op1=ALU.add),
                       r=Sf.k + [WC.k[b2 // 2]] + pq.k, w=Sf.k)
                    op("act", lambda e: e.copy(out=Sbf[0:64, 0, :], in_=Sf[0:64, :]), r=Sf.k, w=Sbf.k)
                    op("act", lambda e: e.copy(out=Sbf[64:128, 1, :], in_=Sf[64:128, :]), r=Sf.k, w=Sbf.k)
                    ys = ystg[tt % 2]
                    op("act", lambda e: e.copy(out=ys[:], in_=pq[:, 256:384]), r=pq.k, w=ys.k)
                    dma("sp", y_d[tt][:, hp * 128:(hp + 1) * 128], ys[:], r=ys.k, w=[y_d.k[tt]])
                units += [u_x, u_u, u_y, u_s]
            return units

        flush_prep(hp, 0)
        for u in pre_units(0):
            u()
        for b2 in range(8):
            sq = seq_units(b2)
            if b2 + 1 < 8:
                flush_prep(hp, (b2 + 1) // 2)
                pr = pre_units(b2 + 1)
            else:
                pr = []
            for k in range(8):
                if k < len(pr):
                    pr[k]()
                sq[k]()
                pop_prep(int(os.environ.get("POPN", "3")))
            if hp + 1 < 4 and b2 % 2 == 1:
                queue_prep(hp + 1, b2 // 2)
    flush_prep(9, 9)
    for _ in range(int(os.environ.get("EXTRA", "0"))):
        op("pe", lambda e: e.matmul(psF[0][:, 0:128], lhsT=identb[:], rhs=identb[:], start=True, stop=True), r=identb.k, w=psF[0].k)
    C.barrier()


def _outproj(C, l):
    sc = C.sc; op = sc.op; dma = sc.dma
    psF = C.psF; psB = C.psB; identb = C.identb
    Wl = C.W[l]
    att = C.att; sgd = C.sgd; sgd2 = C.sgd2; g2b = C.g2b; g2b2 = C.g2b2; lnw_bc = C.lnw_bc; lnb_bc = C.lnb_bc
    y_d = C.y_d; vr_d = C.vr_d; xs_d = C.xs_d; prod_d = C.prod_d; headsel = C.headsel
    G1 = C.G1
    L4 = C.Lay()
    yt = [L4.get([128, RW], F32) for _ in range(2)]
    vt = [L4.get([128, RW], BF16) for _ in range(2)]
    pr = [L4.get([128, 4, 128], BF16) for _ in range(2)]
    sq = L4.get([128, RW], F32); yc = L4.get([128, RW], F32); bo = L4.get([128, RW], F32)
    st = L4.get([128, 8, 8], F32)
    rwo = [L4.get([128, RW], BF16) for _ in range(2)]
    catT = [L4.get([128, 8, 128], BF16) for _ in range(2)]
    xt = [L4.get([128, D], F32) for _ in range(2)]
    xn = [L4.get([128, D], F32) for _ in range(2)]
    assert L4.off <= C.TOP_P2
    wo = [C.nextw(), C.nextw()]
    for nh in range(2):
        dma("pool", wo[nh][:], Wl["w_out"][:, nh * 512:(nh + 1) * 512].rearrange("(kc p) n -> p kc n", p=128), w=wo[nh].k)
    def unit_a(tt):
        y_ = yt[tt % 2]; v_ = vt[tt % 2]; p_ = pr[tt % 2]
        dma("sp", y_[:], y_d[tt], r=[y_d.k[tt]], w=y_.k)
        dma("sp", v_[:], vr_d[tt], r=[vr_d.k[tt]], w=v_.k)
        dma("sp", p_[:], prod_d.t[:, :, tt * 128:(tt + 1) * 128].rearrange("a p t -> p a t"), r=prod_d.k, w=p_.k)
        y3 = y_[:].rearrange("p (h d) -> p h d", h=8)
        S1, S2, MEAN, MSQ, VAR, RSTD, RK = [st[:, i, :] for i in range(7)]
        op("dve", lambda e: e.tensor_reduce(out=S1, in_=y3, axis=AX.X, op=ALU.add), r=y_.k, w=st.k)
        op("pool", lambda e: e.tensor_tensor(out=sq[:], in0=y_[:], in1=y_[:], op=ALU.mult), r=y_.k, w=sq.k)
        op("dve", lambda e: e.tensor_reduce(out=S2, in_=sq[:].rearrange("p (h d) -> p h d", h=8), axis=AX.X, op=ALU.add), r=sq.k + st.k, w=st.k)
        op("dve", lambda e: e.tensor_scalar(out=MEAN, in0=S1, scalar1=1.0 / 64, scalar2=None, op0=ALU.mult), r=st.k, w=st.k)
        op("dve", lambda e: e.tensor_tensor(out=MSQ, in0=MEAN, in1=MEAN, op=ALU.mult), r=st.k, w=st.k)
        op("dve", lambda e: e.scalar_tensor_tensor(out=VAR, in0=S2, scalar=1.0 / 64, in1=MSQ, op0=ALU.mult, op1=ALU.subtract), r=st.k, w=st.k)
        op("act", lambda e: e.activation(out=RSTD, in_=VAR, func=AF.Sqrt, bias=GN_EPS, scale=1.0), r=st.k, w=st.k)
        op("dve", lambda e: e.reciprocal(out=RSTD, in_=RSTD), r=st.k, w=st.k)
        yc3 = yc[:].rearrange("p (h d) -> p h d", h=8)
        op("dve", lambda e: e.tensor_tensor(out=yc3, in0=y3, in1=MEAN.unsqueeze(2).to_broadcast([128, 8, 64]), op=ALU.subtract), r=y_.k + st.k, w=yc.k)
        op("dve", lambda e: e.tensor_tensor(out=yc3, in0=yc3, in1=RSTD.unsqueeze(2).to_broadcast([128, 8, 64]), op=ALU.mult), r=yc.k + st.k, w=yc.k)
        op("pool", lambda e: e.tensor_tensor(out=yc[:], in0=yc[:], in1=lnw_bc[:], op=ALU.mult), r=yc.k + lnw_bc.k, w=yc.k)
        op("pool", lambda e: e.tensor_tensor(out=yc[:], in0=yc[:], in1=lnb_bc[:], op=ALU.add), r=yc.k + lnb_bc.k, w=yc.k)
        prk = psF[0]
        for hp in range(4):
            op("pe", lambda e: e.matmul(prk[:, 2 * hp:2 * hp + 2], lhsT=p_[:, hp, :], rhs=headsel[:], start=True, stop=True), r=p_.k + headsel.k, w=prk.k)
        op("act", lambda e: e.copy(out=RK, in_=prk[:, 0:8]), r=prk.k, w=st.k)
        op("dve", lambda e: e.tensor_tensor(out=bo[:].rearrange("p (h d) -> p h d", h=8), in0=v_[:].rearrange("p (h d) -> p h d", h=8),
                                            in1=RK.unsqueeze(2).to_broadcast([128, 8, 64]), op=ALU.mult), r=v_.k + st.k, w=bo.k)
        op("pool", lambda e: e.tensor_tensor(out=yc[:], in0=yc[:], in1=bo[:], op=ALU.add), r=yc.k + bo.k, w=yc.k)
        pg = psF[1]
        tcs = slice(tt * 128, (tt + 1) * 128)
        op("pe", lambda e: e.matmul(pg[:], lhsT=sgd[:, tcs], rhs=g2b[:, :], start=True, stop=False), r=sgd.k + g2b.k, w=pg.k)
        op("pe", lambda e: e.matmul(pg[:], lhsT=sgd2[0:32, tcs], rhs=g2b2[0:32, :], start=False, stop=True), r=sgd2.k + g2b2.k, w=pg.k)
        ro = rwo[tt % 2]
        op("dve", lambda e: e.tensor_tensor(out=ro[:], in0=pg[:], in1=yc[:], op=ALU.mult), r=pg.k + yc.k, w=ro.k)

    def unit_b(tt):
        ro = rwo[tt % 2]
        pb = psB[tt % 2]
        for kc in range(8):
            src = att[:, tt, kc * 128:(kc + 1) * 128] if kc < 4 else ro[:, (kc - 4) * 128:(kc - 3) * 128]
            op("pe", lambda e: e.transpose(out=pb[:, kc, :], in_=src, identity=identb[:]), r=[att.k[tt]] + ro.k + identb.k, w=pb.k)
        cT = catT[tt % 2]
        op("act", lambda e: e.copy(out=cT[:], in_=pb[:]), r=pb.k, w=cT.k)
        x_ = xt[tt % 2]; xo = xn[tt % 2]
        if l == C.layers[0] and C.first_from_input:
            dma("sp", x_[:], C.x_in[tt * 128:(tt + 1) * 128, :], w=x_.k)
        else:
            dma("sp", x_[:], xs_d[tt], r=[xs_d.k[tt]], w=x_.k)
        for nh in range(2):
            po_ = psF[2 + nh]
            for kc in range(8):
                op("pe", lambda e: e.matmul(po_[:], lhsT=cT[:, kc, :], rhs=wo[nh][:, kc, :], start=(kc == 0), stop=(kc == 7)), r=cT.k + wo[nh].k, w=po_.k)
            ns = slice(nh * 512, (nh + 1) * 512)
            op("dve", lambda e: e.tensor_tensor(out=xo[:, ns], in0=po_[:], in1=G1[:, ns], op=ALU.mult), r=po_.k + C.modbc.k, w=xo.k)
        op("dve", lambda e: e.tensor_tensor(out=xo[:], in0=xo[:], in1=x_[:], op=ALU.add), r=xo.k + x_.k, w=xo.k)
        dma("sp", xs_d[tt], xo[:], r=xo.k, w=[xs_d.k[tt]])
        if l in C.tapx:
            dma("sp", C.tapx[l][tt * 128:(tt + 1) * 128, :], xo[:], r=xo.k)
    unit_a(0)
    for tt in range(NT):
        if tt + 1 < NT:
            unit_a(tt + 1)
        unit_b(tt)
    C.barrier()


def _ffn(C, l):
    sc = C.sc; op = sc.op; dma = sc.dma
    psF = C.psF
    Wl = C.W[l]
    xs_d = C.xs_d
    moe = (l % 2 == 1)
    _adaln(C, l, 1)
    L5 = C.Lay()
    hT = L5.get([128, 8, S + 2], BF16, 4)
    gates = L5.get([128, NT, 8], F32, NT)
    base = L5.off
    if moe:
        rwbc = L5.get([128, 8, D], F32)
        rbbc = L5.get([128, 8], F32)
        lgt = L5.get([128, 16], F32)
        jk = L5.get([128, D], F32)
        for e_ in range(NE):
            dma("sp", rwbc[:, e_, :], Wl["rw"][e_].partition_broadcast(128), w=rwbc.k)
        dma("sp", rbbc[:], Wl["rb"].partition_broadcast(128), w=rbbc.k)

        def router(tt, hf):
            LG = lgt[:, 0:8]
            for e_ in range(NE):
                op("dve", lambda e: e.scalar_tensor_tensor(out=jk[:], in0=hf[:], scalar=1.0, in1=rwbc[:, e_, :], op0=ALU.mult, op1=ALU.mult,
                                                           accum_out=lgt[:, e_:e_ + 1]), r=hf.k + rwbc.k, w=jk.k + lgt.k)
            M1, NM1, M2, E2 = [lgt[:, 8 + i:9 + i] for i in range(4)]
            EQ = jk[:, 0:8]; L2_ = jk[:, 8:16]; EX = jk[:, 16:24]; SEL = jk[:, 24:32]
            op("dve", lambda e: e.tensor_tensor(out=LG, in0=LG, in1=rbbc[:], op=ALU.add), r=lgt.k + rbbc.k, w=lgt.k)
            op("dve", lambda e: e.tensor_reduce(out=M1, in_=LG, axis=AX.X, op=ALU.max), r=lgt.k, w=lgt.k)
            op("dve", lambda e: e.tensor_scalar(out=EQ, in0=LG, scalar1=M1, scalar2=None, op0=ALU.is_equal), r=lgt.k, w=jk.k)
            op("dve", lambda e: e.scalar_tensor_tensor(out=L2_, in0=EQ, scalar=-1e30, in1=LG, op0=ALU.mult, op1=ALU.add), r=jk.k + lgt.k, w=jk.k)
            op("dve", lambda e: e.tensor_reduce(out=M2, in_=L2_, axis=AX.X, op=ALU.max), r=jk.k, w=lgt.k)
            op("dve", lambda e: e.tensor_scalar(out=SEL, in0=LG, scalar1=M2, scalar2=None, op0=ALU.is_ge), r=lgt.k, w=jk.k)
            op("dve", lambda e: e.tensor_scalar(out=NM1, in0=M1, scalar1=-1.0, scalar2=None, op0=ALU.mult), r=lgt.k, w=lgt.k)
            op("act", lambda e: e.activation(out=EX, in_=LG, func=AF.Exp, bias=NM1, scale=1.0), r=lgt.k, w=jk.k)
            op("act", lambda e: e.activation(out=E2, in_=M2, func=AF.Exp, bias=NM1, scale=1.0), r=lgt.k, w=lgt.k)
            op("dve", lambda e: e.tensor_scalar(out=E2, in0=E2, scalar1=1.0, scalar2=None, op0=ALU.add), r=lgt.k, w=lgt.k)
            op("dve", lambda e: e.reciprocal(out=E2, in_=E2), r=lgt.k, w=lgt.k)
            op("dve", lambda e: e.scalar_tensor_tensor(out=gates[:, tt, :], in0=EX, scalar=E2, in1=SEL, op0=ALU.mult, op1=ALU.mult),
               r=jk.k + lgt.k, w=[gates.k[tt]])
    else:
        router = None
    norm_off = L5.off
    L5.off = base
    acc = L5.get([128, NT, D], F32, NT)
    actT = [L5.get([128, 2, S], BF16) for _ in range(2)]
    sgt = [L5.get([128, 512], F32) for _ in range(2)]
    xt = [L5.get([128, D], F32) for _ in range(2)]
    L5end = L5.off
    L5.off = norm_off
    nE = NE if moe else 1
    F_ = DFE if moe else DFF
    nfg = F_ // 256
    its = [(e_, fg) for e_ in range(nE) for fg in range(nfg)]
    sets = [(C.wbufs[2 * i], C.wbufs[2 * i + 1]) for i in range(3)]

    def wdview(wdb):
        return wdb[:, 0:4, :].rearrange("p a b -> p (a b)").rearrange("p (c n) -> p c n", c=2)

    def load(i):
        e_, fg = its[i]
        wgu, wdb = sets[i % 3]
        wg = Wl["wg"][e_] if moe else Wl["wg"]
        wu = Wl["wu"][e_] if moe else Wl["wu"]
        wd = Wl["wd"][e_] if moe else Wl["wd"]
        f0 = fg * 256
        if os.environ.get("NOLOAD") and i > 2:
            return
        dma("pool", wgu[:, :, 0:256], wg[:, f0:f0 + 256].rearrange("(kc p) n -> p kc n", p=128), w=wgu.k)
        dma("pool", wgu[:, :, 256:512], wu[:, f0:f0 + 256].rearrange("(kc p) n -> p kc n", p=128), w=wgu.k)
        dma("pool", wdview(wdb), wd[f0:f0 + 256, :].rearrange("(c p) n -> p c n", p=128), w=wdb.k)

    def G_units(i):
        wgu, wdb = sets[i % 3]
        aT = actT[i % 2]
        units = []
        for tg in range(4):
            for fc in range(2):
                def u(tg=tg, fc=fc):
                    pG = psF[(2 * fc) % 4]; pU = psF[(2 * fc + 1) % 4]
                    for kc in range(8):
                        op("pe", lambda e: e.matmul(pG[:], lhsT=wgu[:, kc, fc * 128:(fc + 1) * 128], rhs=hT[:, kc, 1 + tg * 512:1 + (tg + 1) * 512],
                                                    start=(kc == 0), stop=(kc == 7)), r=wgu.k + [hT.k[tg]], w=pG.k)
                    for kc in range(8):
                        op("pe", lambda e: e.matmul(pU[:], lhsT=wgu[:, kc, 256 + fc * 128:256 + (fc + 1) * 128], rhs=hT[:, kc, 1 + tg * 512:1 + (tg + 1) * 512],
                                                    start=(kc == 0), stop=(kc == 7)), r=wgu.k + [hT.k[tg]], w=pU.k)
                    s_ = sgt[fc]
                    op("act", lambda e: e.activation(out=s_[:], in_=pG[:], func=AF.Silu), r=pG.k, w=s_.k)
                    op("dve", lambda e: e.tensor_tensor(out=aT[:, fc, tg * 512:(tg + 1) * 512], in0=pU[:], in1=s_[:], op=ALU.mult), r=pU.k + s_.k, w=aT.k)
                units.append(u)
        return units

    def D_units(i):
        e_, fg = its[i]
        wgu, wdb = sets[i % 3]
        wdv = wdview(wdb)
        aT = actT[i % 2]
        units = []
        for tt in range(NT):
            def u(tt=tt):
                for nh in range(2):
                    pD = psF[4 + (2 * tt + nh) % 4]
                    for fc in range(2):
                        op("pe", lambda e: e.matmul(pD[:], lhsT=aT[:, fc, tt * 128:(tt + 1) * 128], rhs=wdv[:, fc, nh * 512:(nh + 1) * 512],
                                                    start=(fc == 0), stop=(fc == 1)), r=aT.k + wdb.k, w=pD.k)
                    a_ = acc[:, tt, nh * 512:(nh + 1) * 512]
                    gsc = gates[:, tt, e_:e_ + 1] if moe else 1.0
                    rk = ([gates.k[tt]] if moe else [])
                    if i == 0:
                        op("dve", lambda e: e.tensor_scalar(out=a_, in0=pD[:], scalar1=gsc, scalar2=None, op0=ALU.mult), r=pD.k + rk, w=[acc.k[tt]])
                    else:
                        op("dve", lambda e: e.scalar_tensor_tensor(out=a_, in0=pD[:], scalar=gsc, in1=a_, op0=ALU.mult, op1=ALU.add),
                           r=pD.k + rk + [acc.k[tt]], w=[acc.k[tt]])
            units.append(u)
        return units

    n_it = len(its)
    load(0)
    if n_it > 1:
        load(1)
    g0 = G_units(0)

    def hook(tt):
        if tt % 4 == 3:
            tg = tt // 4
            g0[2 * tg]()
            g0[2 * tg + 1]()
    C.norm_phase(L5, lambda tt: (xs_d[tt], xs_d.k[tt]), C.A2, C.SH2, hT, router, hook)
    assert L5.off <= base + NT * D * 4, (L5.off, base)
    C.barrier()
    for i in range(n_it):
        if i + 2 < n_it:
            load(i + 2)
        d = D_units(i)
        g = G_units(i + 1) if i + 1 < n_it else []
        for k in range(8):
            if k < len(g):
                g[k]()
            d[2 * k]()
            d[2 * k + 1]()
    for tt in range(NT):
        x_ = xt[tt % 2]
        dma("sp", x_[:], xs_d[tt], r=[xs_d.k[tt]], w=x_.k)
        op("dve", lambda e: e.tensor_tensor(out=acc[:, tt, :], in0=acc[:, tt, :], in1=C.G2, op=ALU.mult), r=[acc.k[tt]] + C.modbc.k, w=[acc.k[tt]])
        op("dve", lambda e: e.tensor_tensor(out=x_[:], in0=x_[:], in1=acc[:, tt, :], op=ALU.add), r=x_.k + [acc.k[tt]], w=x_.k)
        dma("sp", xs_d[tt], x_[:], r=x_.k, w=[xs_d.k[tt]])
        if l in C.tapo:
            dma("sp", C.tapo[l][tt * 128:(tt + 1) * 128, :], x_[:], r=x_.k)
    C.barrier()


def _final(C):
    sc = C.sc; op = sc.op; dma = sc.dma
    xs_d = C.xs_d
    Lf = C.Lay()
    fg = Lf.get([128, D], F32)
    xt = [Lf.get([128, D], F32) for _ in range(2)]
    xo = [Lf.get([128, D], F32) for _ in range(2)]
    junk = Lf.get([128, D], BF16)
    ss = Lf.get([128, NT, 4], F32, NT)
    dma("sp", fg[:], C.final_g_in.partition_broadcast(128), w=fg.k)
    for tt in range(NT):
        x_ = xt[tt % 2]; o_ = xo[tt % 2]
        dma("sp", x_[:], xs_d[tt], r=[xs_d.k[tt]], w=x_.k)
        op("act", lambda e: e.activation(out=junk[:], in_=x_[:], func=AF.Square, accum_out=ss[:, tt, 0:1]), r=x_.k, w=junk.k + [ss.k[tt]])
        op("act", lambda e: e.activation(out=ss[:, tt, 1:2], in_=ss[:, tt, 0:1], func=AF.Sqrt, bias=RMS_EPS, scale=1.0 / D), r=[ss.k[tt]], w=[ss.k[tt]])
        op("dve", lambda e: e.reciprocal(out=ss[:, tt, 2:3], in_=ss[:, tt, 1:2]), r=[ss.k[tt]], w=[ss.k[tt]])
        op("dve", lambda e: e.scalar_tensor_tensor(out=o_[:], in0=x_[:], scalar=ss[:, tt, 2:3], in1=fg[:], op0=ALU.mult, op1=ALU.mult),
           r=x_.k + [ss.k[tt]] + fg.k, w=o_.k)
        dma("sp", C.out_d[tt * 128:(tt + 1) * 128, :], o_[:], r=o_.k)


_CONSTS = None


def make_in_maps(inputs, layers=(0, 1, 2, 3), cores=range(8)):
    global _CONSTS
    if _CONSTS is None:
        _CONSTS = host_consts()
    cst = _CONSTS
    f = lambda a: np.ascontiguousarray(np.asarray(a, dtype=np.float32))
    rel = np.asarray(inputs["rel_bias"], np.float32)
    abias = np.ascontiguousarray(rel[cst["_bucket"]].transpose(0, 2, 1))
    shared = {"k_" + k: f(v) for k, v in cst.items() if not k.startswith("_")}
    shared["abias"] = abias
    shared["final_g"] = f(inputs["final_g"])
    for l in layers:
        i = l // 2
        def put(nm, a):
            shared["%s_%d" % (nm, l)] = f(a)
        put("w_ada", inputs["w_ada"][l]); put("b_ada", inputs["b_ada"][l]); put("norm1_g", inputs["norm1_g"][l]); put("norm2_g", inputs["norm2_g"][l])
        put("w_in", inputs["w_in"][l]); put("w_out", inputs["w_out"][l]); put("mu", inputs["rwkv_mu"][l])
        put("w0", inputs["rwkv_w0"][l]); put("w2", inputs["rwkv_w2"][l]); put("a0", inputs["rwkv_a0"][l]); put("a2", inputs["rwkv_a2"][l])
        put("g2", inputs["rwkv_g2"][l]); put("k_k", inputs["rwkv_k_k"][l]); put("k_a", inputs["rwkv_k_a"][l])
        put("r_k", np.asarray(inputs["rwkv_r_k"][l]).reshape(-1)); put("ln_w", inputs["rwkv_ln_w"][l]); put("ln_b", inputs["rwkv_ln_b"][l])
        if l > 0:
            put("v0", inputs["rwkv_v0"][l - 1]); put("v1", inputs["rwkv_v1"][l - 1]); put("v2", inputs["rwkv_v2"][l - 1])
        if l % 2 == 0:
            put("wg", inputs["ffn_w_gate"][i]); put("wu", inputs["ffn_w_up"][i]); put("wd", inputs["ffn_w_down"][i])
        else:
            put("rw", np.asarray(inputs["moe_router_w"][i]).T); put("rb", inputs["moe_router_b"][i])
            put("wg", inputs["moe_w_gate"][i]); put("wu", inputs["moe_w_up"][i]); put("wd", inputs["moe_w_down"][i])
    maps = []
    for b in cores:
        m = dict(shared)
        m["x"] = f(inputs["x"][b])
        m["c"] = f(inputs["c"][b])
        maps.append(m)
    return maps


def kernel(**inputs):
    nc = bass.Bass("TRN2", target_bir_lowering=False)
    build(nc)
    maps = make_in_maps(inputs)
    res = run_bass_kernel_spmd(nc, maps, core_ids=list(range(8)))
    out = np.stack([np.asarray(r["out"], dtype=np.float32) for r in res.results], axis=0)
    return out
```

```python
import contextlib
import os
import math
import numpy as np
import concourse.bass as bass
import concourse.mybir as mybir
from concourse.bass_utils import run_bass_kernel_spmd

F32 = mybir.dt.float32
BF16 = mybir.dt.bfloat16
AF = mybir.ActivationFunctionType
ALU = mybir.AluOpType
AX = mybir.AxisListType

S = 2048
D = 1024
NT = 16
DEPTH = 4
HD = 64
AW = 512
RW = 512
NIN = 3360
DFF = 2816
DFE = 3584
NE = 8
RMS_EPS = 1e-6
GN_EPS = 64 * 1e-5
N_DSEM = 48


class Tk:
    __slots__ = ("w", "r")

    def __init__(self):
        self.w = None
        self.r = {}


class Sch:
    def __init__(self, nc, es):
        self.nc = nc
        self.es = es
        self.eng = {"pe": nc.tensor, "dve": nc.vector, "act": nc.scalar, "pool": nc.gpsimd, "sp": nc.sync}
        self.sem = {k: es.enter_context(nc.semaphore("s_" + k)) for k in self.eng}
        self.cnt = {k: 0 for k in self.eng}
        self.waited = {k: {} for k in self.eng}
        self.dsem = [es.enter_context(nc.semaphore("d%d" % i)) for i in range(N_DSEM)]
        self.dval = [0] * N_DSEM
        self.dnext = {"sp": 0, "pool": 0, "act": 0}
        self.drange = {"sp": (0, N_DSEM // 2), "act": (0, N_DSEM // 2), "pool": (N_DSEM // 2, N_DSEM)}
        self.nins = 0

    def _semof(self, key):
        if isinstance(key, tuple):
            return self.dsem[key[1]]
        return self.sem[key]

    def _wait(self, e, deps):
        w = self.waited[e]
        for key, c in deps.items():
            if w.get(key, 0) < c:
                self.eng[e].wait_ge(self._semof(key), c)
                w[key] = c
                self.nins += 1

    def _deps(self, e, r, w):
        deps = {}

        def add(key, c):
            if deps.get(key, 0) < c:
                deps[key] = c
        for t in r:
            if t.w is not None:
                add(*t.w)
        for t in w:
            if t.w is not None and not (t.w[0] == e and e == "pe"):
                add(*t.w)
            for key, c in t.r.items():
                add(key, c)
        return deps

    def op(self, e, fn, r=(), w=()):
        self._wait(e, self._deps(e, r, w))
        ins = fn(self.eng[e])
        self.cnt[e] += 1
        c = self.cnt[e]
        ins.then_inc(self.sem[e], 1)
        self.nins += 1
        for t in r:
            t.r[e] = c
        for t in w:
            t.w = (e, c)
            t.r = {}
        return ins

    def dma(self, q, out, in_, r=(), w=(), **kw):
        lo, hi = self.drange[q]
        i = lo + self.dnext[q]
        self.dnext[q] = (self.dnext[q] + 1) % (hi - lo)
        key = ("d", i)
        deps = self._deps(key, r, w)
        if self.dval[i] > 0:
            deps[key] = max(deps.get(key, 0), self.dval[i])
        self._wait(q, deps)
        self.dval[i] += 16
        c = self.dval[i]
        self.eng[q].dma_start(out=out, in_=in_, **kw).then_inc(self.dsem[i], 16)
        self.nins += 1
        for t in r:
            t.r[key] = c
        for t in w:
            t.w = (key, c)
            t.r = {}

    def wait_all(self, e, tks):
        deps = {}
        for t in tks:
            if t.w is not None:
                if deps.get(t.w[0], 0) < t.w[1]:
                    deps[t.w[0]] = t.w[1]
        self._wait(e, deps)


class Buf:
    def __init__(self, t, n=1):
        self.t = t
        self.k = [Tk() for _ in range(n)]

    def __getitem__(self, idx):
        return self.t[idx]


def t5_bucket(n):
    n = np.asarray(n)
    max_exact = 16
    large = max_exact + (np.log(np.maximum(n, 1) / max_exact) / np.log(2048 / max_exact) * 16).astype(np.int32)
    large = np.minimum(large, 31)
    return np.where(n < max_exact, n, large).astype(np.int32)


def host_consts():
    c = {}
    c["ident"] = np.eye(128, dtype=np.float32)
    j = np.arange(128)[:, None]
    t = np.arange(128)[None, :]
    mus = (j < t).astype(np.float32)
    mui = (j <= t).astype(np.float32)
    c["mu2"] = np.concatenate([mus, mui], axis=1)
    c["mls"] = (j > t).astype(np.float32)
    sm = np.ones((128, S), np.float32)
    sm[:, ::128] = 0.0
    c["scanmask"] = sm
    p = np.arange(128)
    c["headsel"] = np.stack([(p < 64), (p >= 64)], axis=1).astype(np.float32)
    c["blockones"] = ((p[:, None] // 64) == (p[None, :] // 64)).astype(np.float32)
    s_ = np.arange(128)[:, None]
    u_ = np.arange(S)[None, :]
    d = u_ - s_
    mult = ((d >= 0) & (d <= 128)).astype(np.float32) + ((d >= 0) & (d % 4 == 0) & (d <= 512)).astype(np.float32) \
        + ((d >= 0) & (d % 16 == 0) & (d <= 2048)).astype(np.float32)
    c["amult"] = mult.astype(np.float32)
    c["_bucket"] = t5_bucket(np.maximum(d, 0))
    return c


class Ctx:
    pass


def build(nc, layers=(0, 1, 2, 3), taps=(), final=True, upto=None):
    es = contextlib.ExitStack()
    C = Ctx()
    C.nc = nc
    C.es = es
    C.taps = {}
    with es:
        _build(C, layers, taps, final, upto)
    return C


def _dram_in(nc, name, shape, dt=F32):
    return nc.dram_tensor(name, list(shape), dt, kind="ExternalInput").ap()


def _build(C, layers, taps, final, upto):
    nc = C.nc
    es = C.es
    sc = Sch(nc, es)
    C.sc = sc

    def sb(name, shape, dt, n=1):
        return Buf(es.enter_context(nc.sbuf_tensor(name, list(shape), dt)), n)

    def ps(name, shape, dt=F32):
        return Buf(es.enter_context(nc.psum_tensor(name, list(shape), dt)), 1)

    def dscr(name, shape, dt=F32, n=1):
        return Buf(nc.dram_tensor(name, list(shape), dt).ap(), n)

    def tap(name, shape):
        if name in taps:
            C.taps[name] = nc.dram_tensor("tap_" + name, list(shape), F32, kind="ExternalOutput").ap()
            return C.taps[name]
        return None

    x_in = _dram_in(nc, "x", [S, D])
    c_in = _dram_in(nc, "c", [D])
    cst = {k: _dram_in(nc, "k_" + k, v.shape) for k, v in host_consts().items() if not k.startswith("_")}
    abias_in = _dram_in(nc, "abias", [128, 8, S])
    final_g_in = _dram_in(nc, "final_g", [D])
    out_d = nc.dram_tensor("out", [S, D], F32, kind="ExternalOutput").ap()
    W = {}
    for l in layers:
        W[l] = {}
        def di(nm, shape):
            W[l][nm] = _dram_in(nc, "%s_%d" % (nm, l), shape)
        di("w_ada", [D, 6 * D]); di("b_ada", [6 * D]); di("norm1_g", [D]); di("norm2_g", [D])
        di("w_in", [D, NIN]); di("w_out", [D, D]); di("mu", [1824])
        di("w0", [RW]); di("w2", [64, RW]); di("a0", [RW]); di("a2", [64, RW]); di("g2", [160, RW])
        di("k_k", [RW]); di("k_a", [RW]); di("r_k", [RW]); di("ln_w", [RW]); di("ln_b", [RW])
        if l > 0:
            di("v0", [RW]); di("v1", [RW, 32]); di("v2", [32, RW])
        if l % 2 == 0:
            di("wg", [D, DFF]); di("wu", [D, DFF]); di("wd", [DFF, D])
        else:
            di("rw", [NE, D]); di("rb", [NE]); di("wg", [NE, D, DFE]); di("wu", [NE, D, DFE]); di("wd", [NE, DFE, D])

    xs_d = dscr("xs_d", [NT, 128, D], F32, NT)
    rT_d = dscr("rT_d", [4, 128, S], F32, 4)
    kT_d = dscr("kT_d", [4, 128, S], F32, 4)
    y_d = dscr("y_d", [NT, 128, RW], F32, NT)
    vf_d = dscr("vf_d", [NT, 128, RW], F32, NT)

    identb = sb("identb", [128, 128], BF16)
    mu2 = sb("mu2", [128, 256], F32)
    mls = sb("mls", [128, 128], F32)
    headsel = sb("headsel", [128, 2], BF16)
    blockones = sb("blockones", [128, 128], BF16)
    onesrow = sb("onesrow", [1, 128], BF16)
    cact = sb("cact", [128, 8], F32)
    cactb = sb("cactb", [128, 8, 128], BF16)
    modbc = sb("modbc", [128, 3 * D], F32)
    pcol = sb("pcol", [128, 8, 4], F32)
    rowb = sb("rowb", [1, 512], BF16)
    wbufs = [sb("wbuf%d" % i, [128, 8, 512], BF16, 2) for i in range(6)]
    C.wnext = 0
    ARENA = 143360
    arena_t = es.enter_context(nc.sbuf_tensor("arena", [128, ARENA // 2], BF16))
    amask_d = dscr("amask_d", [128, 8 * S], BF16)
    vr_d = dscr("vr_d", [NT, 128, RW], BF16, NT)
    prod_d = dscr("prod_d", [4, 128, S], BF16, 4)
    C.layers = list(layers)
    C.first_from_input = True
    C.tapx = {}
    C.tapo = {}
    for nm in taps:
        if nm.startswith("xmid"):
            C.tapx[int(nm[4:])] = nc.dram_tensor("tap_" + nm, [S, D], F32, kind="ExternalOutput").ap()
        if nm.startswith("xout"):
            C.tapo[int(nm[4:])] = nc.dram_tensor("tap_" + nm, [S, D], F32, kind="ExternalOutput").ap()
    psF = [ps("psF%d" % i, [128, 512], F32) for i in range(8)]
    psB = []
    for i in range(2):
        b_ = Buf(psF[6 + i].t[:, :].bitcast(BF16).rearrange("p (a b) -> p a b", a=8), 1)
        b_.k = psF[6 + i].k
        psB.append(b_)
    C.pf = 0

    def carve(off, shape, dt):
        n = 1
        for d_ in shape[1:]:
            n *= d_
        esz = 4 if dt == F32 else 2
        assert off % 4 == 0 and off + n * esz <= ARENA, (off, shape)
        ap = arena_t[0:shape[0], off // 2: off // 2 + n * esz // 2]
        if dt == F32:
            ap = ap.bitcast(F32)
        if len(shape) == 3:
            ap = ap.rearrange("p (a b) -> p a b", a=shape[1])
        elif len(shape) == 4:
            ap = ap.rearrange("p (a b c) -> p a b c", a=shape[1], b=shape[2])
        return Buf(ap, 1)

    class Lay:
        def __init__(self):
            self.off = 0

        def get(self, shape, dt, n=1):
            nb = (4 if dt == F32 else 2)
            for d_ in shape[1:]:
                nb *= d_
            nb = (nb + 31) // 32 * 32
            b_ = carve(self.off, shape, dt)
            b_.k = [Tk() for _ in range(n)]
            self.off += nb
            return b_

    def barrier():
        engs = list(sc.eng.keys())
        for e in engs:
            deps = {}
            for o in engs:
                if o != e and sc.cnt[o] > 0:
                    deps[o] = sc.cnt[o]
            for i in range(N_DSEM):
                if sc.dval[i] > 0:
                    deps[("d", i)] = sc.dval[i]
            sc._wait(e, deps)

    def nextw():
        b = wbufs[C.wnext]
        C.wnext = (C.wnext + 1) % len(wbufs)
        return b

    def nextps(lo=0, hi=6):
        C.pf = (C.pf + 1) % (hi - lo)
        return psF[lo + C.pf]

    op = sc.op
    dma = sc.dma

    dma("pool", identb[:], cst["ident"], w=identb.k)
    dma("sp", mu2[:], cst["mu2"], w=mu2.k)
    dma("sp", mls[:], cst["mls"], w=mls.k)
    dma("pool", headsel[:], cst["headsel"], w=headsel.k)
    dma("pool", blockones[:], cst["blockones"], w=blockones.k)
    op("dve", lambda e: e.memset(onesrow[:], 1.0), w=onesrow.k)
    L0 = Lay()
    amt = L0.get([128, S], F32)
    amm = L0.get([128, S], F32)
    amo = L0.get([128, S], BF16)
    dma("sp", amm[:], cst["amult"], w=amm.k)
    for h in range(8):
        dma("sp", amt[:], abias_in[:, h, :], w=amt.k)
        op("act", lambda e: e.activation(out=amt[:], in_=amt[:], func=AF.Exp), r=amt.k, w=amt.k)
        op("dve", lambda e: e.tensor_tensor(out=amo[:], in0=amt[:], in1=amm[:], op=ALU.mult), r=amt.k + amm.k, w=amo.k)
        dma("sp", amask_d[:, h * S:(h + 1) * S], amo[:], r=amo.k, w=amask_d.k)
    dma("sp", cact[:], c_in.rearrange("(c p) -> p c", p=128), w=cact.k, allow_slow_non_contiguous=True)
    op("act", lambda e: e.activation(out=cact[:], in_=cact[:], func=AF.Silu), r=cact.k, w=cact.k)
    op("dve", lambda e: e.tensor_copy(out=cactb[:], in_=cact[:].unsqueeze(2).to_broadcast([128, 8, 128])), r=cact.k, w=cactb.k)

    barrier()
    C.__dict__.update(locals())
    for li, l in enumerate(layers):
        _layer(C, l, first=(li == 0))
        if upto is not None and l == upto[0]:
            break
    if final:
        _final(C)
    for i in range(N_DSEM):
        if sc.dval[i]:
            nc.sync.wait_ge(sc.dsem[i], sc.dval[i])


def _layer(C, l, first):
    nc = C.nc; sc = C.sc; op = sc.op; dma = sc.dma
    Wl = C.W[l]
    psF = C.psF; psB = C.psB
    identb = C.identb; modbc = C.modbc; cactb = C.cactb; onesrow = C.onesrow; rowb = C.rowb
    nextw = C.nextw; Lay = C.Lay; carve = C.carve; barrier = C.barrier
    ARENA = C.ARENA
    xs_d = C.xs_d; rT_d = C.rT_d; kT_d = C.kT_d; y_d = C.y_d; vf_d = C.vf_d; vr_d = C.vr_d
    x_in = C.x_in
    SH1, A1, G1 = [modbc[:, i * D:(i + 1) * D] for i in range(3)]
    SH2, A2, G2 = SH1, A1, G1

    def bcast_row(vec_ap, n):
        return vec_ap.partition_broadcast(128)

    top = ARENA
    def topget(shape, dt):
        nonlocal top
        nb = 4 if dt == F32 else 2
        for d_ in shape[1:]:
            nb *= d_
        nb = (nb + 31) // 32 * 32
        top -= nb
        return carve(top, shape, dt)
    w2b = topget([128, RW], BF16); a2b = topget([128, RW], BF16)
    g2b = topget([128, RW], BF16); g2b2 = topget([128, RW], BF16)
    v1b = topget([128, 4, 32], BF16); v2b = topget([128, RW], BF16)
    lnw_bc = topget([128, RW], F32); lnb_bc = topget([128, RW], F32); v0_bc = topget([128, RW], F32)
    pcol = C.pcol
    twd = topget([128, S], BF16); adT = topget([128, S], BF16); sgd = topget([128, S], BF16); sgd2 = topget([128, S], BF16)
    att = topget([128, NT, AW], BF16)
    att.k = [Tk() for _ in range(NT)]
    TOP_P1 = twd_top = top + 16384
    TOP_P2 = top

    dma("pool", w2b[0:64, :], Wl["w2"], w=w2b.k)
    dma("pool", a2b[0:64, :], Wl["a2"], w=a2b.k)
    dma("pool", g2b[:, :], Wl["g2"][0:128, :], w=g2b.k)
    dma("pool", g2b2[0:32, :], Wl["g2"][128:160, :], w=g2b2.k)
    dma("sp", lnw_bc[:], Wl["ln_w"].partition_broadcast(128), w=lnw_bc.k)
    dma("sp", lnb_bc[:], Wl["ln_b"].partition_broadcast(128), w=lnb_bc.k)
    if l > 0:
        dma("pool", v1b[:], Wl["v1"].rearrange("(c p) n -> p c n", p=128), w=v1b.k)
        dma("pool", v2b[0:32, :], Wl["v2"], w=v2b.k)
        dma("sp", v0_bc[:], Wl["v0"].partition_broadcast(128), w=v0_bc.k)
    for i, nm in enumerate(["w0", "a0", "k_k", "k_a", "k_a", "r_k"]):
        dma("sp", pcol[:, i, :], Wl[nm].rearrange("(c p) -> p c", p=128), w=pcol.k, allow_slow_non_contiguous=True)
    op("dve", lambda e: e.tensor_scalar(out=pcol[:, 4, :], in0=pcol[:, 4, :], scalar1=-1.0, scalar2=1.0, op0=ALU.mult, op1=ALU.add),
       r=pcol.k, w=pcol.k)

    C.__dict__.update({k_: v_ for k_, v_ in locals().items() if k_ not in ("C",)})
    _adaln(C, l, 0)
    if os.environ.get("KSTOP", "") == "A":
        return

    L1 = Lay()
    QT = L1.get([128, 4, S], BF16); KT = L1.get([128, 4, S], BF16)
    Vaug = L1.get([128, NT, 8, 65], BF16)
    P2base = L1.off
    hT = L1.get([128, 8, S + 2], BF16)
    P1base = L1.off

    def src1(tt):
        if first:
            return x_in[tt * 128:(tt + 1) * 128, :], None
        return xs_d[tt], xs_d.k[tt]

    def norm_phase(L_, src, Abc, SHbc, hT_, router=None, hook=None):
        modbc = C.modbc
        xt = [L_.get([128, D], F32) for _ in range(2)]
        hf = L_.get([128, D], F32)
        hb = [L_.get([128, D], BF16) for _ in range(2)]
        junk = L_.get([128, D], BF16)
        ss = L_.get([128, NT, 4], F32, NT)
        op("dve", lambda e: e.memset(hT_[:, :, 0:1], 0.0), w=hT_.k)
        for tt in range(NT):
            x_ = xt[tt % 2]
            ap, tk = src(tt)
            dma("sp", x_[:], ap, r=([tk] if tk is not None else []), w=x_.k)
            op("act", lambda e: e.activation(out=junk[:], in_=x_[:], func=AF.Square, accum_out=ss[:, tt, 0:1]),
               r=x_.k, w=junk.k + [ss.k[tt]])
            op("act", lambda e: e.activation(out=ss[:, tt, 1:2], in_=ss[:, tt, 0:1], func=AF.Sqrt, bias=RMS_EPS, scale=1.0 / D),
               r=[ss.k[tt]], w=[ss.k[tt]])
            op("dve", lambda e: e.reciprocal(out=ss[:, tt, 2:3], in_=ss[:, tt, 1:2]), r=[ss.k[tt]], w=[ss.k[tt]])
            op("dve", lambda e: e.scalar_tensor_tensor(out=hf[:], in0=x_[:], scalar=ss[:, tt, 2:3], in1=Abc, op0=ALU.mult, op1=ALU.mult),
               r=x_.k + [ss.k[tt]] + modbc.k, w=hf.k)
            h_ = hb[tt % 2]
            if router is None:
                op("dve", lambda e: e.tensor_tensor(out=h_[:], in0=hf[:], in1=SHbc, op=ALU.add), r=hf.k + modbc.k, w=h_.k)
            else:
                op("pool", lambda e: e.tensor_tensor(out=hf[:], in0=hf[:], in1=SHbc, op=ALU.add), r=hf.k + modbc.k, w=hf.k)
                op("act", lambda e: e.copy(out=h_[:], in_=hf[:]), r=hf.k, w=h_.k)
                router(tt, hf)
            pb = psB[tt % 2]
            for kc in range(8):
                op("pe", lambda e: e.transpose(out=pb[:, kc, :], in_=h_[:, kc * 128:(kc + 1) * 128], identity=identb[:]),
                   r=h_.k + identb.k, w=pb.k)
            op("act", lambda e: e.copy(out=hT_[:, :, 1 + tt * 128:1 + (tt + 1) * 128], in_=pb[:]), r=pb.k, w=[hT_.k[(tt // 4) % len(hT_.k)]])
            if hook is not None:
                hook(tt)

    norm_phase(L1, src1, A1, SH1, hT)
    barrier()
    L1.off = P1base
    mubc = L1.get([128, 1824], F32)
    dma("sp", mubc[:], Wl["mu"].partition_broadcast(128), w=mubc.k)
    stg = [L1.get([128, 512], F32) for _ in range(2)]
    stgb = [L1.get([128, 512], BF16) for _ in range(2)]
    vTs = [L1.get([128, 512], BF16) for _ in range(2)]
    zT = L1.get([128, S], BF16)
    vft = [L1.get([128, 512], F32) for _ in range(2)]
    gt = L1.get([128, 512], F32)
    assert L1.off <= TOP_P1, (L1.off, TOP_P1)
    op("pool", lambda e: e.memset(Vaug[:, :, :, 64:65], 1.0), w=Vaug.k)
    C.stgi = 0

    def load_w(c0, cw):
        wb_ = nextw()
        dma("pool", wb_[:, :, 0:cw], Wl["w_in"][:, c0:c0 + cw].rearrange("(kc p) n -> p kc n", p=128), w=wb_.k)
        return wb_

    def scaled(wb_, c0, cw):
        W1 = nextw(); W2 = nextw()
        mu_b = mubc[:, c0 - 1536:c0 - 1536 + cw].unsqueeze(1).to_broadcast([128, 8, cw])
        op("dve", lambda e: e.tensor_tensor(out=W2[:, :, 0:cw], in0=wb_[:, :, 0:cw], in1=mu_b, op=ALU.mult), r=wb_.k + mubc.k, w=W2.k)
        op("pool", lambda e: e.tensor_tensor(out=W1[:, :, 0:cw], in0=wb_[:, :, 0:cw], in1=W2[:, :, 0:cw], op=ALU.subtract),
           r=wb_.k + W2.k, w=W1.k)
        return [(W1, 0), (W2, 1)]

    def fm_proj(wlist, sub0, subw, tg, evac):
        pt = psF[C.pf % 4]; C.pf += 1
        n = len(wlist) * 8
        i = 0
        for (w_, shift) in wlist:
            for kc in range(8):
                o = 1 + tg * 512 - shift
                op("pe", lambda e: e.matmul(pt[0:subw, :], lhsT=w_[:, kc, sub0:sub0 + subw], rhs=hT[:, kc, o:o + 512],
                                            start=(i == 0), stop=(i == n - 1)), r=w_.k + hT.k, w=pt.k)
                i += 1
        evac(pt)

    def tm_proj(wlist, tt, ncols):
        pt = psF[C.pf % 4]; C.pf += 1
        n = len(wlist) * 8
        i = 0
        for (w_, shift) in wlist:
            for kc in range(8):
                o = 1 + tt * 128 - shift
                op("pe", lambda e: e.matmul(pt[:, 0:ncols], lhsT=hT[:, kc, o:o + 128], rhs=w_[:, kc, 0:ncols],
                                            start=(i == 0), stop=(i == n - 1)), r=w_.k + hT.k, w=pt.k)
                i += 1
        return pt

    for (dst, c0, scl) in ((QT, 0, 0.125), (KT, 512, 1.0)):
        wb = load_w(c0, 512)
        for sub in range(4):
            for tg in range(4):
                def ev(pt, dst=dst, sub=sub, tg=tg, scl=scl):
                    op("act", lambda e: e.mul(out=dst[:, sub, tg * 512:(tg + 1) * 512], in_=pt[:], mul=scl), r=pt.k, w=dst.k)
                fm_proj([(wb, 0)], sub * 128, 128, tg, ev)
    wb = load_w(1024, 512)
    for tt in range(NT):
        pt = tm_proj([(wb, 0)], tt, 512)
        op("dve", lambda e: e.tensor_copy(out=Vaug[:, tt, :, 0:64], in_=pt[:].rearrange("p (h d) -> p h d", h=8)), r=pt.k, w=Vaug.k)
    for (dst_d, c0) in ((rT_d, 1536), (kT_d, 2048)):
        wb = load_w(c0, 512)
        wl = scaled(wb, c0, 512)
        for sub in range(4):
            for tg in range(4):
                def ev(pt, dst_d=dst_d, sub=sub, tg=tg):
                    s_ = stg[C.stgi % 2]; C.stgi += 1
                    op("act", lambda e: e.copy(out=s_[:], in_=pt[:]), r=pt.k, w=s_.k)
                    dma("sp", dst_d[sub, :, tg * 512:(tg + 1) * 512], s_[:], r=s_.k, w=[dst_d.k[sub]])
                fm_proj(wl, sub * 128, 128, tg, ev)
    wb = load_w(2560, 512)
    wl = scaled(wb, 2560, 512)
    if l > 0:
        for tg in range(4):
            zp = psF[4]
            for sub in range(4):
                def ev(pt, sub=sub, tg=tg):
                    v_ = vTs[sub % 2]
                    op("act", lambda e: e.copy(out=v_[:], in_=pt[:]), r=pt.k, w=v_.k)
                    op("pe", lambda e: e.matmul(zp[0:32, :], lhsT=v1b[:, sub, :], rhs=v_[:], start=(sub == 0), stop=(sub == 3)),
                       r=v1b.k + v_.k, w=zp.k)
                fm_proj(wl, sub * 128, 128, tg, ev)
            op("act", lambda e: e.copy(out=zT[0:32, tg * 512:(tg + 1) * 512], in_=zp[0:32, :]), r=zp.k, w=zT.k)
    for tt in range(NT):
        pt = tm_proj(wl, tt, 512)
        s_ = stg[tt % 2]; sb_ = stgb[tt % 2]
        op("act", lambda e: e.copy(out=s_[:], in_=pt[:]), r=pt.k, w=s_.k)
        if l == 0:
            dma("sp", vf_d[tt], s_[:], r=s_.k, w=[vf_d.k[tt]])
            op("dve", lambda e: e.tensor_copy(out=sb_[:], in_=s_[:]), r=s_.k, w=sb_.k)
        else:
            vf = vft[tt % 2]
            dma("sp", vf[:], vf_d[tt], r=[vf_d.k[tt]], w=vf.k)
            gp = psF[5]
            op("pe", lambda e: e.matmul(gp[:], lhsT=zT[0:32, tt * 128:(tt + 1) * 128], rhs=v2b[0:32, :], start=True, stop=True),
               r=zT.k + v2b.k, w=gp.k)
            op("dve", lambda e: e.tensor_tensor(out=gt[:], in0=gp[:], in1=v0_bc[:], op=ALU.add), r=gp.k + v0_bc.k, w=gt.k)
            op("act", lambda e: e.activation(out=gt[:], in_=gt[:], func=AF.Sigmoid), r=gt.k, w=gt.k)
            op("dve", lambda e: e.tensor_tensor(out=vf[:], in0=vf[:], in1=s_[:], op=ALU.subtract), r=vf.k + s_.k, w=vf.k)
            op("dve", lambda e: e.tensor_tensor(out=vf[:], in0=vf[:], in1=gt[:], op=ALU.mult), r=vf.k + gt.k, w=vf.k)
            op("dve", lambda e: e.tensor_tensor(out=sb_[:], in0=vf[:], in1=s_[:], op=ALU.add), r=vf.k + s_.k, w=sb_.k)
        dma("sp", vr_d[tt], sb_[:], r=sb_.k, w=[vr_d.k[tt]])
    wb = load_w(3072, 288)
    wl = scaled(wb, 3072, 288)
    for (sub0, subw, dstb, fn) in ((0, 64, twd, AF.Tanh), (64, 64, adT, AF.Copy), (128, 128, sgd, AF.Sigmoid), (256, 32, sgd2, AF.Sigmoid)):
        for tg in range(4):
            def ev(pt, subw=subw, dstb=dstb, fn=fn, tg=tg):
                op("act", lambda e: e.activation(out=dstb[0:subw, tg * 512:(tg + 1) * 512], in_=pt[0:subw, :], func=fn), r=pt.k, w=dstb.k)
            fm_proj(wl, sub0, subw, tg, ev)
    barrier()
    C.__dict__.update({k_: v_ for k_, v_ in locals().items() if k_ not in ("C",)})
    stop = os.environ.get("KSTOP", "")
    if stop == "P1":
        return
    _attention(C, l)
    if stop == "P2":
        return
    _rwkv(C, l)
    if stop == "P3":
        return
    _outproj(C, l)
    if stop == "P4":
        return
    _ffn(C, l)


def _adaln(C, l, half):
    sc = C.sc; op = sc.op; dma = sc.dma
    Wl = C.W[l]
    psF = C.psF; modbc = C.modbc; cactb = C.cactb; onesrow = C.onesrow; rowb = C.rowb
    LA = C.Lay()
    ng = LA.get([128, D], F32)
    dma("sp", ng[:], Wl["norm1_g" if half == 0 else "norm2_g"].partition_broadcast(128), w=ng.k)
    for ch in range(6):
        gch = half * 6 + ch
        wb = C.nextw()
        dma("pool", wb[:], Wl["w_ada"][:, gch * 512:(gch + 1) * 512].rearrange("(kc p) n -> p kc n", p=128), w=wb.k)
        dma("pool", rowb[:], Wl["b_ada"][gch * 512:(gch + 1) * 512].rearrange("(o n) -> o n", o=1), w=rowb.k)
        pt = psF[ch % 2]
        for kc in range(8):
            op("pe", lambda e: e.matmul(pt[:], lhsT=cactb[:, kc, :], rhs=wb[:, kc, :], start=(kc == 0), stop=False),
               r=cactb.k + wb.k, w=pt.k)
        op("pe", lambda e: e.matmul(pt[:], lhsT=onesrow[0:1, :], rhs=rowb[0:1, :], start=False, stop=True),
           r=onesrow.k + rowb.k, w=pt.k)
        which, hh = ch // 2, ch % 2
        dst = modbc[:, ch * 512:(ch + 1) * 512]
        if which == 1:
            op("dve", lambda e: e.scalar_tensor_tensor(out=dst, in0=pt[:], scalar=1.0, in1=ng[:, hh * 512:(hh + 1) * 512],
                                                       op0=ALU.add, op1=ALU.mult), r=pt.k + ng.k, w=modbc.k)
        else:
            op("act", lambda e: e.copy(out=dst, in_=pt[:]), r=pt.k, w=modbc.k)
    C.barrier()


def _attention(C, l):
    sc = C.sc; op = sc.op; dma = sc.dma
    psF = C.psF
    QT = C.QT; KT = C.KT; Vaug = C.Vaug; att = C.att
    L2 = C.Lay()
    L2.off = C.P2base
    amask = L2.get([128, 8, S], BF16)
    Pt = [L2.get([128, 512], BF16) for _ in range(4)]
    rz = L2.get([128, 8], F32)
    assert L2.off <= C.TOP_P2
    for h in range(8):
        dma("sp", amask[:, h, :], C.amask_d[:, h * S:(h + 1) * S], r=C.amask_d.k, w=amask.k)
    steps = [(h, qg, j) for h in range(8) for qg in range(4) for j in range(4 * qg + 4)]

    def unit_a(i):
        h, qg, j = steps[i]
        hp, po = h // 2, (h % 2) * 64
        qlo = max(4 * qg, j)
        nq = (4 * qg + 4 - qlo) * 128
        pS = psF[4 + (i % 4)]
        P_ = Pt[i % 4]
        op("pe", lambda e: e.matmul(pS[:, 0:nq], lhsT=KT[po:po + 64, hp, j * 128:(j + 1) * 128],
                                    rhs=QT[po:po + 64, hp, qlo * 128:qlo * 128 + nq], start=True, stop=True),
           r=KT.k + QT.k, w=pS.k)
        op("act", lambda e: e.activation(out=P_[:, 0:nq], in_=pS[:, 0:nq], func=AF.Exp), r=pS.k, w=P_.k)
        u0 = (qlo - j) * 128
        op("dve", lambda e: e.tensor_tensor(out=P_[:, 0:nq], in0=P_[:, 0:nq], in1=amask[:, h, u0:u0 + nq], op=ALU.mult),
           r=P_.k + amask.k, w=P_.k)

    def unit_b(i):
        h, qg, j = steps[i]
        qlo = max(4 * qg, j)
        P_ = Pt[i % 4]
        for ii in range(qlo, 4 * qg + 4):
            pO = psF[ii - 4 * qg]
            op("pe", lambda e: e.matmul(pO[:, 0:65], lhsT=P_[:, (ii - qlo) * 128:(ii - qlo + 1) * 128], rhs=Vaug[:, j, h, :],
                                        start=(j == 0), stop=(j == ii)), r=P_.k + Vaug.k, w=pO.k)
        if j == 4 * qg + 3:
            for ib in range(4):
                ii = 4 * qg + ib
                pO = psF[ib]
                op("dve", lambda e: e.reciprocal(out=rz[:, h:h + 1], in_=pO[:, 64:65]), r=pO.k, w=rz.k)
                op("dve", lambda e: e.tensor_scalar(out=att[:, ii, h * 64:(h + 1) * 64], in0=pO[:, 0:64], scalar1=rz[:, h:h + 1], scalar2=None,
                                                    op0=ALU.mult), r=pO.k + rz.k, w=[att.k[ii]])

    LOOK = 3
    for i in range(min(LOOK, len(steps))):
        unit_a(i)
    for i in range(len(steps)):
        unit_b(i)
        if i + LOOK < len(steps):
            unit_a(i + LOOK)
    barrier = C.barrier
    barrier()


def _rwkv(C, l):
    sc = C.sc
    defer = {"lst": None}

    def op(e, fn, r=(), w=()):
        if defer["lst"] is not None:
            defer["lst"].append(lambda: sc.op(e, fn, r, w))
            return None
        return sc.op(e, fn, r, w)

    def dma(q, out, in_, r=(), w=(), **kw):
        if defer["lst"] is not None:
            defer["lst"].append(lambda: sc.dma(q, out, in_, r, w, **kw))
            return None
        return sc.dma(q, out, in_, r, w, **kw)

    def atomic(fns):
        def run():
            for f in fns:
                f()
        if defer["lst"] is not None:
            defer["lst"].append(run)
        else:
            run()
    psF = C.psF; psB = C.psB
    identb = C.identb; mu2 = C.mu2; mls = C.mls; blockones = C.blockones
    pcol = C.pcol; w2b = C.w2b; a2b = C.a2b; twd = C.twd; adT = C.adT
    rT_d = C.rT_d; kT_d = C.kT_d; y_d = C.y_d; vr_d = C.vr_d
    L3 = C.Lay()
    ARt = L3.get([128, NT, 256], BF16, 4)
    scanmask = L3.get([128, S], BF16)
    dma("pool", scanmask[:], C.cst["scanmask"], w=scanmask.k)
    bt = L3.get([128, S], BF16, 4); kt = L3.get([128, S], BF16, 4); bh = L3.get([128, S], BF16, 4); kh = L3.get([128, S], BF16, 4)
    prodb = L3.get([128, S], BF16)
    WC = L3.get([128, NT], F32, 4)
    rf = L3.get([128, 512], F32); kf = L3.get([128, 512], F32); lw = L3.get([128, 512], F32); af = L3.get([128, 512], F32)
    Lc = L3.get([128, 512], F32); t1 = L3.get([128, 512], F32); t2 = L3.get([128, 512], F32); t3 = L3.get([128, 512], F32)
    t4 = L3.get([128, 512], F32); sqb = L3.get([128, 512], BF16)
    N1s = [L3.get([128, 4, 256], BF16) for _ in range(3)]
    N2s = [L3.get([128, 4, 256], BF16) for _ in range(3)]
    N3s = [L3.get([128, 4, 128], BF16) for _ in range(2)]
    Ab = [[L3.get([128, 4, 128], BF16) for _ in range(2)] for _c in range(2)]
    ATb = [[L3.get([128, 4, 128], BF16) for _ in range(2)] for _c in range(2)]
    Pb = [[L3.get([128, 4, 128], BF16) for _ in range(2)] for _c in range(2)]
    Tinv = [L3.get([128, 4, 128], BF16) for _ in range(3)]
    ZB = [L3.get([128, 2, 2, 128], BF16) for _ in range(3)]
    ZK = [L3.get([128, 2, 2, 128], BF16) for _ in range(3)]
    Vt = [L3.get([128, RW], BF16) for _ in range(4)]
    Xs = L3.get([128, 128], BF16); Us = L3.get([128, 128], BF16)
    Sf = L3.get([128, 64], F32); Sbf = L3.get([128, 2, 64], BF16)
    ystg = [L3.get([128, 128], F32) for _ in range(2)]
    assert L3.off <= C.TOP_P2, (L3.off, C.TOP_P2)
    if os.environ.get("KDEBUG"):
        print("L3.off", L3.off, "TOP_P2", C.TOP_P2)
    prod_d = C.prod_d
    NEG = -math.exp(-0.5)
    for z_ in ZB + ZK:
        op("pool", lambda e: e.memset(z_[:], 0.0), w=z_.k)

    def prep_unit(hp, tg):
        pc = lambda i: pcol[:, i, hp:hp + 1]
        hcols = slice(hp * 128, (hp + 1) * 128)
        ts_ = slice(tg * 512, (tg + 1) * 512)
        dma("sp", rf[:], rT_d[hp, :, ts_], r=[rT_d.k[hp]], w=rf.k)
        dma("sp", kf[:], kT_d[hp, :, ts_], r=[kT_d.k[hp]], w=kf.k)
        pz = psF[6]
        atomic([lambda: sc.op("pe", lambda e: e.matmul(pz[:], lhsT=w2b[0:64, hcols], rhs=twd[0:64, ts_], start=True, stop=True), r=w2b.k + twd.k, w=pz.k),
                lambda: sc.op("act", lambda e: e.activation(out=lw[:], in_=pz[:], func=AF.Sigmoid, bias=pc(0), scale=1.0), r=pz.k + pcol.k, w=lw.k)])
        op("pool", lambda e: e.tensor_scalar(out=lw[:], in0=lw[:], scalar1=NEG, scalar2=None, op0=ALU.mult), r=lw.k, w=lw.k)
        pz2 = psF[6]
        atomic([lambda: sc.op("pe", lambda e: e.matmul(pz2[:], lhsT=a2b[0:64, hcols], rhs=adT[0:64, ts_], start=True, stop=True), r=a2b.k + adT.k, w=pz2.k),
                lambda: sc.op("act", lambda e: e.activation(out=af[:], in_=pz2[:], func=AF.Sigmoid, bias=pc(1), scale=1.0), r=pz2.k + pcol.k, w=af.k)])
        op("dve", lambda e: e.tensor_tensor_scan(out=Lc[:], data0=scanmask[:, ts_], data1=lw[:], initial=0.0, op0=ALU.mult, op1=ALU.add),
           r=scanmask.k + lw.k, w=Lc.k)
        op("dve", lambda e: e.tensor_scalar(out=t1[:], in0=kf[:], scalar1=pc(2), scalar2=None, op0=ALU.mult), r=kf.k + pcol.k, w=t1.k)
        op("act", lambda e: e.activation(out=sqb[:], in_=t1[:], func=AF.Square), r=t1.k, w=sqb.k)
        pn = psF[6]
        atomic([lambda: sc.op("pe", lambda e: e.matmul(pn[:], lhsT=blockones[:], rhs=sqb[:], start=True, stop=True), r=blockones.k + sqb.k, w=pn.k),
                lambda: sc.op("act", lambda e: e.activation(out=t2[:], in_=pn[:], func=AF.Sqrt), r=pn.k, w=t2.k)])
        op("dve", lambda e: e.tensor_scalar(out=t2[:], in0=t2[:], scalar1=1e-12, scalar2=None, op0=ALU.max), r=t2.k, w=t2.k)
        op("dve", lambda e: e.reciprocal(out=t2[:], in_=t2[:]), r=t2.k, w=t2.k)
        op("dve", lambda e: e.tensor_tensor(out=t1[:], in0=t1[:], in1=t2[:], op=ALU.mult), r=t1.k + t2.k, w=t1.k)
        op("dve", lambda e: e.tensor_scalar(out=t3[:], in0=af[:], scalar1=pc(3), scalar2=pc(4), op0=ALU.mult, op1=ALU.add),
           r=af.k + pcol.k, w=t3.k)
        op("dve", lambda e: e.tensor_tensor(out=kf[:], in0=kf[:], in1=t3[:], op=ALU.mult), r=kf.k + t3.k, w=kf.k)
        op("dve", lambda e: e.tensor_tensor(out=t3[:], in0=t1[:], in1=af[:], op=ALU.mult), r=t1.k + af.k, w=t3.k)
        op("dve", lambda e: e.scalar_tensor_tensor(out=prodb[:, ts_], in0=rf[:], scalar=pc(5), in1=kf[:], op0=ALU.mult, op1=ALU.mult),
           r=rf.k + kf.k + pcol.k, w=prodb.k)
        op("act", lambda e: e.activation(out=t2[:], in_=Lc[:], func=AF.Exp), r=Lc.k, w=t2.k)
        op("dve", lambda e: e.tensor_tensor(out=ARt[:, 4 * tg:4 * tg + 4, 128:256], in0=rf[:].rearrange("p (a b) -> p a b", a=4),
                                            in1=t2[:].rearrange("p (a b) -> p a b", a=4), op=ALU.mult), r=rf.k + t2.k, w=[ARt.k[tg]])
        op("pool", lambda e: e.tensor_tensor(out=t4[:], in0=Lc[:], in1=lw[:], op=ALU.subtract), r=Lc.k + lw.k, w=t4.k)
        op("act", lambda e: e.activation(out=t4[:], in_=t4[:], func=AF.Exp), r=t4.k, w=t4.k)
        op("dve", lambda e: e.scalar_tensor_tensor(out=ARt[:, 4 * tg:4 * tg + 4, 0:128], in0=t1[:].rearrange("p (a b) -> p a b", a=4),
                                                   scalar=-1.0, in1=t4[:].rearrange("p (a b) -> p a b", a=4), op0=ALU.mult, op1=ALU.mult),
           r=t1.k + t4.k, w=[ARt.k[tg]])
        op("act", lambda e: e.activation(out=t2[:], in_=Lc[:], func=AF.Exp, scale=-1.0), r=Lc.k, w=t2.k)
        op("dve", lambda e: e.tensor_tensor(out=bt[:, ts_], in0=t3[:], in1=t2[:], op=ALU.mult), r=t3.k + t2.k, w=[bt.k[tg]])
        op("dve", lambda e: e.tensor_tensor(out=kt[:, ts_], in0=kf[:], in1=t2[:], op=ALU.mult), r=kf.k + t2.k, w=[kt.k[tg]])
        for q in range(4):
            cs = slice(q * 128, (q + 1) * 128)
            op("act", lambda e, cs=cs, q=q: e.activation(out=t4[:, cs], in_=Lc[:, cs], func=AF.Exp, scale=-1.0, bias=Lc[:, q * 128 + 127:q * 128 + 128]),
               r=Lc.k, w=t4.k)
        op("dve", lambda e: e.tensor_tensor(out=bh[:, ts_], in0=t3[:], in1=t4[:], op=ALU.mult), r=t3.k + t4.k, w=[bh.k[tg]])
        op("dve", lambda e: e.tensor_tensor(out=kh[:, ts_], in0=kf[:], in1=t4[:], op=ALU.mult), r=kf.k + t4.k, w=[kh.k[tg]])
        op("act", lambda e: e.activation(out=WC[:, 4 * tg:4 * tg + 4], in_=Lc[:, 127::128], func=AF.Exp), r=Lc.k, w=[WC.k[tg]])
        if tg == 3:
            dma("sp", prod_d[hp], prodb[:], r=prodb.k, w=[prod_d.k[hp]])

    pending = []

    def queue_prep(hp_, tg_):
        defer["lst"] = []
        prep_unit(hp_, tg_)
        lst = defer["lst"]
        defer["lst"] = None
        pending.extend((hp_, tg_, t) for t in lst)

    def pop_prep(n):
        for _ in range(n):
            if pending:
                pending.pop(0)[2]()

    def flush_prep(hp_, tg_):
        while pending and (pending[0][0], pending[0][1]) <= (hp_, tg_):
            pending.pop(0)[2]()

    for tg in range(4):
        prep_unit(0, tg)
    for hp in range(4):
        op("dve", lambda e: e.memset(Sf[:], 0.0), w=Sf.k)
        op("dve", lambda e: e.memset(Sbf[:], 0.0), w=Sbf.k)
        def pre_units(b2):
            par = b2 % 3
            ch = b2 % 2
            B0, B1, B2 = psF[3 * ch], psF[3 * ch + 1], psF[3 * ch + 2]
            n3 = N3s[ch]
            n1 = N1s[par]; n2 = N2s[par]; ti_ = Tinv[par]; zb = ZB[par]; zk = ZK[par]
            st = {}
            units = []

            def u_nmat():
                mu2b = mu2[:].unsqueeze(1).to_broadcast([128, 2, 256])
                mlsb = mls[:].unsqueeze(1).to_broadcast([128, 2, 128])
                tg_ = b2 // 2
                for rnd in range(2):
                    for idx in range(4):
                        hd, ti = idx // 2, idx % 2
                        tt = 2 * b2 + ti
                        po = hd * 64
                        tcs = slice(tt * 128, (tt + 1) * 128)
                        pb_ = B0 if hd == 0 else B1
                        src = bt if rnd == 0 else kt
                        op("pe", lambda e: e.matmul(pb_[:, ti * 256:ti * 256 + 256], lhsT=src[po:po + 64, tcs], rhs=ARt[po:po + 64, tt, :], start=True, stop=True),
                           r=[src.k[tg_], ARt.k[tg_]], w=pb_.k)
                        if hd == rnd:
                            op("pe", lambda e: e.matmul(B2[:, ti * 128:(ti + 1) * 128], lhsT=ARt[po:po + 64, tt, 0:128], rhs=bt[po:po + 64, tcs],
                                                        start=True, stop=True), r=[bt.k[tg_], ARt.k[tg_]], w=B2.k)
                    dst = n1 if rnd == 0 else n2
                    for b_ in range(2):
                        pb_ = B0 if b_ == 0 else B1
                        op("dve", lambda e: e.tensor_tensor(out=dst[:, 2 * b_:2 * b_ + 2, :], in0=pb_[:].rearrange("p (a b) -> p a b", a=2),
                                                            in1=mu2b, op=ALU.mult), r=pb_.k + mu2.k, w=dst.k)
                    op("dve", lambda e: e.tensor_tensor(out=n3[:, 2 * rnd:2 * rnd + 2, :], in0=B2[:, 0:256].rearrange("p (a b) -> p a b", a=2),
                                                        in1=mlsb, op=ALU.mult), r=B2.k + mls.k, w=n3.k)
                op("dve", lambda e: e.tensor_tensor(out=Pb[ch][0][:], in0=n1[:, :, 0:128], in1=identb[:].unsqueeze(1).to_broadcast([128, 4, 128]), op=ALU.add),
                   r=n1.k + identb.k, w=Pb[ch][0].k)
                st["A"] = (lambda idx: n1[:, idx, 0:128]); st["AT"] = (lambda idx: n3[:, idx, :])
                st["Ak"] = n1.k; st["ATk"] = n3.k; st["P"] = Pb[ch][0]
            units.append(u_nmat)

            def mk_level(lev):
                def u():
                    pA, pAT, pP = B0, B1, B2
                    An = Ab[ch][lev % 2]; ATn = ATb[ch][lev % 2]
                    Pn = Pb[ch][lev % 2] if lev < 6 else ti_
                    A_cur, AT_cur, A_k, AT_k, P_cur = st["A"], st["AT"], st["Ak"], st["ATk"], st["P"]
                    for idx in range(4):
                        cs = slice(idx * 128, (idx + 1) * 128)
                        if lev < 6:
                            op("pe", lambda e: e.matmul(pA[:, cs], lhsT=AT_cur(idx), rhs=A_cur(idx), start=True, stop=True), r=A_k + AT_k, w=pA.k)
                        op("pe", lambda e: e.matmul(pAT[:, cs], lhsT=A_cur(idx), rhs=AT_cur(idx), start=True, stop=True), r=A_k + AT_k, w=pAT.k)
                    if lev < 6:
                        op("act", lambda e: e.copy(out=An[:], in_=pA[:].rearrange("p (a b) -> p a b", a=4)), r=pA.k, w=An.k)
                    op("dve", lambda e: e.tensor_copy(out=ATn[:], in_=pAT[:].rearrange("p (a b) -> p a b", a=4)), r=pAT.k, w=ATn.k)
                    for idx in range(4):
                        cs = slice(idx * 128, (idx + 1) * 128)
                        op("pe", lambda e: e.matmul(pP[:, cs], lhsT=ATn[:, idx, :], rhs=P_cur[:, idx, :], start=True, stop=False), r=ATn.k + P_cur.k, w=pP.k)
                        op("pe", lambda e: e.matmul(pP[:, cs], lhsT=identb[:], rhs=P_cur[:, idx, :], start=False, stop=True), r=identb.k + P_cur.k, w=pP.k)
                    op("act", lambda e: e.copy(out=Pn[:], in_=pP[:].rearrange("p (a b) -> p a b", a=4)), r=pP.k, w=Pn.k)
                    st["A"] = (lambda idx: An[:, idx, :]); st["AT"] = (lambda idx: ATn[:, idx, :])
                    st["Ak"] = An.k; st["ATk"] = ATn.k; st["P"] = Pn
                return u
            for lev in range(1, 7):
                units.append(mk_level(lev))

            def u_tr():
                pb = psB[0]
                for ti in range(2):
                    tt = 2 * b2 + ti
                    tcs = slice(tt * 128, (tt + 1) * 128)
                    op("pe", lambda e: e.transpose(out=pb[:, 2 * ti, :], in_=bh[:, tcs], identity=identb[:]), r=[bh.k[b2 // 2]] + identb.k, w=pb.k)
                    op("pe", lambda e: e.transpose(out=pb[:, 2 * ti + 1, :], in_=kh[:, tcs], identity=identb[:]), r=[kh.k[b2 // 2]] + identb.k, w=pb.k)
                for ti in range(2):
                    for hd in range(2):
                        hs = slice(hd * 64, hd * 64 + 64)
                        op("act", lambda e: e.copy(out=zb[:, ti, hd, hs], in_=pb[:, 2 * ti, hs]), r=pb.k, w=zb.k)
                        op("act", lambda e: e.copy(out=zk[:, ti, hd, hs], in_=pb[:, 2 * ti + 1, hs]), r=pb.k, w=zk.k)
            units.append(u_tr)
            return units

        def seq_units(b2):
            par = b2 % 3
            n1 = N1s[par]; n2 = N2s[par]; ti_ = Tinv[par]; zb = ZB[par]; zk = ZK[par]
            pq = psF[7]
            units = []
            for ti in range(2):
                tt = 2 * b2 + ti
                v_ = Vt[tt % 4]

                def hv(hd, v_=v_):
                    return v_[:, (2 * hp + hd) * 64:(2 * hp + hd) * 64 + 64]

                def u_x(ti=ti, tt=tt, v_=v_, hv=hv):
                    dma("sp", v_[:], vr_d[tt], r=[vr_d.k[tt]], w=v_.k)
                    for hd in range(2):
                        idx = hd * 2 + ti
                        op("pe", lambda e: e.matmul(pq[:, hd * 64:hd * 64 + 64], lhsT=n2[:, idx, 0:128], rhs=hv(hd), start=True, stop=False),
                           r=n2.k + v_.k, w=pq.k)
                        op("pe", lambda e: e.matmul(pq[:, hd * 64:hd * 64 + 64], lhsT=ARt[:, tt, 0:128], rhs=Sbf[:, hd, :], start=False, stop=True),
                           r=[ARt.k[b2 // 2]] + Sbf.k, w=pq.k)
                    op("act", lambda e: e.copy(out=Xs[:], in_=pq[:, 0:128]), r=pq.k, w=Xs.k)

                def u_u(ti=ti, tt=tt):
                    for hd in range(2):
                        idx = hd * 2 + ti
                        op("pe", lambda e: e.matmul(pq[:, 128 + hd * 64:128 + hd * 64 + 64], lhsT=ti_[:, idx, :], rhs=Xs[:, hd * 64:hd * 64 + 64], start=True, stop=True),
                           r=ti_.k + Xs.k, w=pq.k)
                    op("dve", lambda e: e.tensor_copy(out=Us[:], in_=pq[:, 128:256]), r=pq.k, w=Us.k)

                def u_y(ti=ti, tt=tt, v_=v_, hv=hv):
                    for hd in range(2):
                        idx = hd * 2 + ti
                        yc = slice(256 + hd * 64, 256 + hd * 64 + 64)
                        op("pe", lambda e: e.matmul(pq[:, yc], lhsT=ARt[:, tt, 128:256], rhs=Sbf[:, hd, :], start=True, stop=False),
                           r=[ARt.k[b2 // 2]] + Sbf.k, w=pq.k)
                        op("pe", lambda e: e.matmul(pq[:, yc], lhsT=n1[:, idx, 128:256], rhs=Us[:, hd * 64:hd * 64 + 64], start=False, stop=False),
                           r=n1.k + Us.k, w=pq.k)
                        op("pe", lambda e: e.matmul(pq[:, yc], lhsT=n2[:, idx, 128:256], rhs=hv(hd), start=False, stop=True), r=n2.k + v_.k, w=pq.k)
                    for hd in range(2):
                        op("pe", lambda e: e.matmul(pq[:, 384:448], lhsT=zb[:, ti, hd, :], rhs=Us[:, hd * 64:hd * 64 + 64], start=(hd == 0), stop=False),
                           r=zb.k + Us.k, w=pq.k)
                        op("pe", lambda e: e.matmul(pq[:, 384:448], lhsT=zk[:, ti, hd, :], rhs=hv(hd), start=False, stop=(hd == 1)), r=zk.k + v_.k, w=pq.k)

                def u_s(ti=ti, tt=tt):
                    op("dve", lambda e: e.scalar_tensor_tensor(out=Sf[:], in0=Sf[:], scalar=WC[:, tt:tt + 1], in1=pq[:, 384:448], op0=ALU.mult, op1=ALU.add),
                       r=Sf.k + [WC.k[b2 // 2]] + pq.k, w=Sf.k)
                    op("act", lambda e: e.copy(out=Sbf[0:64, 0, :], in_=Sf[0:64, :]), r=Sf.k, w=Sbf.k)
                    op("act", lambda e: e.copy(out=Sbf[64:128, 1, :], in_=Sf[64:128, :]), r=Sf.k, w=Sbf.k)
                    ys = ystg[tt % 2]
                    op("act", lambda e: e.copy(out=ys[:], in_=pq[:, 256:384]), r=pq.k, w=ys.k)
                    dma("sp", y_d[tt][:, hp * 128:(hp + 1) * 128], ys[:], r=ys.k, w=[y_d.k[tt]])
                units += [u_x, u_u, u_y, u_s]
            return units

        flush_prep(hp, 0)
        pre = {0: pre_units(0), 1: pre_units(1)}
        for u in pre[0]:
            u()
        for u in pre[1][0:4]:
            u()
        for b2 in range(8):
            sq = seq_units(b2)
            p1 = pre[b2 + 1][4:8] if b2 + 1 < 8 else []
            if b2 + 2 < 8:
                flush_prep(hp, (b2 + 2) // 2)
                pre[b2 + 2] = pre_units(b2 + 2)
                p2 = pre[b2 + 2][0:4]
            else:
                p2 = []
            for k in range(8):
                j = k // 2
                if k % 2 == 0:
                    if j < len(p2):
                        p2[j]()
                else:
                    if j < len(p1):
                        p1[j]()
                sq[k]()
                pop_prep(int(os.environ.get("POPN", "3")))
            if hp + 1 < 4 and b2 % 2 == 1:
                queue_prep(hp + 1, b2 // 2)
    flush_prep(9, 9)
    for _ in range(int(os.environ.get("EXTRA", "0"))):
        op("pe", lambda e: e.matmul(psF[0][:, 0:128], lhsT=identb[:], rhs=identb[:], start=True, stop=True), r=identb.k, w=psF[0].k)
    C.barrier()


def _outproj(C, l):
    sc = C.sc; op = sc.op; dma = sc.dma
    psF = C.psF; psB = C.psB; identb = C.identb
    Wl = C.W[l]
    att = C.att; sgd = C.sgd; sgd2 = C.sgd2; g2b = C.g2b; g2b2 = C.g2b2; lnw_bc = C.lnw_bc; lnb_bc = C.lnb_bc
    y_d = C.y_d; vr_d = C.vr_d; xs_d = C.xs_d; prod_d = C.prod_d; headsel = C.headsel
    G1 = C.G1
    L4 = C.Lay()
    yt = [L4.get([128, RW], F32) for _ in range(2)]
    vt = [L4.get([128, RW], BF16) for _ in range(2)]
    pr = [L4.get([128, 4, 128], BF16) for _ in range(2)]
    sq = L4.get([128, RW], F32); yc = L4.get([128, RW], F32); bo = L4.get([128, RW], F32)
    st = L4.get([128, 8, 8], F32)
    rwo = [L4.get([128, RW], BF16) for _ in range(2)]
    catT = [L4.get([128, 8, 128], BF16) for _ in range(2)]
    xt = [L4.get([128, D], F32) for _ in range(2)]
    xn = [L4.get([128, D], F32) for _ in range(2)]
    assert L4.off <= C.TOP_P2
    wo = [C.nextw(), C.nextw()]
    for nh in range(2):
        dma("pool", wo[nh][:], Wl["w_out"][:, nh * 512:(nh + 1) * 512].rearrange("(kc p) n -> p kc n", p=128), w=wo[nh].k)
    def unit_a(tt):
        y_ = yt[tt % 2]; v_ = vt[tt % 2]; p_ = pr[tt % 2]
        dma("sp", y_[:], y_d[tt], r=[y_d.k[tt]], w=y_.k)
        dma("sp", v_[:], vr_d[tt], r=[vr_d.k[tt]], w=v_.k)
        dma("sp", p_[:], prod_d.t[:, :, tt * 128:(tt + 1) * 128].rearrange("a p t -> p a t"), r=prod_d.k, w=p_.k)
        y3 = y_[:].rearrange("p (h d) -> p h d", h=8)
        S1, S2, MEAN, MSQ, VAR, RSTD, RK = [st[:, i, :] for i in range(7)]
        op("dve", lambda e: e.tensor_reduce(out=S1, in_=y3, axis=AX.X, op=ALU.add), r=y_.k, w=st.k)
        op("pool", lambda e: e.tensor_tensor(out=sq[:], in0=y_[:], in1=y_[:], op=ALU.mult), r=y_.k, w=sq.k)
        op("dve", lambda e: e.tensor_reduce(out=S2, in_=sq[:].rearrange("p (h d) -> p h d", h=8), axis=AX.X, op=ALU.add), r=sq.k + st.k, w=st.k)
        op("dve", lambda e: e.tensor_scalar(out=MEAN, in0=S1, scalar1=1.0 / 64, scalar2=None, op0=ALU.mult), r=st.k, w=st.k)
        op("dve", lambda e: e.tensor_tensor(out=MSQ, in0=MEAN, in1=MEAN, op=ALU.mult), r=st.k, w=st.k)
        op("dve", lambda e: e.scalar_tensor_tensor(out=VAR, in0=S2, scalar=1.0 / 64, in1=MSQ, op0=ALU.mult, op1=ALU.subtract), r=st.k, w=st.k)
        op("act", lambda e: e.activation(out=RSTD, in_=VAR, func=AF.Sqrt, bias=GN_EPS, scale=1.0), r=st.k, w=st.k)
        op("dve", lambda e: e.reciprocal(out=RSTD, in_=RSTD), r=st.k, w=st.k)
        yc3 = yc[:].rearrange("p (h d) -> p h d", h=8)
        op("dve", lambda e: e.tensor_tensor(out=yc3, in0=y3, in1=MEAN.unsqueeze(2).to_broadcast([128, 8, 64]), op=ALU.subtract), r=y_.k + st.k, w=yc.k)
        op("dve", lambda e: e.tensor_tensor(out=yc3, in0=yc3, in1=RSTD.unsqueeze(2).to_broadcast([128, 8, 64]), op=ALU.mult), r=yc.k + st.k, w=yc.k)
        op("pool", lambda e: e.tensor_tensor(out=yc[:], in0=yc[:], in1=lnw_bc[:], op=ALU.mult), r=yc.k + lnw_bc.k, w=yc.k)
        op("pool", lambda e: e.tensor_tensor(out=yc[:], in0=yc[:], in1=lnb_bc[:], op=ALU.add), r=yc.k + lnb_bc.k, w=yc.k)
        prk = psF[0]
        for hp in range(4):
            op("pe", lambda e: e.matmul(prk[:, 2 * hp:2 * hp + 2], lhsT=p_[:, hp, :], rhs=headsel[:], start=True, stop=True), r=p_.k + headsel.k, w=prk.k)
        op("act", lambda e: e.copy(out=RK, in_=prk[:, 0:8]), r=prk.k, w=st.k)
        op("dve", lambda e: e.tensor_tensor(out=bo[:].rearrange("p (h d) -> p h d", h=8), in0=v_[:].rearrange("p (h d) -> p h d", h=8),
                                            in1=RK.unsqueeze(2).to_broadcast([128, 8, 64]), op=ALU.mult), r=v_.k + st.k, w=bo.k)
        op("pool", lambda e: e.tensor_tensor(out=yc[:], in0=yc[:], in1=bo[:], op=ALU.add), r=yc.k + bo.k, w=yc.k)
        pg = psF[1]
        tcs = slice(tt * 128, (tt + 1) * 128)
        op("pe", lambda e: e.matmul(pg[:], lhsT=sgd[:, tcs], rhs=g2b[:, :], start=True, stop=False), r=sgd.k + g2b.k, w=pg.k)
        op("pe", lambda e: e.matmul(pg[:], lhsT=sgd2[0:32, tcs], rhs=g2b2[0:32, :], start=False, stop=True), r=sgd2.k + g2b2.k, w=pg.k)
        ro = rwo[tt % 2]
        op("dve", lambda e: e.tensor_tensor(out=ro[:], in0=pg[:], in1=yc[:], op=ALU.mult), r=pg.k + yc.k, w=ro.k)

    def unit_b(tt):
        ro = rwo[tt % 2]
        pb = psB[tt % 2]
        for kc in range(8):
            src = att[:, tt, kc * 128:(kc + 1) * 128] if kc < 4 else ro[:, (kc - 4) * 128:(kc - 3) * 128]
            op("pe", lambda e: e.transpose(out=pb[:, kc, :], in_=src, identity=identb[:]), r=[att.k[tt]] + ro.k + identb.k, w=pb.k)
        cT = catT[tt % 2]
        op("act", lambda e: e.copy(out=cT[:], in_=pb[:]), r=pb.k, w=cT.k)
        x_ = xt[tt % 2]; xo = xn[tt % 2]
        if l == C.layers[0] and C.first_from_input:
            dma("sp", x_[:], C.x_in[tt * 128:(tt + 1) * 128, :], w=x_.k)
        else:
            dma("sp", x_[:], xs_d[tt], r=[xs_d.k[tt]], w=x_.k)
        for nh in range(2):
            po_ = psF[2 + nh]
            for kc in range(8):
                op("pe", lambda e: e.matmul(po_[:], lhsT=cT[:, kc, :], rhs=wo[nh][:, kc, :], start=(kc == 0), stop=(kc == 7)), r=cT.k + wo[nh].k, w=po_.k)
            ns = slice(nh * 512, (nh + 1) * 512)
            op("dve", lambda e: e.tensor_tensor(out=xo[:, ns], in0=po_[:], in1=G1[:, ns], op=ALU.mult), r=po_.k + C.modbc.k, w=xo.k)
        op("dve", lambda e: e.tensor_tensor(out=xo[:], in0=xo[:], in1=x_[:], op=ALU.add), r=xo.k + x_.k, w=xo.k)
        dma("sp", xs_d[tt], xo[:], r=xo.k, w=[xs_d.k[tt]])
        if l in C.tapx:
            dma("sp", C.tapx[l][tt * 128:(tt + 1) * 128, :], xo[:], r=xo.k)
    unit_a(0)
    for tt in range(NT):
        if tt + 1 < NT:
            unit_a(tt + 1)
        unit_b(tt)
    C.barrier()


def _ffn(C, l):
    sc = C.sc; op = sc.op; dma = sc.dma
    psF = C.psF
    Wl = C.W[l]
    xs_d = C.xs_d
    moe = (l % 2 == 1)
    _adaln(C, l, 1)
    L5 = C.Lay()
    hT = L5.get([128, 8, S + 2], BF16, 4)
    gates = L5.get([128, NT, 8], F32, NT)
    base = L5.off
    if moe:
        rwbc = L5.get([128, 8, D], F32)
        rbbc = L5.get([128, 8], F32)
        lgt = L5.get([128, 16], F32)
        jk = L5.get([128, D], F32)
        for e_ in range(NE):
            dma("sp", rwbc[:, e_, :], Wl["rw"][e_].partition_broadcast(128), w=rwbc.k)
        dma("sp", rbbc[:], Wl["rb"].partition_broadcast(128), w=rbbc.k)

        def router(tt, hf):
            LG = lgt[:, 0:8]
            for e_ in range(NE):
                op("dve", lambda e: e.scalar_tensor_tensor(out=jk[:], in0=hf[:], scalar=1.0, in1=rwbc[:, e_, :], op0=ALU.mult, op1=ALU.mult,
                                                           accum_out=lgt[:, e_:e_ + 1]), r=hf.k + rwbc.k, w=jk.k + lgt.k)
            M1, NM1, M2, E2 = [lgt[:, 8 + i:9 + i] for i in range(4)]
            EQ = jk[:, 0:8]; L2_ = jk[:, 8:16]; EX = jk[:, 16:24]; SEL = jk[:, 24:32]
            op("dve", lambda e: e.tensor_tensor(out=LG, in0=LG, in1=rbbc[:], op=ALU.add), r=lgt.k + rbbc.k, w=lgt.k)
            op("dve", lambda e: e.tensor_reduce(out=M1, in_=LG, axis=AX.X, op=ALU.max), r=lgt.k, w=lgt.k)
            op("dve", lambda e: e.tensor_scalar(out=EQ, in0=LG, scalar1=M1, scalar2=None, op0=ALU.is_equal), r=lgt.k, w=jk.k)
            op("dve", lambda e: e.scalar_tensor_tensor(out=L2_, in0=EQ, scalar=-1e30, in1=LG, op0=ALU.mult, op1=ALU.add), r=jk.k + lgt.k, w=jk.k)
            op("dve", lambda e: e.tensor_reduce(out=M2, in_=L2_, axis=AX.X, op=ALU.max), r=jk.k, w=lgt.k)
            op("dve", lambda e: e.tensor_scalar(out=SEL, in0=LG, scalar1=M2, scalar2=None, op0=ALU.is_ge), r=lgt.k, w=jk.k)
            op("dve", lambda e: e.tensor_scalar(out=NM1, in0=M1, scalar1=-1.0, scalar2=None, op0=ALU.mult), r=lgt.k, w=lgt.k)
            op("act", lambda e: e.activation(out=EX, in_=LG, func=AF.Exp, bias=NM1, scale=1.0), r=lgt.k, w=jk.k)
            op("act", lambda e: e.activation(out=E2, in_=M2, func=AF.Exp, bias=NM1, scale=1.0), r=lgt.k, w=lgt.k)
            op("dve", lambda e: e.tensor_scalar(out=E2, in0=E2, scalar1=1.0, scalar2=None, op0=ALU.add), r=lgt.k, w=lgt.k)
            op("dve", lambda e: e.reciprocal(out=E2, in_=E2), r=lgt.k, w=lgt.k)
            op("dve", lambda e: e.scalar_tensor_tensor(out=gates[:, tt, :], in0=EX, scalar=E2, in1=SEL, op0=ALU.mult, op1=ALU.mult),
               r=jk.k + lgt.k, w=[gates.k[tt]])
    else:
        router = None
    norm_off = L5.off
    L5.off = base
    acc = L5.get([128, NT, D], F32, NT)
    actT = [L5.get([128, 2, S], BF16) for _ in range(2)]
    sgt = [L5.get([128, 512], F32) for _ in range(2)]
    xt = [L5.get([128, D], F32) for _ in range(2)]
    L5end = L5.off
    L5.off = norm_off
    nE = NE if moe else 1
    F_ = DFE if moe else DFF
    nfg = F_ // 256
    its = [(e_, fg) for e_ in range(nE) for fg in range(nfg)]
    sets = [(C.wbufs[2 * i], C.wbufs[2 * i + 1]) for i in range(3)]

    def wdview(wdb):
        return wdb[:, 0:4, :].rearrange("p a b -> p (a b)").rearrange("p (c n) -> p c n", c=2)

    def load(i):
        e_, fg = its[i]
        wgu, wdb = sets[i % 3]
        wg = Wl["wg"][e_] if moe else Wl["wg"]
        wu = Wl["wu"][e_] if moe else Wl["wu"]
        wd = Wl["wd"][e_] if moe else Wl["wd"]
        f0 = fg * 256
        if os.environ.get("NOLOAD") and i > 2:
            return
        dma("pool", wgu[:, :, 0:256], wg[:, f0:f0 + 256].rearrange("(kc p) n -> p kc n", p=128), w=wgu.k)
        dma("pool", wgu[:, :, 256:512], wu[:, f0:f0 + 256].rearrange("(kc p) n -> p kc n", p=128), w=wgu.k)
        dma("pool", wdview(wdb), wd[f0:f0 + 256, :].rearrange("(c p) n -> p c n", p=128), w=wdb.k)

    def G_units(i):
        wgu, wdb = sets[i % 3]
        aT = actT[i % 2]
        units = []
        for tg in range(4):
            for fc in range(2):
                def u(tg=tg, fc=fc):
                    pG = psF[(2 * fc) % 4]; pU = psF[(2 * fc + 1) % 4]
                    for kc in range(8):
                        op("pe", lambda e: e.matmul(pG[:], lhsT=wgu[:, kc, fc * 128:(fc + 1) * 128], rhs=hT[:, kc, 1 + tg * 512:1 + (tg + 1) * 512],
                                                    start=(kc == 0), stop=(kc == 7)), r=wgu.k + [hT.k[tg]], w=pG.k)
                    for kc in range(8):
                        op("pe", lambda e: e.matmul(pU[:], lhsT=wgu[:, kc, 256 + fc * 128:256 + (fc + 1) * 128], rhs=hT[:, kc, 1 + tg * 512:1 + (tg + 1) * 512],
                                                    start=(kc == 0), stop=(kc == 7)), r=wgu.k + [hT.k[tg]], w=pU.k)
                    s_ = sgt[fc]
                    op("act", lambda e: e.activation(out=s_[:], in_=pG[:], func=AF.Silu), r=pG.k, w=s_.k)
                    op("dve", lambda e: e.tensor_tensor(out=aT[:, fc, tg * 512:(tg + 1) * 512], in0=pU[:], in1=s_[:], op=ALU.mult), r=pU.k + s_.k, w=aT.k)
                units.append(u)
        return units

    def D_units(i):
        e_, fg = its[i]
        wgu, wdb = sets[i % 3]
        wdv = wdview(wdb)
        aT = actT[i % 2]
        units = []
        for tt in range(NT):
            def u(tt=tt):
                for nh in range(2):
                    pD = psF[4 + (2 * tt + nh) % 4]
                    for fc in range(2):
                        op("pe", lambda e: e.matmul(pD[:], lhsT=aT[:, fc, tt * 128:(tt + 1) * 128], rhs=wdv[:, fc, nh * 512:(nh + 1) * 512],
                                                    start=(fc == 0), stop=(fc == 1)), r=aT.k + wdb.k, w=pD.k)
                    a_ = acc[:, tt, nh * 512:(nh + 1) * 512]
                    gsc = gates[:, tt, e_:e_ + 1] if moe else 1.0
                    rk = ([gates.k[tt]] if moe else [])
                    if i == 0:
                        op("dve", lambda e: e.tensor_scalar(out=a_, in0=pD[:], scalar1=gsc, scalar2=None, op0=ALU.mult), r=pD.k + rk, w=[acc.k[tt]])
                    else:
                        op("dve", lambda e: e.scalar_tensor_tensor(out=a_, in0=pD[:], scalar=gsc, in1=a_, op0=ALU.mult, op1=ALU.add),
                           r=pD.k + rk + [acc.k[tt]], w=[acc.k[tt]])
            units.append(u)
        return units

    n_it = len(its)
    load(0)
    if n_it > 1:
        load(1)
    g0 = G_units(0)

    def hook(tt):
        if tt % 4 == 3:
            tg = tt // 4
            g0[2 * tg]()
            g0[2 * tg + 1]()
    C.norm_phase(L5, lambda tt: (xs_d[tt], xs_d.k[tt]), C.A2, C.SH2, hT, router, hook)
    assert L5.off <= base + NT * D * 4, (L5.off, base)
    C.barrier()
    for i in range(n_it):
        if i + 2 < n_it:
            load(i + 2)
        d = D_units(i)
        g = G_units(i + 1) if i + 1 < n_it else []
        for k in range(8):
            if k < len(g):
                g[k]()
            d[2 * k]()
            d[2 * k + 1]()
    for tt in range(NT):
        x_ = xt[tt % 2]
        dma("sp", x_[:], xs_d[tt], r=[xs_d.k[tt]], w=x_.k)
        op("dve", lambda e: e.tensor_tensor(out=acc[:, tt, :], in0=acc[:, tt, :], in1=C.G2, op=ALU.mult), r=[acc.k[tt]] + C.modbc.k, w=[acc.k[tt]])
        op("dve", lambda e: e.tensor_tensor(out=x_[:], in0=x_[:], in1=acc[:, tt, :], op=ALU.add), r=x_.k + [acc.k[tt]], w=x_.k)
        dma("sp", xs_d[tt], x_[:], r=x_.k, w=[xs_d.k[tt]])
        if l in C.tapo:
            dma("sp", C.tapo[l][tt * 128:(tt + 1) * 128, :], x_[:], r=x_.k)
    C.barrier()


def _final(C):
    sc = C.sc; op = sc.op; dma = sc.dma
    xs_d = C.xs_d
    Lf = C.Lay()
    fg = Lf.get([128, D], F32)
    xt = [Lf.get([128, D], F32) for _ in range(2)]
    xo = [Lf.get([128, D], F32) for _ in range(2)]
    junk = Lf.get([128, D], BF16)
    ss = Lf.get([128, NT, 4], F32, NT)
    dma("sp", fg[:], C.final_g_in.partition_broadcast(128), w=fg.k)
    for tt in range(NT):
        x_ = xt[tt % 2]; o_ = xo[tt % 2]
        dma("sp", x_[:], xs_d[tt], r=[xs_d.k[tt]], w=x_.k)
        op("act", lambda e: e.activation(out=junk[:], in_=x_[:], func=AF.Square, accum_out=ss[:, tt, 0:1]), r=x_.k, w=junk.k + [ss.k[tt]])
        op("act", lambda e: e.activation(out=ss[:, tt, 1:2], in_=ss[:, tt, 0:1], func=AF.Sqrt, bias=RMS_EPS, scale=1.0 / D), r=[ss.k[tt]], w=[ss.k[tt]])
        op("dve", lambda e: e.reciprocal(out=ss[:, tt, 2:3], in_=ss[:, tt, 1:2]), r=[ss.k[tt]], w=[ss.k[tt]])
        op("dve", lambda e: e.scalar_tensor_tensor(out=o_[:], in0=x_[:], scalar=ss[:, tt, 2:3], in1=fg[:], op0=ALU.mult, op1=ALU.mult),
           r=x_.k + [ss.k[tt]] + fg.k, w=o_.k)
        dma("sp", C.out_d[tt * 128:(tt + 1) * 128, :], o_[:], r=o_.k)


_CONSTS = None


def make_in_maps(inputs, layers=(0, 1, 2, 3), cores=range(8)):
    global _CONSTS
    if _CONSTS is None:
        _CONSTS = host_consts()
    cst = _CONSTS
    f = lambda a: np.ascontiguousarray(np.asarray(a, dtype=np.float32))
    rel = np.asarray(inputs["rel_bias"], np.float32)
    abias = np.ascontiguousarray(rel[cst["_bucket"]].transpose(0, 2, 1))
    shared = {"k_" + k: f(v) for k, v in cst.items() if not k.startswith("_")}
    shared["abias"] = abias
    shared["final_g"] = f(inputs["final_g"])
    for l in layers:
        i = l // 2
        def put(nm, a):
            shared["%s_%d" % (nm, l)] = f(a)
        put("w_ada", inputs["w_ada"][l]); put("b_ada", inputs["b_ada"][l]); put("norm1_g", inputs["norm1_g"][l]); put("norm2_g", inputs["norm2_g"][l])
        put("w_in", inputs["w_in"][l]); put("w_out", inputs["w_out"][l]); put("mu", inputs["rwkv_mu"][l])
        put("w0", inputs["rwkv_w0"][l]); put("w2", inputs["rwkv_w2"][l]); put("a0", inputs["rwkv_a0"][l]); put("a2", inputs["rwkv_a2"][l])
        put("g2", inputs["rwkv_g2"][l]); put("k_k", inputs["rwkv_k_k"][l]); put("k_a", inputs["rwkv_k_a"][l])
        put("r_k", np.asarray(inputs["rwkv_r_k"][l]).reshape(-1)); put("ln_w", inputs["rwkv_ln_w"][l]); put("ln_b", inputs["rwkv_ln_b"][l])
        if l > 0:
            put("v0", inputs["rwkv_v0"][l - 1]); put("v1", inputs["rwkv_v1"][l - 1]); put("v2", inputs["rwkv_v2"][l - 1])
        if l % 2 == 0:
            put("wg", inputs["ffn_w_gate"][i]); put("wu", inputs["ffn_w_up"][i]); put("wd", inputs["ffn_w_down"][i])
        else:
            put("rw", np.asarray(inputs["moe_router_w"][i]).T); put("rb", inputs["moe_router_b"][i])
            put("wg", inputs["moe_w_gate"][i]); put("wu", inputs["moe_w_up"][i]); put("wd", inputs["moe_w_down"][i])
    maps = []
    for b in cores:
        m = dict(shared)
        m["x"] = f(inputs["x"][b])
        m["c"] = f(inputs["c"][b])
        maps.append(m)
    return maps


def kernel(**inputs):
    nc = bass.Bass("TRN2", target_bir_lowering=False)
    build(nc)
    maps = make_in_maps(inputs)
    res = run_bass_kernel_spmd(nc, maps, core_ids=list(range(8)))
    out = np.stack([np.asarray(r["out"], dtype=np.float32) for r in res.results], axis=0)
    return out
```
